# Optimizing a Trainium2 kernel written in Bass

```python
import jax, jax.numpy as jnp
from jax import lax
import numpy as np

D_MODEL = 2048
BATCH = 8
SEQ = 2048
DEPTH = 4

CHUNK = 64
D_MIX = D_MODEL
HEAD_DIM = 128
W_ATTN = D_MIX // 2
H_ATTN = W_ATTN // HEAD_DIM
W_RET = D_MIX - W_ATTN
H_RET = W_RET // HEAD_DIM
LEFT_CHUNKS = 8
BAND = (LEFT_CHUNKS + 1) * CHUNK
REL_CLIP = 128
N_REL = 2 * REL_CLIP + 1
ROPE_BASE = 10000.0
IN_COLS = 3 * W_ATTN + 4 * W_RET
PEER_HEADS = 8
PEER_DQ = 256
N_KEYS = 128
N_EXPERTS = N_KEYS * N_KEYS
PEER_TOPK = 16
PEER_TOKEN_BLOCK = 128
EPS = 1e-6
NEG_INF = -1e30

kernel_name = "hybrid_chunkattn_retention_peer"


def rms_norm(x, g):
    xf = x.astype(jnp.float32)
    y = xf * lax.rsqrt(jnp.mean(xf * xf, axis=-1, keepdims=True) + EPS)
    return (y * g.astype(jnp.float32)).astype(x.dtype)


def split_heads(t, n_heads):
    b, s, _ = t.shape
    return t.reshape(b, s, n_heads, HEAD_DIM).transpose(0, 2, 1, 3)


def merge_heads(t):
    b, h, s, d = t.shape
    return t.transpose(0, 2, 1, 3).reshape(b, s, h * d)


def rotary(x, pos):
    half = x.shape[-1] // 2
    inv_freq = ROPE_BASE ** (-jnp.arange(half, dtype=jnp.float32) / half)
    ang = pos.astype(jnp.float32)[:, None] * inv_freq[None, :]
    cos, sin = jnp.cos(ang), jnp.sin(ang)
    xf = x.astype(jnp.float32)
    x1, x2 = xf[..., :half], xf[..., half:]
    return jnp.concatenate([x1 * cos - x2 * sin, x2 * cos + x1 * sin], axis=-1).astype(x.dtype)


def chunked_rel_attention(q, k, v, rel_bias):
    b, h, s, dh = q.shape
    n_chunks = s // CHUNK
    pad = LEFT_CHUNKS * CHUNK
    k_pad = jnp.pad(k, ((0, 0), (0, 0), (pad, 0), (0, 0)))
    v_pad = jnp.pad(v, ((0, 0), (0, 0), (pad, 0), (0, 0)))
    q_off = jnp.arange(CHUNK)[:, None] + pad
    k_off = jnp.arange(BAND)[None, :]
    rel = jnp.clip(q_off - k_off, -REL_CLIP, REL_CLIP) + REL_CLIP
    bias = rel_bias[:, rel].astype(jnp.float32)
    scale = dh ** -0.5

    def one_chunk(c):
        start = c * CHUNK
        qc = lax.dynamic_slice_in_dim(q, start, CHUNK, axis=2)
        kb = lax.dynamic_slice_in_dim(k_pad, start, BAND, axis=2)
        vb = lax.dynamic_slice_in_dim(v_pad, start, BAND, axis=2)
        sc = jnp.einsum('bhqd,bhkd->bhqk', qc, kb, preferred_element_type=jnp.float32) * scale + bias
        valid = (start + k_off - pad) >= 0
        sc = jnp.where(valid[None, None], sc, NEG_INF)
        p = jax.nn.softmax(sc, axis=-1)
        return jnp.einsum('bhqk,bhkd->bhqd', p.astype(vb.dtype), vb)

    out = lax.map(one_chunk, jnp.arange(n_chunks))
    return out.transpose(1, 2, 0, 3, 4).reshape(b, h, s, dh)


def retention_chunkwise(q, k, v):
    b, h, s, dh = q.shape
    nc = s // CHUNK
    log_gamma = jnp.log1p(-(2.0 ** (-5.0 - jnp.arange(h, dtype=jnp.float32))))
    pos = jnp.arange(CHUNK, dtype=jnp.float32)
    diff = pos[:, None] - pos[None, :]
    decay_intra = jnp.where(diff >= 0, jnp.exp(jnp.maximum(diff, 0.0) * log_gamma[:, None, None]), 0.0)
    q_decay = jnp.exp((pos + 1.0)[None, :] * log_gamma[:, None])
    k_decay = jnp.exp((CHUNK - 1.0 - pos)[None, :] * log_gamma[:, None])
    chunk_decay = jnp.exp(CHUNK * log_gamma)

    qc = q.astype(jnp.float32).reshape(b, h, nc, CHUNK, dh)
    kc = k.astype(jnp.float32).reshape(b, h, nc, CHUNK, dh) * (dh ** -0.5)
    vc = v.astype(jnp.float32).reshape(b, h, nc, CHUNK, dh)

    scores = jnp.einsum('bhnqd,bhnkd->bhnqk', qc, kc) * decay_intra[None, :, None]
    o_intra = jnp.einsum('bhnqk,bhnkd->bhnqd', scores, vc)

    kv = jnp.einsum('bhnkd,bhnke->bhnde', kc * k_decay[None, :, None, :, None], vc)

    def step(state, kv_n):
        return chunk_decay[None, :, None, None] * state + kv_n, state

    state0 = jnp.zeros((b, h, dh, dh), jnp.float32)
    _, states = lax.scan(step, state0, jnp.moveaxis(kv, 2, 0))
    o_cross = jnp.einsum('bhnqd,nbhde->bhnqe', qc * q_decay[None, :, None, :, None], states)
    return (o_intra + o_cross).reshape(b, h, s, dh)


def peer_ffn(h, wq, sub_keys, u_tab, v_tab):
    t = h.shape[0]
    q = (h @ wq).reshape(t, PEER_HEADS, 2, PEER_DQ // 2)
    s = jnp.einsum('thpd,hpkd->thpk', q, sub_keys, preferred_element_type=jnp.float32)
    s_top, i_top = lax.top_k(s, PEER_TOPK)
    cand = (s_top[:, :, 0, :, None] + s_top[:, :, 1, None, :]).reshape(t, PEER_HEADS, PEER_TOPK * PEER_TOPK)
    g_top, c_idx = lax.top_k(cand, PEER_TOPK)
    i1 = jnp.take_along_axis(i_top[:, :, 0], c_idx // PEER_TOPK, axis=-1)
    i2 = jnp.take_along_axis(i_top[:, :, 1], c_idx % PEER_TOPK, axis=-1)
    n_sel = PEER_HEADS * PEER_TOPK
    experts = (i1 * N_KEYS + i2).reshape(t, n_sel)
    gates = jax.nn.softmax(g_top, axis=-1).reshape(t, n_sel)

    nb = t // PEER_TOKEN_BLOCK

    def block(args):
        hb, eb, gb = args
        u = u_tab[eb]
        v = v_tab[eb]
        a = jax.nn.gelu(jnp.einsum('td,ted->te', hb, u, preferred_element_type=jnp.float32), approximate=False)
        return jnp.einsum('te,ted->td', (gb * a).astype(v.dtype), v)

    out = lax.map(block, (h.reshape(nb, PEER_TOKEN_BLOCK, -1),
                          experts.reshape(nb, PEER_TOKEN_BLOCK, n_sel),
                          gates.reshape(nb, PEER_TOKEN_BLOCK, n_sel)))
    return out.reshape(t, -1)


def hybrid_layer(x, norm1_g, w_in, qa_g, ka_g, rel_bias, attn_g, ret_g, w_out,
                 norm2_g, peer_wq, peer_keys, peer_u, peer_v):
    b, s, d = x.shape
    hn = rms_norm(x, norm1_g)
    proj = hn @ w_in
    qa, ka, va, qr, kr, vr, gr = jnp.split(
        proj, [W_ATTN, 2 * W_ATTN, 3 * W_ATTN, 3 * W_ATTN + W_RET,
               3 * W_ATTN + 2 * W_RET, 3 * W_ATTN + 3 * W_RET], axis=-1)

    qa = rms_norm(split_heads(qa, H_ATTN), qa_g)
    ka = rms_norm(split_heads(ka, H_ATTN), ka_g)
    oa = chunked_rel_attention(qa, ka, split_heads(va, H_ATTN), rel_bias)
    oa = merge_heads(rms_norm(oa, attn_g.reshape(H_ATTN, 1, HEAD_DIM)))

    pos = jnp.arange(s)
    qr = rotary(split_heads(qr, H_RET), pos)
    kr = rotary(split_heads(kr, H_RET), pos)
    orr = retention_chunkwise(qr, kr, split_heads(vr, H_RET))
    orr = merge_heads(rms_norm(orr, ret_g.reshape(H_RET, 1, HEAD_DIM))).astype(x.dtype)
    orr = jax.nn.silu(gr) * orr

    x = x + jnp.concatenate([oa, orr], axis=-1) @ w_out

    h2 = rms_norm(x, norm2_g).reshape(b * s, d)
    x = x + peer_ffn(h2, peer_wq, peer_keys, peer_u, peer_v).reshape(b, s, d)
    return x


def setup_inputs(seed: int = 0) -> dict:
    key = jax.random.key(seed)
    ks = jax.random.split(key, 15)

    def nrm(k, shape, scale):
        return jax.random.normal(k, shape, jnp.float32) * scale

    def gain(k, shape):
        return 1.0 + 0.01 * jax.random.normal(k, shape, jnp.float32)

    return {
        "x": nrm(ks[0], (BATCH, SEQ, D_MODEL), 1.0),
        "norm1_g": gain(ks[1], (DEPTH, D_MODEL)),
        "w_in": nrm(ks[2], (DEPTH, D_MODEL, IN_COLS), D_MODEL ** -0.5),
        "qa_norm_g": gain(ks[3], (DEPTH, HEAD_DIM)),
        "ka_norm_g": gain(ks[4], (DEPTH, HEAD_DIM)),
        "rel_bias": nrm(ks[5], (DEPTH, H_ATTN, N_REL), 0.1),
        "attn_out_g": gain(ks[6], (DEPTH, W_ATTN)),
        "ret_out_g": gain(ks[7], (DEPTH, W_RET)),
        "w_out": nrm(ks[8], (DEPTH, D_MIX, D_MODEL), D_MIX ** -0.5),
        "norm2_g": gain(ks[9], (DEPTH, D_MODEL)),
        "peer_wq": nrm(ks[10], (DEPTH, D_MODEL, PEER_HEADS * PEER_DQ), D_MODEL ** -0.5),
        "peer_subkeys": nrm(ks[11], (DEPTH, PEER_HEADS, 2, N_KEYS, PEER_DQ // 2), (PEER_DQ // 2) ** -0.5),
        "peer_u": nrm(ks[12], (DEPTH, N_EXPERTS, D_MODEL), D_MODEL ** -0.5),
        "peer_v": nrm(ks[13], (DEPTH, N_EXPERTS, D_MODEL), 0.5 * PEER_HEADS ** -0.5),
    }


def reference(x, norm1_g, w_in, qa_norm_g, ka_norm_g, rel_bias, attn_out_g, ret_out_g,
              w_out, norm2_g, peer_wq, peer_subkeys, peer_u, peer_v):
    for l in range(DEPTH):
        x = hybrid_layer(x, norm1_g[l], w_in[l], qa_norm_g[l], ka_norm_g[l], rel_bias[l],
                         attn_out_g[l], ret_out_g[l], w_out[l], norm2_g[l],
                         peer_wq[l], peer_subkeys[l], peer_u[l], peer_v[l])
    return x
```

```python
import numpy as np
import concourse.bass as bass
import concourse.mybir as mybir
from concourse.bass_utils import run_bass_kernel_spmd
from contextlib import ExitStack

F32 = mybir.dt.float32
BF16 = mybir.dt.bfloat16
U32 = mybir.dt.uint32
I32 = mybir.dt.int32
AF = mybir.ActivationFunctionType
ALU = mybir.AluOpType
AX = mybir.AxisListType

D = 2048
KC = 16
HD = 128
NH = 8
EPS = 1e-6
NEXP = 16384
IN_COLS = 7168
SCALE = HD ** -0.5
EPOCH = 20000

C_ID, C_SW, C_ON, C_IO, C_I16, C_DEC, C_QD, C_KD, C_CD = 0, 128, 256, 384, 512, 2560, 3072, 3584, 3592
CW = 3600


class Sched:
    ENGS = ("pe", "act", "dve", "pool", "sp")

    def __init__(self, nc, stack, n_dma=32, n_ep=8):
        self.nc = nc
        self.prog = {e: [] for e in self.ENGS}
        self.esems = {e: [stack.enter_context(nc.semaphore(f"s_{e}_{i}")) for i in range(n_ep)] for e in self.ENGS}
        self.eidx = {e: 0 for e in self.ENGS}
        self.ecnt = {e: 0 for e in self.ENGS}
        self.dsems = [stack.enter_context(nc.semaphore(f"s_dma_{i}")) for i in range(n_dma)]
        self.dval = {s: 0 for s in self.dsems}
        self.di = 0
        self.res = {}
        self.waited = {e: {} for e in self.ENGS}
        self.pe_sems = set(self.esems["pe"])
        self.banks = None
        self.bi = 0
        self.ninstr = 0

    def _deps(self, reads, writes):
        deps = {}

        def add(s, v):
            if deps.get(s, 0) < v:
                deps[s] = v
        for r in reads:
            st = self.res.get(r)
            if st and st[0]:
                add(*st[0])
        for w in writes:
            st = self.res.get(w)
            if st:
                if st[0]:
                    add(*st[0])
                for s, v in st[1].items():
                    add(s, v)
        return deps

    def _waits(self, eng, deps):
        wd = self.waited[eng]
        for s, v in deps.items():
            if eng == "pe" and s in self.pe_sems:
                continue
            if wd.get(s, 0) >= v:
                continue
            wd[s] = v
            self.prog[eng].append(lambda e, s=s, v=v: e.wait_ge(s, v))
            self.ninstr += 1

    def _mark(self, reads, writes, s, v):
        for r in reads:
            st = self.res.setdefault(r, [None, {}])
            if st[1].get(s, 0) < v:
                st[1][s] = v
        for w in writes:
            self.res[w] = [(s, v), {}]

    def _tick(self, eng):
        if self.ecnt[eng] >= EPOCH:
            self.eidx[eng] += 1
            self.ecnt[eng] = 0
        s = self.esems[eng][self.eidx[eng]]
        self.ecnt[eng] += 1
        return s, self.ecnt[eng]

    def group(self, eng, fns, reads=(), writes=()):
        self._waits(eng, self._deps(reads, writes))
        s, v = self._tick(eng)
        n = len(fns)
        for i, fn in enumerate(fns):
            if i == n - 1:
                self.prog[eng].append(lambda e, fn=fn, s=s: fn(e).then_inc(s, 1))
            else:
                self.prog[eng].append(fn)
        self.ninstr += n
        self._mark(reads, writes, s, v)

    def op(self, eng, fn, reads=(), writes=()):
        self.group(eng, [fn], reads, writes)

    def dma(self, q, out, in_, reads=(), writes=()):
        s = self.dsems[self.di % len(self.dsems)]
        self.di += 1
        deps = self._deps(reads, writes)
        prev = self.dval[s]
        if prev > 0 and deps.get(s, 0) < prev:
            deps[s] = prev
        self._waits(q, deps)
        self.dval[s] = prev + 16
        self.prog[q].append(lambda e, s=s: e.dma_start(out=out, in_=in_).then_inc(s, 16))
        self.ninstr += 1
        self._mark(reads, writes, s, prev + 16)

    def barrier(self):
        cur = {}
        for e in self.ENGS:
            if self.ecnt[e] > 0:
                cur[self.esems[e][self.eidx[e]]] = self.ecnt[e]
        for s, v in self.dval.items():
            if v > 0:
                cur[s] = v
        for e in self.ENGS:
            wd = self.waited[e]
            for s, v in cur.items():
                if wd.get(s, 0) >= v:
                    continue
                wd[s] = v
                self.prog[e].append(lambda en, s=s, v=v: en.wait_ge(s, v))
        self.res = {}

    def finish(self):
        for s, v in self.dval.items():
            if v > 0 and self.waited["sp"].get(s, 0) < v:
                self.prog["sp"].append(lambda e, s=s, v=v: e.wait_ge(s, v))

    def bank(self):
        b = self.banks[self.bi % 8]
        self.bi += 1
        return b


class Tile:
    def __init__(self, ap, key):
        self.ap = ap
        self.key = key

    def __getitem__(self, k):
        return self.ap[k]


class Arena:
    def __init__(self, big, nbytes):
        self.big = big
        self.nbytes = nbytes
        self.off = 0
        self.n = 0

    def reset(self):
        self.off = 0

    def tile(self, name, free, dtype, parts=128):
        esz = 2 if dtype == BF16 else 4
        n = int(np.prod(free))
        nb = (n * esz + 31) // 32 * 32
        assert self.off + nb <= self.nbytes, f"arena overflow at {name}: {self.off}+{nb}>{self.nbytes}"
        a = self.big[:, self.off // 2:(self.off + n * esz) // 2]
        self.off += nb
        if dtype != BF16:
            a = a.bitcast(dtype)
        if len(free) == 2:
            a = a.rearrange("p (a b) -> p a b", b=free[1])
        elif len(free) == 3:
            a = a.rearrange("p (a b c) -> p a b c", b=free[1], c=free[2])
        if parts != 128:
            a = a[0:parts]
        self.n += 1
        return Tile(a, f"{name}#{self.n}")


def build(T, DEPTH, debug=False):
    NT = T // 128
    NB = T // 512
    NCH = T // 64
    assert T % 512 == 0
    TG = min(8, NT)
    nc = bass.Bass("TRN2", target_bir_lowering=False)

    def din(name, shape, dt=F32):
        return nc.dram_tensor(name, list(shape), dt, kind="ExternalInput").ap()

    x_in = din("x", [T, D])
    w_in = din("w_in", [DEPTH, D, IN_COLS])
    w_out = din("w_out", [DEPTH, D, D])
    wq = din("wq", [DEPTH, D, D])
    skT = din("skT", [DEPTH, 16, 128, 128])
    uT = din("uT", [DEPTH, D, NEXP])
    pv = din("pv", [DEPTH, NEXP, D])
    g1 = din("g1", [DEPTH, D])
    g2 = din("g2", [DEPTH, D])
    gvec = din("gvec", [DEPTH, 128, 18])
    biasT = din("biasT", [DEPTH, NH, 640, 128])
    cst = din("cst", [128, CW])
    cosT = din("cosT", [128, T])
    sinT = din("sinT", [128, T])
    y = nc.dram_tensor("y", [T, D], F32, kind="ExternalOutput").ap()
    skind = "ExternalOutput" if debug else "Internal"
    hnT_d = nc.dram_tensor("hnT_d", [D, T], BF16, kind=skind).ap()
    catT_d = nc.dram_tensor("catT_d", [D, T], BF16, kind=skind).ap()
    h2T_d = nc.dram_tensor("h2T_d", [D, T], BF16, kind=skind).ap()
    G_d = nc.dram_tensor("G_d", [128, 128, T], BF16, kind=skind).ap()
    gaT_d = nc.dram_tensor("gaT_d", [NEXP, T], BF16, kind="Internal").ap()

    ARENA_BYTES = 168 * 1024
    with ExitStack() as stack:
        ec = stack.enter_context
        S = Sched(nc, stack)
        cst_sb = ec(nc.sbuf_tensor("cst_sb", [128, CW], F32))
        cbf = ec(nc.sbuf_tensor("cbf", [128, 256], BF16))
        gv_sb = ec(nc.sbuf_tensor("gv_sb", [128, 20], F32))
        big = ec(nc.sbuf_tensor("arena", [128, ARENA_BYTES // 2], BF16))
        S.banks = [Tile(ec(nc.psum_tensor(f"bank{i}", [128, 512], F32))[:], f"bank{i}") for i in range(8)]
        AR = Arena(big, ARENA_BYTES)

        ident_f = cst_sb[:, C_ID:C_ID + 128]
        swap_f = cst_sb[:, C_SW:C_SW + 128]
        ones_f = cst_sb[:, C_ON:C_ON + 128]
        iota_f = cst_sb[:, C_IO:C_IO + 128]
        iota16 = cst_sb[:, C_I16:C_I16 + 2048]
        ident_b = cbf[:, 0:128]
        ones_b = cbf[:, 128:256]
        CST = "cst"

        S.dma("sp", cst_sb[:], cst, writes=[CST])
        S.op("dve", lambda e: e.tensor_copy(out=cbf[:, 0:128], in_=cst_sb[:, C_ID:C_ID + 128]), reads=[CST], writes=["cbf"])
        S.op("dve", lambda e: e.tensor_copy(out=cbf[:, 128:256], in_=cst_sb[:, C_ON:C_ON + 128]), reads=[CST], writes=["cbf"])
        CONST = [CST, "cbf"]

        def rowsT(d_ap):
            return d_ap.rearrange("(kc p) t -> p kc t", p=128)

        def norm_tiles(l, gsrc, src_fn, dstT_d, phase_tag, post_fn=None):
            gbc = AR.tile("gbc", [D], F32)
            S.dma("sp", gbc[:], gsrc[l:l + 1, :].broadcast_to([128, D]), writes=[gbc.key])
            junk = AR.tile("junk", [D], BF16)
            hn = [AR.tile("hn", [D], BF16) for _ in range(2)]
            hT = [AR.tile("hT", [KC, 512], BF16) for _ in range(2)]
            ss = [AR.tile("ss", [4], F32) for _ in range(2)]
            for i in range(NT):
                xt = src_fn(i)
                s_ = ss[i % 2]
                h_ = hn[i % 2]
                ht = hT[(i // 4) % 2]
                S.op("dve", lambda e, s_=s_: e.memset(s_[:, 0:1], 0.0), writes=[s_.key])
                S.op("act", lambda e, s_=s_, xt=xt: e.activation(out=junk[:], in_=xt[:], func=AF.Square, accum_out=s_[:, 0:1]),
                     reads=[xt.key], writes=[s_.key, junk.key])
                S.op("act", lambda e, s_=s_: e.activation(out=s_[:, 1:2], in_=s_[:, 0:1], func=AF.Sqrt, scale=1.0 / D, bias=EPS),
                     reads=[s_.key], writes=[s_.key])
                S.op("dve", lambda e, s_=s_: e.reciprocal(out=s_[:, 2:3], in_=s_[:, 1:2]), reads=[s_.key], writes=[s_.key])
                S.op("dve", lambda e, s_=s_, xt=xt, h_=h_: e.scalar_tensor_tensor(out=h_[:], in0=xt[:], scalar=s_[:, 2:3], in1=gbc[:],
                                                                                 op0=ALU.mult, op1=ALU.mult),
                     reads=[xt.key, s_.key, gbc.key], writes=[h_.key])
                for half in range(2):
                    bk = S.bank()
                    bv = bk.ap.bitcast(BF16)
                    S.group("pe", [lambda e, c=c, half=half, bv=bv, h_=h_: e.transpose(out=bv[:, c * 128:(c + 1) * 128],
                                                                                       in_=h_[:, (half * 8 + c) * 128:(half * 8 + c + 1) * 128],
                                                                                       identity=ident_b) for c in range(8)],
                            reads=[h_.key] + CONST, writes=[bk.key])
                    S.op("act", lambda e, half=half, bv=bv, ht=ht, i=i: e.copy(out=ht[:, half * 8:(half + 1) * 8, (i % 4) * 128:(i % 4 + 1) * 128],
                                                                             in_=bv.rearrange("p (c t) -> p c t", t=128)),
                         reads=[bk.key], writes=[ht.key])
                if post_fn:
                    post_fn(i, xt)
                if i % 4 == 3:
                    tb = i // 4
                    S.dma("sp", rowsT(dstT_d)[:, :, tb * 512:(tb + 1) * 512], ht[:], reads=[ht.key], writes=[phase_tag])

        def fm_rmsnorm(src_ap, src_keys, gcol_ap, gkeys, out_ap, out_keys, tmp):
            sq, rt, ri = tmp
            bk = S.bank()
            S.op("act", lambda e: e.activation(out=sq[:], in_=src_ap, func=AF.Square), reads=src_keys, writes=[sq.key])
            S.op("pe", lambda e: e.matmul(bk.ap, lhsT=ones_f, rhs=sq[:], start=True, stop=True), reads=[sq.key] + CONST, writes=[bk.key])
            S.op("act", lambda e: e.activation(out=rt[:], in_=bk.ap, func=AF.Sqrt, scale=1.0 / HD, bias=EPS), reads=[bk.key], writes=[rt.key])
            S.op("dve", lambda e: e.reciprocal(out=ri[:], in_=rt[:]), reads=[rt.key], writes=[ri.key])
            S.op("dve", lambda e: e.scalar_tensor_tensor(out=out_ap, in0=src_ap, scalar=gcol_ap, in1=ri[:], op0=ALU.mult, op1=ALU.mult),
                 reads=src_keys + [ri.key] + gkeys, writes=out_keys)

        def load_w(dst, src_ap, l):
            S.dma("pool", dst[:], src_ap, writes=[dst.key])

        def proj_fm(bk, wt, hb, n=512, c0=0):
            S.group("pe", [lambda e, kc=kc: e.matmul(bk.ap[:, 0:n], lhsT=wt[:, kc, :], rhs=hb[:, kc, c0:c0 + n], start=(kc == 0), stop=(kc == KC - 1))
                           for kc in range(KC)], reads=[wt.key, hb.key], writes=[bk.key])

        w_in_v = [w_in[l].rearrange("(kc p) c -> p kc c", p=128) for l in range(DEPTH)]

        for l in range(DEPTH):
            xsrc = x_in if l == 0 else y
            AR.reset()
            xts = [AR.tile("xt", [D], F32) for _ in range(2)]

            def src1(i):
                xt = xts[i % 2]
                S.dma("sp", xt[:], xsrc[i * 128:(i + 1) * 128, :], reads=["ydram"], writes=[xt.key])
                return xt
            norm_tiles(l, g1, src1, hnT_d, "hnT")
            S.dma("sp", gv_sb[:, 0:18], gvec[l], writes=["gv"])
            S.op("dve", lambda e: e.tensor_scalar(out=gv_sb[:, 18:19], in0=gv_sb[:, 0:1], scalar1=SCALE, scalar2=None, op0=ALU.mult),
                 reads=["gv"], writes=["gv"])
            S.barrier()

            AR.reset()
            hblk = [AR.tile("hblk", [KC, 512], BF16) for _ in range(2)]
            wts = [AR.tile("wt", [KC, 128], BF16) for _ in range(6)]
            q_sb = AR.tile("q_sb", [T], BF16)
            k_sb = AR.tile("k_sb", [T], BF16)
            qd_sb = AR.tile("qd_sb", [T], BF16)
            v_sb = AR.tile("v_sb", [NT, 128], BF16)
            kd_c = AR.tile("kd_c", [NCH, 128], BF16)
            vr_c = AR.tile("vr_c", [NCH, 128], BF16)
            gs_sb = AR.tile("gs_sb", [T], F32)
            o_sb = AR.tile("o_sb", [T], F32)
            cat_sb = [AR.tile("cat_sb", [T], BF16) for _ in range(2)]
            bias_sb = AR.tile("bias_sb", [5, 128], F32)
            pTs = [AR.tile("pT", [5, 128], BF16) for _ in range(2)]
            e1s = [AR.tile("e1", [5, 128], F32) for _ in range(2)]
            tmpn = [[AR.tile("sq", [512], F32), AR.tile("rt", [512], F32), AR.tile("ri", [512], F32)] for _ in range(2)]
            xs_t = [AR.tile("xs", [512], F32) for _ in range(2)]
            t1_t = [AR.tile("t1", [512], F32) for _ in range(2)]
            t2_t = [AR.tile("t2", [512], F32) for _ in range(2)]
            rot_t = [AR.tile("rot", [512], F32) for _ in range(2)]
            cs_t = [AR.tile("cs", [2, 512], F32) for _ in range(2)]
            rz_t = [AR.tile("rz", [128], F32) for _ in range(2)]
            sm_t = [AR.tile("sm", [64], BF16) for _ in range(2)]
            st_f = AR.tile("st_f", [128], F32)
            st_b = [AR.tile("st_b", [128], BF16) for _ in range(2)]
            nrm_f = [AR.tile("nrm_f", [512], F32) for _ in range(2)]
            cnt = {"hb": 0, "tn": 0, "w": 0, "cat": 0, "pt": 0, "sm": 0, "sb": 0, "x": 0}

            def next_hblk(tb):
                hb = hblk[cnt["hb"] % 2]
                cnt["hb"] += 1
                S.dma("sp", hb[:], rowsT(hnT_d)[:, :, tb * 512:(tb + 1) * 512], reads=["hnT"], writes=[hb.key])
                return hb

            def next_tmp():
                cnt["tn"] += 1
                return tmpn[cnt["tn"] % 2]

            def get_w(col0):
                wt = wts[cnt["w"] % 6]
                cnt["w"] += 1
                load_w(wt, w_in_v[l][:, :, col0:col0 + 128], l)
                return wt

            for h in range(NH):
                wq_t = get_w(h * 128)
                wk_t = get_w(1024 + h * 128)
                wv_t = get_w(2048 + h * 128)
                S.dma("sp", bias_sb[:], biasT[l, h].rearrange("(a p) q -> p a q", p=128), writes=[bias_sb.key])
                for tb in range(NB):
                    hb = next_hblk(tb)
                    for wt, dst, gc in ((wq_t, q_sb, 18), (wk_t, k_sb, 1)):
                        bk = S.bank()
                        proj_fm(bk, wt, hb)
                        fm_rmsnorm(bk.ap, [bk.key], gv_sb[:, gc:gc + 1], ["gv"], dst[:, tb * 512:(tb + 1) * 512], [dst.key], next_tmp())
                    bk = S.bank()
                    for ti in range(4):
                        S.group("pe", [lambda e, kc=kc, ti=ti, bk=bk, hb=hb, wv_t=wv_t: e.matmul(bk.ap[:, ti * 128:(ti + 1) * 128], lhsT=hb[:, kc, ti * 128:(ti + 1) * 128],
                                                                                               rhs=wv_t[:, kc, :], start=(kc == 0), stop=(kc == KC - 1)) for kc in range(KC)],
                                reads=[hb.key, wv_t.key], writes=[bk.key])
                    S.op("act", lambda e, bk=bk, tb=tb: e.copy(out=v_sb[:, tb * 4:(tb + 1) * 4, :], in_=bk.ap.rearrange("p (a d) -> p a d", d=128)),
                         reads=[bk.key], writes=[v_sb.key])
                for j in range(NT):
                    a0 = max(0, 4 - j)
                    pT = pTs[cnt["pt"] % 2]
                    e1 = e1s[cnt["pt"] % 2]
                    cnt["pt"] += 1
                    bkA = S.bank()
                    bkB = S.bank()
                    for a in range(a0, 5):
                        kt = j - 4 + a
                        bk_, col = (bkA, a) if a < 4 else (bkB, 0)
                        S.op("pe", lambda e, bk_=bk_, col=col, kt=kt, j=j: e.matmul(bk_.ap[:, col * 128:(col + 1) * 128], lhsT=k_sb[:, kt * 128:(kt + 1) * 128],
                                                                                  rhs=q_sb[:, j * 128:(j + 1) * 128], start=True, stop=True),
                             reads=[k_sb.key, q_sb.key], writes=[bk_.key])
                    if a0 < 4:
                        S.op("dve", lambda e, a0=a0, bkA=bkA, e1=e1: e.tensor_tensor(out=e1[:, a0:4, :], in0=bkA.ap.rearrange("p (a q) -> p a q", q=128)[:, a0:4, :],
                                                                                    in1=bias_sb[:, a0:4, :], op=ALU.add),
                             reads=[bkA.key, bias_sb.key], writes=[e1.key])
                    S.op("dve", lambda e, bkB=bkB, e1=e1: e.tensor_tensor(out=e1[:, 4, :], in0=bkB.ap[:, 0:128], in1=bias_sb[:, 4, :], op=ALU.add),
                         reads=[bkB.key, bias_sb.key], writes=[e1.key])
                    S.op("act", lambda e, a0=a0, e1=e1, pT=pT: e.activation(out=pT[:, a0:5, :], in_=e1[:, a0:5, :], func=AF.Exp),
                         reads=[e1.key], writes=[pT.key])
                    bko = S.bank()
                    fns = []
                    for a in range(a0, 5):
                        kt = j - 4 + a
                        fns.append(lambda e, a=a, kt=kt, bko=bko, pT=pT, a0=a0: e.matmul(bko.ap[:, 0:128], lhsT=v_sb[:, kt, :], rhs=pT[:, a, :],
                                                                                        start=(a == a0), stop=(a == 4)))
                    for a in range(a0, 5):
                        fns.append(lambda e, a=a, bko=bko, pT=pT, a0=a0: e.matmul(bko.ap[:, 128:256], lhsT=ones_b, rhs=pT[:, a, :],
                                                                                 start=(a == a0), stop=(a == 4)))
                    S.group("pe", fns, reads=[v_sb.key, pT.key] + CONST, writes=[bko.key])
                    rz = rz_t[j % 2]
                    S.op("dve", lambda e, rz=rz, bko=bko: e.reciprocal(out=rz[:], in_=bko.ap[:, 128:256]), reads=[bko.key], writes=[rz.key])
                    S.op("dve", lambda e, rz=rz, bko=bko, j=j: e.tensor_tensor(out=o_sb[:, j * 128:(j + 1) * 128], in0=bko.ap[:, 0:128], in1=rz[:], op=ALU.mult),
                         reads=[bko.key, rz.key], writes=[o_sb.key])
                cat = cat_sb[cnt["cat"] % 2]
                cnt["cat"] += 1
                for tb in range(NB):
                    fm_rmsnorm(o_sb[:, tb * 512:(tb + 1) * 512], [o_sb.key], gv_sb[:, 2 + h:3 + h], ["gv"], cat[:, tb * 512:(tb + 1) * 512], [cat.key], next_tmp())
                S.dma("sp", catT_d[h * 128:(h + 1) * 128, :], cat[:], reads=[cat.key], writes=["catT"])

            for h in range(NH):
                wq_t = get_w(3072 + h * 128)
                wk_t = get_w(4096 + h * 128)
                wv_t = get_w(5120 + h * 128)
                wg_t = get_w(6144 + h * 128)
                for tb in range(NB):
                    hb = next_hblk(tb)
                    cs = cs_t[tb % 2]
                    S.dma("sp", cs[:, 0, :], cosT[:, tb * 512:(tb + 1) * 512], writes=[cs.key])
                    S.dma("sp", cs[:, 1, :], sinT[:, tb * 512:(tb + 1) * 512], writes=[cs.key])
                    for which, wt in (("q", wq_t), ("k", wk_t)):
                        bk = S.bank()
                        proj_fm(bk, wt, hb)
                        xs = xs_t[cnt["x"] % 2]
                        t1 = t1_t[cnt["x"] % 2]
                        t2 = t2_t[cnt["x"] % 2]
                        rot = rot_t[cnt["x"] % 2]
                        cnt["x"] += 1
                        S.op("act", lambda e, xs=xs, bk=bk: e.copy(out=xs[:], in_=bk.ap), reads=[bk.key], writes=[xs.key])
                        bk2 = S.bank()
                        S.op("pe", lambda e, bk2=bk2, xs=xs: e.matmul(bk2.ap, lhsT=swap_f, rhs=xs[:], start=True, stop=True), reads=[xs.key] + CONST, writes=[bk2.key])
                        S.op("dve", lambda e, t1=t1, xs=xs, cs=cs: e.tensor_tensor(out=t1[:], in0=xs[:], in1=cs[:, 0, :], op=ALU.mult),
                             reads=[xs.key, cs.key], writes=[t1.key])
                        S.op("dve", lambda e, t2=t2, bk2=bk2, cs=cs: e.tensor_tensor(out=t2[:], in0=bk2.ap, in1=cs[:, 1, :], op=ALU.mult),
                             reads=[bk2.key, cs.key], writes=[t2.key])
                        sl = slice(tb * 512, (tb + 1) * 512)
                        if which == "q":
                            S.op("dve", lambda e, rot=rot, t1=t1, t2=t2: e.tensor_tensor(out=rot[:], in0=t1[:], in1=t2[:], op=ALU.add),
                                 reads=[t1.key, t2.key], writes=[rot.key])
                            S.op("act", lambda e, rot=rot, sl=sl: e.copy(out=q_sb[:, sl], in_=rot[:]), reads=[rot.key], writes=[q_sb.key])
                            S.op("dve", lambda e, rot=rot, sl=sl, h=h: e.tensor_tensor(
                                out=qd_sb[:, sl].rearrange("p (c q) -> p c q", q=64), in0=rot[:].rearrange("p (c q) -> p c q", q=64),
                                in1=cst_sb[:, C_QD + h * 64:C_QD + (h + 1) * 64].unsqueeze(1).broadcast_to([128, 8, 64]), op=ALU.mult),
                                reads=[rot.key] + CONST, writes=[qd_sb.key])
                        else:
                            S.op("dve", lambda e, t1=t1, t2=t2, sl=sl: e.tensor_tensor(out=k_sb[:, sl], in0=t1[:], in1=t2[:], op=ALU.add),
                                 reads=[t1.key, t2.key], writes=[k_sb.key])
                    bk = S.bank()
                    bv = bk.ap.bitcast(BF16)
                    S.group("pe", [lambda e, c=c, bv=bv, tb=tb: e.transpose(out=bv[0:64, c * 128:(c + 1) * 128], in_=k_sb[:, (tb * 8 + c) * 64:(tb * 8 + c + 1) * 64],
                                                                          identity=ident_b) for c in range(8)],
                            reads=[k_sb.key] + CONST, writes=[bk.key])
                    S.op("dve", lambda e, bv=bv, tb=tb, h=h: e.tensor_scalar(out=kd_c[0:64, tb * 8:(tb + 1) * 8, :], in0=bv[0:64, :].rearrange("p (c d) -> p c d", d=128),
                                                                            scalar1=cst_sb[0:64, C_KD + h:C_KD + h + 1], scalar2=None, op0=ALU.mult),
                         reads=[bk.key] + CONST, writes=[kd_c.key])
                    for half in range(2):
                        bk = S.bank()
                        for c in range(4):
                            ch = half * 4 + c
                            S.group("pe", [lambda e, kc=kc, c=c, ch=ch, bk=bk, hb=hb, wv_t=wv_t: e.matmul(bk.ap[0:64, c * 128:(c + 1) * 128], lhsT=hb[:, kc, ch * 64:(ch + 1) * 64],
                                                                                                        rhs=wv_t[:, kc, :], start=(kc == 0), stop=(kc == KC - 1)) for kc in range(KC)],
                                    reads=[hb.key, wv_t.key], writes=[bk.key])
                        S.op("act", lambda e, bk=bk, tb=tb, half=half: e.copy(out=vr_c[0:64, tb * 8 + half * 4:tb * 8 + half * 4 + 4, :],
                                                                             in_=bk.ap[0:64, :].rearrange("p (c d) -> p c d", d=128)),
                             reads=[bk.key], writes=[vr_c.key])
                    bk = S.bank()
                    proj_fm(bk, wg_t, hb)
                    S.op("act", lambda e, bk=bk, tb=tb: e.activation(out=gs_sb[:, tb * 512:(tb + 1) * 512], in_=bk.ap, func=AF.Silu), reads=[bk.key], writes=[gs_sb.key])
                for n in range(NCH):
                    csl = slice(n * 64, (n + 1) * 64)
                    sm = sm_t[cnt["sm"] % 2]
                    cnt["sm"] += 1
                    bks = S.bank()
                    S.op("pe", lambda e, bks=bks, csl=csl: e.matmul(bks.ap[0:64, 0:64], lhsT=k_sb[:, csl], rhs=q_sb[:, csl], start=True, stop=True),
                         reads=[k_sb.key, q_sb.key], writes=[bks.key])
                    S.op("dve", lambda e, bks=bks, sm=sm, h=h: e.tensor_tensor(out=sm[0:64, :], in0=bks.ap[0:64, 0:64], in1=cst_sb[0:64, C_DEC + h * 64:C_DEC + (h + 1) * 64], op=ALU.mult),
                         reads=[bks.key] + CONST, writes=[sm.key])
                    bko = S.bank()
                    fns = [lambda e, bko=bko, sm=sm, n=n: e.matmul(bko.ap[:, 0:64], lhsT=vr_c[0:64, n, :], rhs=sm[0:64, :], start=True, stop=(n == 0))]
                    rds = [vr_c.key, sm.key]
                    if n > 0:
                        sb = st_b[(cnt["sb"] - 1) % 2]
                        fns.append(lambda e, bko=bko, sb=sb, csl=csl: e.matmul(bko.ap[:, 0:64], lhsT=sb[:], rhs=qd_sb[:, csl], start=False, stop=True))
                        rds += [sb.key, qd_sb.key]
                    S.group("pe", fns, reads=rds, writes=[bko.key])
                    S.op("act", lambda e, bko=bko, csl=csl: e.copy(out=o_sb[:, csl], in_=bko.ap[:, 0:64]), reads=[bko.key], writes=[o_sb.key])
                    if n < NCH - 1:
                        bkv = S.bank()
                        S.op("pe", lambda e, bkv=bkv, n=n: e.matmul(bkv.ap[:, 0:128], lhsT=kd_c[0:64, n, :], rhs=vr_c[0:64, n, :], start=True, stop=True),
                             reads=[kd_c.key, vr_c.key], writes=[bkv.key])
                        if n == 0:
                            S.op("dve", lambda e, bkv=bkv: e.tensor_copy(out=st_f[:], in_=bkv.ap[:, 0:128]), reads=[bkv.key], writes=[st_f.key])
                        else:
                            S.op("dve", lambda e, bkv=bkv, h=h: e.scalar_tensor_tensor(out=st_f[:], in0=st_f[:], scalar=cst_sb[:, C_CD + h:C_CD + h + 1], in1=bkv.ap[:, 0:128],
                                                                                      op0=ALU.mult, op1=ALU.add),
                                 reads=[bkv.key, st_f.key] + CONST, writes=[st_f.key])
                        sb = st_b[cnt["sb"] % 2]
                        cnt["sb"] += 1
                        S.op("act", lambda e, sb=sb: e.copy(out=sb[:], in_=st_f[:]), reads=[st_f.key], writes=[sb.key])
                cat = cat_sb[cnt["cat"] % 2]
                cnt["cat"] += 1
                for tb in range(NB):
                    nf = nrm_f[tb % 2]
                    sl = slice(tb * 512, (tb + 1) * 512)
                    fm_rmsnorm(o_sb[:, sl], [o_sb.key], gv_sb[:, 10 + h:11 + h], ["gv"], nf[:], [nf.key], next_tmp())
                    S.op("dve", lambda e, nf=nf, sl=sl, cat=cat: e.tensor_tensor(out=cat[:, sl], in0=nf[:], in1=gs_sb[:, sl], op=ALU.mult),
                         reads=[nf.key, gs_sb.key], writes=[cat.key])
                S.dma("sp", catT_d[(NH + h) * 128:(NH + h + 1) * 128, :], cat[:], reads=[cat.key], writes=["catT"])
            S.barrier()

            AR.reset()
            wob = [AR.tile("wob", [KC, 512], BF16) for _ in range(2)]
            cblk = [AR.tile("cblk", [KC, 512], BF16) for _ in range(2)]
            xp = [AR.tile("xp", [512], F32) for _ in range(4)]
            w_out_v = w_out[l].rearrange("(kc p) c -> p kc c", p=128)
            k3 = 0
            for cb in range(4):
                wo = wob[cb % 2]
                S.dma("pool", wo[:], w_out_v[:, :, cb * 512:(cb + 1) * 512], writes=[wo.key])
                for tb in range(NB):
                    cbk = cblk[(cb * NB + tb) % 2]
                    S.dma("sp", cbk[:], rowsT(catT_d)[:, :, tb * 512:(tb + 1) * 512], reads=["catT"], writes=[cbk.key])
                    for ti in range(4):
                        i = tb * 4 + ti
                        xq = xp[k3 % 4]
                        k3 += 1
                        S.dma("sp", xq[:], xsrc[i * 128:(i + 1) * 128, cb * 512:(cb + 1) * 512], reads=["ydram"], writes=[xq.key])
                        bk = S.bank()
                        S.group("pe", [lambda e, kc=kc, bk=bk, cbk=cbk, ti=ti, wo=wo: e.matmul(bk.ap, lhsT=cbk[:, kc, ti * 128:(ti + 1) * 128], rhs=wo[:, kc, :],
                                                                                             start=(kc == 0), stop=(kc == KC - 1)) for kc in range(KC)],
                                reads=[cbk.key, wo.key], writes=[bk.key])
                        S.op("dve", lambda e, xq=xq, bk=bk: e.tensor_tensor(out=xq[:], in0=bk.ap, in1=xq[:], op=ALU.add), reads=[bk.key, xq.key], writes=[xq.key])
                        S.dma("sp", y[i * 128:(i + 1) * 128, cb * 512:(cb + 1) * 512], xq[:], reads=[xq.key], writes=["y1"])
            S.barrier()

            AR.reset()
            xts = [AR.tile("xt", [D], F32) for _ in range(2)]

            def src2(i):
                xt = xts[i % 2]
                S.dma("sp", xt[:], y[i * 128:(i + 1) * 128, :], writes=[xt.key])
                return xt
            norm_tiles(l, g2, src2, h2T_d, "h2T")
            S.barrier()

            AR.reset()
            hblk = [AR.tile("hblk", [KC, 512], BF16) for _ in range(2)]
            wqj = [AR.tile("wqj", [KC, 128], BF16) for _ in range(3)]
            qp_blk = AR.tile("qp_blk", [16, 512], F32)
            sk_sb = AR.tile("sk_sb", [16, 128], F32)
            Gblk = AR.tile("Gblk", [128, 128], BF16)
            s_sb = AR.tile("s_sb", [16, 128], F32)
            m1 = AR.tile("m1", [16, 16], F32)
            idx = AR.tile("idx", [16, 16], U32)
            idxf = AR.tile("idxf", [16, 16], F32)
            wk = [AR.tile("wk", [128], F32) for _ in range(2)]
            cand = AR.tile("cand", [8, 16, 16], F32)
            wk2 = [AR.tile("wk2", [256], F32) for _ in range(2)]
            g16 = AR.tile("g16", [8, 16], F32)
            cidx = AR.tile("cidx", [8, 16], U32)
            cf = AR.tile("cf", [128], F32)
            ci = AR.tile("ci", [128], I32)
            a0t = AR.tile("a0t", [128], F32)
            b0t = AR.tile("b0t", [128], F32)
            ngt = AR.tile("ngt", [128], F32)
            cabf = AR.tile("cabf", [2, 8, 16], F32)
            E = AR.tile("E", [8, 16, 16], F32)
            sel = AR.tile("sel", [3, 128], F32)
            gz = AR.tile("gz", [16], F32)
            tr_sb = AR.tile("tr_sb", [3, 128], F32)
            A4 = [AR.tile("A4", [4, 128], BF16) for _ in range(3)]
            B4 = [AR.tile("B4", [4, 128], BF16) for _ in range(3)]
            S.dma("sp", sk_sb[:], skT[l].rearrange("j p k -> p j k"), writes=[sk_sb.key])
            wq_v = wq[l].rearrange("(kc p) c -> p kc c", p=128)
            kw = 0
            k4 = 0
            for tb in range(NB):
                hb = hblk[tb % 2]
                S.dma("sp", hb[:], rowsT(h2T_d)[:, :, tb * 512:(tb + 1) * 512], reads=["h2T"], writes=[hb.key])
                for j in range(16):
                    wt = wqj[kw % 3]
                    kw += 1
                    S.dma("pool", wt[:], wq_v[:, :, j * 128:(j + 1) * 128], writes=[wt.key])
                    bk = S.bank()
                    proj_fm(bk, wt, hb)
                    S.op("act", lambda e, bk=bk, j=j: e.copy(out=qp_blk[:, j, :], in_=bk.ap), reads=[bk.key], writes=[qp_blk.key])
                for ti in range(4):
                    i = tb * 4 + ti
                    tsl = slice(ti * 128, (ti + 1) * 128)
                    for jb in range(4):
                        bk = S.bank()
                        S.group("pe", [lambda e, jj=jj, jb=jb, bk=bk, tsl=tsl: e.matmul(bk.ap[:, jj * 128:(jj + 1) * 128], lhsT=qp_blk[:, jb * 4 + jj, tsl], rhs=sk_sb[:, jb * 4 + jj, :],
                                                                                      start=True, stop=True) for jj in range(4)],
                                reads=[qp_blk.key, sk_sb.key], writes=[bk.key])
                        S.op("act", lambda e, bk=bk, jb=jb: e.copy(out=s_sb[:, jb * 4:(jb + 1) * 4, :], in_=bk.ap.rearrange("p (a k) -> p a k", k=128)),
                             reads=[bk.key], writes=[s_sb.key])
                    for j in range(16):
                        w_ = wk[j % 2]
                        S.op("dve", lambda e, j=j: e.max(out=m1[:, j, 0:8], in_=s_sb[:, j, :]), reads=[s_sb.key], writes=[m1.key])
                        S.op("dve", lambda e, j=j, w_=w_: e.match_replace(out=w_[:], in_to_replace=m1[:, j, 0:8], in_values=s_sb[:, j, :], imm_value=-1e30),
                             reads=[s_sb.key, m1.key], writes=[w_.key])
                        S.op("dve", lambda e, j=j, w_=w_: e.max(out=m1[:, j, 8:16], in_=w_[:]), reads=[w_.key], writes=[m1.key])
                        S.op("dve", lambda e, j=j: e.max_index(out=idx[:, j, 0:8], in_max=m1[:, j, 0:8], in_values=s_sb[:, j, :]), reads=[s_sb.key, m1.key], writes=[idx.key])
                        S.op("dve", lambda e, j=j, w_=w_: e.max_index(out=idx[:, j, 8:16], in_max=m1[:, j, 8:16], in_values=w_[:]), reads=[w_.key, m1.key], writes=[idx.key])
                    S.op("dve", lambda e: e.tensor_copy(out=idxf[:], in_=idx[:]), reads=[idx.key], writes=[idxf.key])
                    m1v = m1[:].rearrange("p (h two) k -> p h two k", two=2)
                    idv = idxf[:].rearrange("p (h two) k -> p h two k", two=2)
                    S.op("dve", lambda e, m1v=m1v: e.tensor_tensor(out=cand[:], in0=m1v[:, :, 0, :].unsqueeze(3).broadcast_to([128, 8, 16, 16]),
                                                                  in1=m1v[:, :, 1, :].unsqueeze(2).broadcast_to([128, 8, 16, 16]), op=ALU.add),
                         reads=[m1.key], writes=[cand.key])
                    for h in range(8):
                        w_ = wk2[h % 2]
                        cv = cand[:, h].rearrange("p a b -> p (a b)")
                        S.op("dve", lambda e, h=h, cv=cv: e.max(out=g16[:, h, 0:8], in_=cv), reads=[cand.key], writes=[g16.key])
                        S.op("dve", lambda e, h=h, cv=cv, w_=w_: e.match_replace(out=w_[:], in_to_replace=g16[:, h, 0:8], in_values=cv, imm_value=-1e30),
                             reads=[cand.key, g16.key], writes=[w_.key])
                        S.op("dve", lambda e, h=h, w_=w_: e.max(out=g16[:, h, 8:16], in_=w_[:]), reads=[w_.key], writes=[g16.key])
                        S.op("dve", lambda e, h=h, cv=cv: e.max_index(out=cidx[:, h, 0:8], in_max=g16[:, h, 0:8], in_values=cv), reads=[cand.key, g16.key], writes=[cidx.key])
                        S.op("dve", lambda e, h=h, w_=w_: e.max_index(out=cidx[:, h, 8:16], in_max=g16[:, h, 8:16], in_values=w_[:]), reads=[w_.key, g16.key], writes=[cidx.key])
                    cflat = cidx[:].rearrange("p h k -> p (h k)")
                    ca_v = cabf[:, 0].rearrange("p h k -> p (h k)")
                    cb_v = cabf[:, 1].rearrange("p h k -> p (h k)")
                    S.op("dve", lambda e, cflat=cflat: e.tensor_copy(out=cf[:], in_=cflat), reads=[cidx.key], writes=[cf.key])
                    S.op("dve", lambda e: e.tensor_scalar(out=ci[:], in0=cf[:], scalar1=1.0 / 16.0, scalar2=None, op0=ALU.mult), reads=[cf.key], writes=[ci.key])
                    S.op("dve", lambda e: e.tensor_copy(out=a0t[:], in_=ci[:]), reads=[ci.key], writes=[a0t.key])
                    S.op("dve", lambda e: e.scalar_tensor_tensor(out=b0t[:], in0=a0t[:], scalar=-16.0, in1=cf[:], op0=ALU.mult, op1=ALU.add),
                         reads=[a0t.key, cf.key], writes=[b0t.key])
                    S.op("dve", lambda e: e.tensor_single_scalar(out=ngt[:], in_=b0t[:], scalar=0.0, op=ALU.is_lt), reads=[b0t.key], writes=[ngt.key])
                    S.op("dve", lambda e, ca_v=ca_v: e.tensor_tensor(out=ca_v, in0=a0t[:], in1=ngt[:], op=ALU.subtract), reads=[a0t.key, ngt.key], writes=[cabf.key])
                    S.op("dve", lambda e, cb_v=cb_v: e.scalar_tensor_tensor(out=cb_v, in0=ngt[:], scalar=16.0, in1=b0t[:], op0=ALU.mult, op1=ALU.add),
                         reads=[ngt.key, b0t.key, cabf.key], writes=[cabf.key])
                    io4 = iota16.rearrange("p (h k a) -> p h k a", k=16, a=16)
                    for p_ in range(2):
                        S.op("dve", lambda e, p_=p_: e.tensor_tensor(out=E[:], in0=io4, in1=cabf[:, p_].unsqueeze(3).broadcast_to([128, 8, 16, 16]), op=ALU.is_equal),
                             reads=[cabf.key] + CONST, writes=[E.key])
                        S.op("dve", lambda e, p_=p_, idv=idv: e.tensor_tensor(out=E[:], in0=E[:], in1=idv[:, :, p_, :].unsqueeze(2).broadcast_to([128, 8, 16, 16]), op=ALU.mult),
                             reads=[idxf.key, E.key], writes=[E.key])
                        S.op("dve", lambda e, p_=p_: e.tensor_reduce(out=sel[:, p_, :], in_=E[:].rearrange("p h k a -> p (h k) a"), axis=AX.X, op=ALU.add),
                             reads=[E.key], writes=[sel.key])
                    gsel = sel[:, 2, :].rearrange("p (h k) -> p h k", k=16)
                    S.op("dve", lambda e, gsel=gsel: e.tensor_tensor(out=gsel, in0=g16[:], in1=g16[:, :, 0:1].broadcast_to([128, 8, 16]), op=ALU.subtract),
                         reads=[g16.key], writes=[sel.key])
                    S.op("act", lambda e, gsel=gsel: e.activation(out=gsel, in_=gsel, func=AF.Exp), reads=[sel.key], writes=[sel.key])
                    S.op("dve", lambda e, gsel=gsel: e.tensor_reduce(out=gz[:, 0:8], in_=gsel, axis=AX.X, op=ALU.add), reads=[sel.key], writes=[gz.key])
                    S.op("dve", lambda e: e.reciprocal(out=gz[:, 8:16], in_=gz[:, 0:8]), reads=[gz.key], writes=[gz.key])
                    S.op("dve", lambda e, gsel=gsel: e.tensor_tensor(out=gsel, in0=gsel, in1=gz[:, 8:16].unsqueeze(2).broadcast_to([128, 8, 16]), op=ALU.mult),
                         reads=[sel.key, gz.key], writes=[sel.key])
                    bk = S.bank()
                    S.group("pe", [lambda e, c=c, bk=bk: e.transpose(out=bk.ap[:, c * 128:(c + 1) * 128], in_=sel[:, c, :], identity=ident_f) for c in range(3)],
                            reads=[sel.key] + CONST, writes=[bk.key])
                    S.op("act", lambda e, bk=bk: e.copy(out=tr_sb[:], in_=bk.ap[:, 0:384].rearrange("p (c t) -> p c t", t=128)), reads=[bk.key], writes=[tr_sb.key])
                    for t4 in range(32):
                        a4 = A4[k4 % 3]
                        b4 = B4[k4 % 3]
                        k4 += 1
                        fns = []
                        for tt in range(4):
                            t = t4 * 4 + tt
                            fns.append(lambda e, a4=a4, tt=tt, t=t: e.tensor_scalar(out=a4[:, tt, :], in0=iota_f, scalar1=tr_sb[:, 0, t:t + 1], scalar2=tr_sb[:, 2, t:t + 1],
                                                                                 op0=ALU.is_equal, op1=ALU.mult))
                            fns.append(lambda e, b4=b4, tt=tt, t=t: e.tensor_scalar(out=b4[:, tt, :], in0=iota_f, scalar1=tr_sb[:, 1, t:t + 1], scalar2=None,
                                                                                 op0=ALU.is_equal))
                        S.group("dve", fns, reads=[tr_sb.key] + CONST, writes=[a4.key, b4.key])
                        bk = S.bank()
                        S.group("pe", [lambda e, tt=tt, bk=bk, a4=a4, b4=b4: e.matmul(bk.ap[:, tt * 128:(tt + 1) * 128], lhsT=b4[:, tt, :], rhs=a4[:, tt, :], start=True, stop=True)
                                       for tt in range(4)], reads=[a4.key, b4.key], writes=[bk.key])
                        S.op("act", lambda e, bk=bk, t4=t4: e.copy(out=Gblk[:, :, t4 * 4:(t4 + 1) * 4].rearrange("p i t -> p t i"),
                                                                 in_=bk.ap.rearrange("p (t i) -> p t i", i=128)),
                             reads=[bk.key], writes=[Gblk.key])
                    for qd4 in range(4):
                        S.dma("sp", G_d.rearrange("a b t -> b a t")[:, qd4 * 32:(qd4 + 1) * 32, i * 128:(i + 1) * 128], Gblk[:, qd4 * 32:(qd4 + 1) * 32, :],
                              reads=[Gblk.key], writes=["Gd"])
            S.barrier()

            AR.reset()
            h2_sb = AR.tile("h2_sb", [KC, T], BF16)
            ublk = [AR.tile("ublk", [KC, 512], BF16) for _ in range(2)]
            gt = [AR.tile("gt", [T], BF16) for _ in range(2)]
            ga = [AR.tile("ga", [T], BF16) for _ in range(2)]
            ge = [AR.tile("ge", [512], F32) for _ in range(3)]
            for tb in range(NB):
                S.dma("sp", h2_sb[:, :, tb * 512:(tb + 1) * 512], rowsT(h2T_d)[:, :, tb * 512:(tb + 1) * 512], reads=["h2T"], writes=[h2_sb.key])
            uT_v = uT[l].rearrange("(kc p) e -> p kc e", p=128)
            k6 = 0
            for eg in range(NEXP // 512):
                ub = ublk[eg % 2]
                S.dma("pool", ub[:], uT_v[:, :, eg * 512:(eg + 1) * 512], writes=[ub.key])
                for ei in range(4):
                    g = eg * 4 + ei
                    gt_ = gt[g % 2]
                    ga_ = ga[g % 2]
                    S.dma("sp", gt_[:], G_d[g], reads=["Gd"], writes=[gt_.key])
                    for tb in range(NB):
                        sl = slice(tb * 512, (tb + 1) * 512)
                        bk = S.bank()
                        S.group("pe", [lambda e, kc=kc, bk=bk, ub=ub, ei=ei, sl=sl: e.matmul(bk.ap, lhsT=ub[:, kc, ei * 128:(ei + 1) * 128], rhs=h2_sb[:, kc, sl],
                                                                                           start=(kc == 0), stop=(kc == KC - 1)) for kc in range(KC)],
                                reads=[ub.key, h2_sb.key], writes=[bk.key])
                        ge_ = ge[k6 % 3]
                        k6 += 1
                        S.op("act", lambda e, bk=bk, ge_=ge_: e.activation(out=ge_[:], in_=bk.ap, func=AF.Gelu), reads=[bk.key], writes=[ge_.key])
                        S.op("dve", lambda e, ge_=ge_, gt_=gt_, ga_=ga_, sl=sl: e.tensor_tensor(out=ga_[:, sl], in0=ge_[:], in1=gt_[:, sl], op=ALU.mult),
                             reads=[ge_.key, gt_.key], writes=[ga_.key])
                    S.dma("sp", gaT_d[g * 128:(g + 1) * 128, :], ga_[:], reads=[ga_.key], writes=["gaT"])
            S.barrier()

            AR.reset()
            ga4 = [AR.tile("ga4", [4, TG * 128], BF16) for _ in range(3)]
            v4 = [AR.tile("v4", [4, 512], BF16) for _ in range(3)]
            xo = [AR.tile("xo", [512], F32) for _ in range(4)]
            gaT_v = gaT_d.rearrange("(g p) t -> p g t", p=128)
            pv_v = pv[l].rearrange("(g p) d -> p g d", p=128)
            k7 = 0
            k8 = 0
            for hf in range(NT // TG):
                tsl = slice(hf * TG * 128, (hf + 1) * TG * 128)
                for cb in range(4):
                    for g4 in range(32):
                        ga_ = ga4[k7 % 3]
                        v_ = v4[k7 % 3]
                        k7 += 1
                        S.dma("sp", ga_[:], gaT_v[:, g4 * 4:(g4 + 1) * 4, tsl], reads=["gaT"], writes=[ga_.key])
                        S.dma("pool", v_[:], pv_v[:, g4 * 4:(g4 + 1) * 4, cb * 512:(cb + 1) * 512], writes=[v_.key])
                        fns = []
                        for gi in range(4):
                            g = g4 * 4 + gi
                            for tt in range(TG):
                                fns.append(lambda e, gi=gi, tt=tt, g=g, ga_=ga_, v_=v_: e.matmul(S.banks[tt].ap, lhsT=ga_[:, gi, tt * 128:(tt + 1) * 128], rhs=v_[:, gi, :],
                                                                                           start=(g == 0), stop=(g == 127)))
                        S.group("pe", fns, reads=[ga_.key, v_.key], writes=[S.banks[tt].key for tt in range(TG)])
                    for tt in range(TG):
                        i = hf * TG + tt
                        xq = xo[k8 % 4]
                        k8 += 1
                        S.dma("sp", xq[:], y[i * 128:(i + 1) * 128, cb * 512:(cb + 1) * 512], reads=["y1"], writes=[xq.key])
                        S.op("dve", lambda e, xq=xq, tt=tt: e.tensor_tensor(out=xq[:], in0=S.banks[tt].ap, in1=xq[:], op=ALU.add),
                             reads=[S.banks[tt].key, xq.key], writes=[xq.key])
                        S.dma("sp", y[i * 128:(i + 1) * 128, cb * 512:(cb + 1) * 512], xq[:], reads=[xq.key], writes=["ydram"])
            S.barrier()

        S.finish()
        with nc.Block() as block:
            @block.tensor
            def _(e):
                for f in S.prog["pe"]:
                    f(e)

            @block.scalar
            def _(e):
                for f in S.prog["act"]:
                    f(e)

            @block.vector
            def _(e):
                for f in S.prog["dve"]:
                    f(e)

            @block.gpsimd
            def _(e):
                for f in S.prog["pool"]:
                    f(e)

            @block.sync
            def _(e):
                for f in S.prog["sp"]:
                    f(e)
    return nc


def make_consts(T):
    cst = np.zeros((128, CW), np.float32)
    cst[:, C_ID:C_ID + 128] = np.eye(128, dtype=np.float32)
    k = np.arange(128)
    sw = np.zeros((128, 128), np.float32)
    sw[(k + 64) % 128, k] = 1.0
    cst[:, C_SW:C_SW + 128] = sw
    cst[:, C_ON:C_ON + 128] = 1.0
    cst[:, C_IO:C_IO + 128] = np.arange(128, dtype=np.float32)[None, :]
    cst[:, C_I16:C_I16 + 2048] = (np.arange(2048) % 16).astype(np.float32)[None, :]
    hh = np.arange(8, dtype=np.float32)
    lg = np.log1p(-(np.float32(2.0) ** (-5.0 - hh))).astype(np.float32)
    pos = np.arange(64, dtype=np.float32)
    diff = pos[:, None] - pos[None, :]
    dec = np.where(diff >= 0, np.exp(np.maximum(diff, 0.0)[None] * lg[:, None, None]), 0.0).astype(np.float32)
    decT = np.transpose(dec, (2, 0, 1)) * np.float32(SCALE)
    cst[0:64, C_DEC:C_DEC + 512] = decT.reshape(64, 512)
    qd = np.exp((pos + 1.0)[None, :] * lg[:, None]).astype(np.float32)
    cst[:, C_QD:C_QD + 512] = qd.reshape(1, 512)
    kd = (np.exp((63.0 - pos)[None, :] * lg[:, None]) * SCALE).astype(np.float32)
    cst[0:64, C_KD:C_KD + 8] = kd.T
    cst[64:128, C_KD:C_KD + 8] = kd.T
    cst[:, C_CD:C_CD + 8] = np.exp(64.0 * lg)[None, :]
    half = 64
    inv_freq = (np.float32(10000.0) ** (-np.arange(half, dtype=np.float32) / half)).astype(np.float32)
    ang = np.arange(T, dtype=np.float32)[:, None] * inv_freq[None, :]
    cos = np.cos(ang).astype(np.float32).T
    sin = np.sin(ang).astype(np.float32).T
    cosT = np.ascontiguousarray(np.concatenate([cos, cos], 0))
    sinT = np.ascontiguousarray(np.concatenate([-sin, sin], 0))
    return cst, cosT, sinT


def bias_layout(rel_bias):
    kk = np.arange(640)[:, None]
    qq = np.arange(128)[None, :]
    rel = np.clip(qq + 512 - kk, -128, 128) + 128
    ck = kk // 64 - 8
    cq = qq // 64
    valid = (ck >= cq - 8) & (ck <= cq)
    b = rel_bias[:, :, rel]
    return np.ascontiguousarray(np.where(valid[None, None], b, np.float32(-30000.0)).astype(np.float32))


def layout_weights(inp, layers):
    L = list(layers)
    d = {}
    d["w_in"] = np.ascontiguousarray(inp["w_in"][L])
    d["w_out"] = np.ascontiguousarray(inp["w_out"][L])
    d["wq"] = np.ascontiguousarray(inp["peer_wq"][L])
    sk = np.asarray(inp["peer_subkeys"])[L]
    d["skT"] = np.ascontiguousarray(np.transpose(sk, (0, 1, 2, 4, 3)).reshape(len(L), 16, 128, 128))
    d["uT"] = np.ascontiguousarray(np.transpose(np.asarray(inp["peer_u"])[L], (0, 2, 1)))
    d["pv"] = np.ascontiguousarray(inp["peer_v"][L])
    d["g1"] = np.ascontiguousarray(inp["norm1_g"][L])
    d["g2"] = np.ascontiguousarray(inp["norm2_g"][L])
    gv = np.zeros((len(L), 128, 18), np.float32)
    gv[:, :, 0] = np.asarray(inp["qa_norm_g"])[L]
    gv[:, :, 1] = np.asarray(inp["ka_norm_g"])[L]
    gv[:, :, 2:10] = np.transpose(np.asarray(inp["attn_out_g"])[L].reshape(len(L), 8, 128), (0, 2, 1))
    gv[:, :, 10:18] = np.transpose(np.asarray(inp["ret_out_g"])[L].reshape(len(L), 8, 128), (0, 2, 1))
    d["gvec"] = gv
    d["biasT"] = bias_layout(np.asarray(inp["rel_bias"])[L])
    return d


_CACHE = {}


def kernel(**inputs):
    inp = {k: np.asarray(v) for k, v in inputs.items()}
    x = inp["x"]
    B, T, _ = x.shape
    DEPTH = inp["w_in"].shape[0]
    key = (T, DEPTH)
    if key not in _CACHE:
        _CACHE[key] = build(T, DEPTH)
    nc = _CACHE[key]
    cst, cosT, sinT = make_consts(T)
    wd = layout_weights(inp, range(DEPTH))
    wd.update(cst=cst, cosT=cosT, sinT=sinT)
    in_maps = []
    for b in range(B):
        m = dict(wd)
        m["x"] = np.ascontiguousarray(x[b])
        in_maps.append(m)
    res = run_bass_kernel_spmd(nc, in_maps, core_ids=list(range(B)))
    return np.stack([res.results[b]["y"] for b in range(B)], 0).astype(np.float32)
```

```python
import numpy as np
import concourse.bass as bass
import concourse.mybir as mybir
from concourse.bass_utils import run_bass_kernel_spmd
from contextlib import ExitStack

F32 = mybir.dt.float32
BF16 = mybir.dt.bfloat16
U32 = mybir.dt.uint32
I32 = mybir.dt.int32
AF = mybir.ActivationFunctionType
ALU = mybir.AluOpType
AX = mybir.AxisListType

D = 2048
KC = 16
HD = 128
NH = 8
EPS = 1e-6
NEXP = 16384
IN_COLS = 7168
SCALE = HD ** -0.5
EPOCH = 20000

C_ID, C_SW, C_ON, C_IO, C_I16, C_DEC, C_QD, C_KD, C_CD = 0, 128, 256, 384, 512, 2560, 3072, 3584, 3592
CW = 3600


class Sched:
    ENGS = ("pe", "act", "dve", "pool", "sp")

    def __init__(self, nc, stack, n_dma=32, n_ep=8):
        self.nc = nc
        self.prog = {e: [] for e in self.ENGS}
        self.esems = {e: [stack.enter_context(nc.semaphore(f"s_{e}_{i}")) for i in range(n_ep)] for e in self.ENGS}
        self.eidx = {e: 0 for e in self.ENGS}
        self.ecnt = {e: 0 for e in self.ENGS}
        self.dsems = [stack.enter_context(nc.semaphore(f"s_dma_{i}")) for i in range(n_dma)]
        self.dval = {s: 0 for s in self.dsems}
        self.di = 0
        self.res = {}
        self.waited = {e: {} for e in self.ENGS}
        self.pe_sems = set(self.esems["pe"])
        self.banks = None
        self.bi = 0
        self.ninstr = 0

    def _deps(self, reads, writes):
        deps = {}

        def add(s, v):
            if deps.get(s, 0) < v:
                deps[s] = v
        for r in reads:
            st = self.res.get(r)
            if st and st[0]:
                add(*st[0])
        for w in writes:
            st = self.res.get(w)
            if st:
                if st[0]:
                    add(*st[0])
                for s, v in st[1].items():
                    add(s, v)
        return deps

    def _waits(self, eng, deps):
        wd = self.waited[eng]
        for s, v in deps.items():
            if eng == "pe" and s in self.pe_sems:
                continue
            if wd.get(s, 0) >= v:
                continue
            wd[s] = v
            self.prog[eng].append(lambda e, s=s, v=v: e.wait_ge(s, v))
            self.ninstr += 1

    def _mark(self, reads, writes, s, v):
        for r in reads:
            st = self.res.setdefault(r, [None, {}])
            if st[1].get(s, 0) < v:
                st[1][s] = v
        for w in writes:
            self.res[w] = [(s, v), {}]

    def _tick(self, eng):
        if self.ecnt[eng] >= EPOCH:
            self.eidx[eng] += 1
            self.ecnt[eng] = 0
        s = self.esems[eng][self.eidx[eng]]
        self.ecnt[eng] += 1
        return s, self.ecnt[eng]

    def group(self, eng, fns, reads=(), writes=()):
        self._waits(eng, self._deps(reads, writes))
        s, v = self._tick(eng)
        n = len(fns)
        for i, fn in enumerate(fns):
            if i == n - 1:
                self.prog[eng].append(lambda e, fn=fn, s=s: fn(e).then_inc(s, 1))
            else:
                self.prog[eng].append(fn)
        self.ninstr += n
        self._mark(reads, writes, s, v)

    def op(self, eng, fn, reads=(), writes=()):
        self.group(eng, [fn], reads, writes)

    def dma(self, q, out, in_, reads=(), writes=()):
        s = self.dsems[self.di % len(self.dsems)]
        self.di += 1
        deps = self._deps(reads, writes)
        prev = self.dval[s]
        if prev > 0 and deps.get(s, 0) < prev:
            deps[s] = prev
        self._waits(q, deps)
        self.dval[s] = prev + 16
        self.prog[q].append(lambda e, s=s: e.dma_start(out=out, in_=in_).then_inc(s, 16))
        self.ninstr += 1
        self._mark(reads, writes, s, prev + 16)

    def barrier(self):
        cur = {}
        for e in self.ENGS:
            if self.ecnt[e] > 0:
                cur[self.esems[e][self.eidx[e]]] = self.ecnt[e]
        for s, v in self.dval.items():
            if v > 0:
                cur[s] = v
        for e in self.ENGS:
            wd = self.waited[e]
            for s, v in cur.items():
                if wd.get(s, 0) >= v:
                    continue
                wd[s] = v
                self.prog[e].append(lambda en, s=s, v=v: en.wait_ge(s, v))
        self.res = {}

    def finish(self):
        for s, v in self.dval.items():
            if v > 0 and self.waited["sp"].get(s, 0) < v:
                self.prog["sp"].append(lambda e, s=s, v=v: e.wait_ge(s, v))

    def bank(self):
        b = self.banks[self.bi % 8]
        self.bi += 1
        return b


class Tile:
    def __init__(self, ap, key):
        self.ap = ap
        self.key = key

    def __getitem__(self, k):
        return self.ap[k]


class Arena:
    def __init__(self, big, nbytes):
        self.big = big
        self.nbytes = nbytes
        self.off = 0
        self.n = 0

    def reset(self):
        self.off = 0

    def tile(self, name, free, dtype, parts=128):
        esz = 2 if dtype == BF16 else 4
        n = int(np.prod(free))
        nb = (n * esz + 31) // 32 * 32
        assert self.off + nb <= self.nbytes, f"arena overflow at {name}: {self.off}+{nb}>{self.nbytes}"
        a = self.big[:, self.off // 2:(self.off + n * esz) // 2]
        self.off += nb
        if dtype != BF16:
            a = a.bitcast(dtype)
        if len(free) == 2:
            a = a.rearrange("p (a b) -> p a b", b=free[1])
        elif len(free) == 3:
            a = a.rearrange("p (a b c) -> p a b c", b=free[1], c=free[2])
        if parts != 128:
            a = a[0:parts]
        self.n += 1
        return Tile(a, f"{name}#{self.n}")


def build(T, DEPTH, debug=False):
    NT = T // 128
    NB = T // 512
    NCH = T // 64
    assert T % 512 == 0
    TG = min(8, NT)
    nc = bass.Bass("TRN2", target_bir_lowering=False)

    def din(name, shape, dt=F32):
        return nc.dram_tensor(name, list(shape), dt, kind="ExternalInput").ap()

    x_in = din("x", [T, D])
    w_in = din("w_in", [DEPTH, D, IN_COLS])
    w_out = din("w_out", [DEPTH, D, D])
    wq = din("wq", [DEPTH, D, D])
    skT = din("skT", [DEPTH, 16, 128, 128])
    uT = din("uT", [DEPTH, D, NEXP])
    pv = din("pv", [DEPTH, NEXP, D])
    g1 = din("g1", [DEPTH, D])
    g2 = din("g2", [DEPTH, D])
    gvec = din("gvec", [DEPTH, 128, 18])
    biasT = din("biasT", [DEPTH, NH, 640, 128])
    cst = din("cst", [128, CW])
    cosT = din("cosT", [128, T])
    sinT = din("sinT", [128, T])
    y = nc.dram_tensor("y", [T, D], F32, kind="ExternalOutput").ap()
    skind = "ExternalOutput" if debug else "Internal"
    hnT_d = nc.dram_tensor("hnT_d", [D, T], BF16, kind=skind).ap()
    catT_d = nc.dram_tensor("catT_d", [D, T], BF16, kind=skind).ap()
    h2T_d = nc.dram_tensor("h2T_d", [D, T], BF16, kind=skind).ap()
    G_d = nc.dram_tensor("G_d", [128, 128, T], BF16, kind=skind).ap()
    gaT_d = nc.dram_tensor("gaT_d", [NEXP, T], BF16, kind="Internal").ap()
    geT_d = nc.dram_tensor("geT_d", [NEXP, T], BF16, kind="Internal").ap()

    ARENA_BYTES = 190 * 1024
    with ExitStack() as stack:
        ec = stack.enter_context
        S = Sched(nc, stack)
        cst_sb = ec(nc.sbuf_tensor("cst_sb", [128, CW], F32))
        cbf = ec(nc.sbuf_tensor("cbf", [128, 384], BF16))
        gv_sb = ec(nc.sbuf_tensor("gv_sb", [128, 20], F32))
        big = ec(nc.sbuf_tensor("arena", [128, ARENA_BYTES // 2], BF16))
        S.banks = [Tile(ec(nc.psum_tensor(f"bank{i}", [128, 512], F32))[:], f"bank{i}") for i in range(8)]
        AR = Arena(big, ARENA_BYTES)

        ident_f = cst_sb[:, C_ID:C_ID + 128]
        swap_f = cst_sb[:, C_SW:C_SW + 128]
        ones_f = cst_sb[:, C_ON:C_ON + 128]
        iota_f = cst_sb[:, C_IO:C_IO + 128]
        iota16 = cst_sb[:, C_I16:C_I16 + 2048]
        ident_b = cbf[:, 0:128]
        ones_b = cbf[:, 128:256]
        iota_b = cbf[:, 256:384]
        CST = "cst"

        S.dma("sp", cst_sb[:], cst, writes=[CST])
        S.op("dve", lambda e: e.tensor_copy(out=cbf[:, 0:128], in_=cst_sb[:, C_ID:C_ID + 128]), reads=[CST], writes=["cbf"])
        S.op("dve", lambda e: e.tensor_copy(out=cbf[:, 128:256], in_=cst_sb[:, C_ON:C_ON + 128]), reads=[CST], writes=["cbf"])
        S.op("dve", lambda e: e.tensor_copy(out=cbf[:, 256:384], in_=cst_sb[:, C_IO:C_IO + 128]), reads=[CST], writes=["cbf"])
        CONST = [CST, "cbf"]

        def rowsT(d_ap):
            return d_ap.rearrange("(kc p) t -> p kc t", p=128)

        def norm_tiles(l, gsrc, src_fn, dstT_d, phase_tag, post_fn=None):
            gbc = AR.tile("gbc", [D], F32)
            S.dma("sp", gbc[:], gsrc[l:l + 1, :].broadcast_to([128, D]), writes=[gbc.key])
            junk = AR.tile("junk", [D], BF16)
            hn = [AR.tile("hn", [D], BF16) for _ in range(2)]
            hT = [AR.tile("hT", [KC, 512], BF16) for _ in range(2)]
            ss = [AR.tile("ss", [4], F32) for _ in range(2)]
            for i in range(NT):
                xt = src_fn(i)
                s_ = ss[i % 2]
                h_ = hn[i % 2]
                ht = hT[(i // 4) % 2]
                S.op("dve", lambda e, s_=s_: e.memset(s_[:, 0:1], 0.0), writes=[s_.key])
                S.op("act", lambda e, s_=s_, xt=xt: e.activation(out=junk[:], in_=xt[:], func=AF.Square, accum_out=s_[:, 0:1]),
                     reads=[xt.key], writes=[s_.key, junk.key])
                S.op("act", lambda e, s_=s_: e.activation(out=s_[:, 1:2], in_=s_[:, 0:1], func=AF.Sqrt, scale=1.0 / D, bias=EPS),
                     reads=[s_.key], writes=[s_.key])
                S.op("dve", lambda e, s_=s_: e.reciprocal(out=s_[:, 2:3], in_=s_[:, 1:2]), reads=[s_.key], writes=[s_.key])
                S.op("dve", lambda e, s_=s_, xt=xt, h_=h_: e.scalar_tensor_tensor(out=h_[:], in0=xt[:], scalar=s_[:, 2:3], in1=gbc[:],
                                                                                 op0=ALU.mult, op1=ALU.mult),
                     reads=[xt.key, s_.key, gbc.key], writes=[h_.key])
                for half in range(2):
                    bk = S.bank()
                    bv = bk.ap.bitcast(BF16)
                    S.group("pe", [lambda e, c=c, half=half, bv=bv, h_=h_: e.transpose(out=bv[:, c * 128:(c + 1) * 128],
                                                                                       in_=h_[:, (half * 8 + c) * 128:(half * 8 + c + 1) * 128],
                                                                                       identity=ident_b) for c in range(8)],
                            reads=[h_.key] + CONST, writes=[bk.key])
                    S.op("act", lambda e, half=half, bv=bv, ht=ht, i=i: e.copy(out=ht[:, half * 8:(half + 1) * 8, (i % 4) * 128:(i % 4 + 1) * 128],
                                                                             in_=bv.rearrange("p (c t) -> p c t", t=128)),
                         reads=[bk.key], writes=[ht.key])
                if post_fn:
                    post_fn(i, xt)
                if i % 4 == 3:
                    tb = i // 4
                    S.dma("sp", rowsT(dstT_d)[:, :, tb * 512:(tb + 1) * 512], ht[:], reads=[ht.key], writes=[phase_tag])

        def fm_rmsnorm(src_ap, src_keys, gcol_ap, gkeys, out_ap, out_keys, tmp):
            sq, rt, ri = tmp
            bk = S.bank()
            S.op("act", lambda e: e.activation(out=sq[:], in_=src_ap, func=AF.Square), reads=src_keys, writes=[sq.key])
            S.op("pe", lambda e: e.matmul(bk.ap, lhsT=ones_f, rhs=sq[:], start=True, stop=True), reads=[sq.key] + CONST, writes=[bk.key])
            S.op("act", lambda e: e.activation(out=rt[:], in_=bk.ap, func=AF.Sqrt, scale=1.0 / HD, bias=EPS), reads=[bk.key], writes=[rt.key])
            S.op("dve", lambda e: e.reciprocal(out=ri[:], in_=rt[:]), reads=[rt.key], writes=[ri.key])
            S.op("dve", lambda e: e.scalar_tensor_tensor(out=out_ap, in0=src_ap, scalar=gcol_ap, in1=ri[:], op0=ALU.mult, op1=ALU.mult),
                 reads=src_keys + [ri.key] + gkeys, writes=out_keys)

        def load_w(dst, src_ap, l):
            S.dma("pool", dst[:], src_ap, writes=[dst.key])

        def proj_fm(bk, wt, hb, n=512, c0=0):
            S.group("pe", [lambda e, kc=kc: e.matmul(bk.ap[:, 0:n], lhsT=wt[:, kc, :], rhs=hb[:, kc, c0:c0 + n], start=(kc == 0), stop=(kc == KC - 1))
                           for kc in range(KC)], reads=[wt.key, hb.key], writes=[bk.key])

        w_in_v = [w_in[l].rearrange("(kc p) c -> p kc c", p=128) for l in range(DEPTH)]

        for l in range(DEPTH):
            xsrc = x_in if l == 0 else y
            AR.reset()
            xts = [AR.tile("xt", [D], F32) for _ in range(2)]

            def src1(i):
                xt = xts[i % 2]
                S.dma("sp", xt[:], xsrc[i * 128:(i + 1) * 128, :], reads=["ydram"], writes=[xt.key])
                return xt
            norm_tiles(l, g1, src1, hnT_d, "hnT")
            S.dma("sp", gv_sb[:, 0:18], gvec[l], writes=["gv"])
            S.op("dve", lambda e: e.tensor_scalar(out=gv_sb[:, 18:19], in0=gv_sb[:, 0:1], scalar1=SCALE, scalar2=None, op0=ALU.mult),
                 reads=["gv"], writes=["gv"])
            S.barrier()

            AR.reset()
            hblk = [AR.tile("hblk", [KC, 512], BF16) for _ in range(2)]
            wts = [AR.tile("wt", [KC, 128], BF16) for _ in range(6)]
            q_sb = AR.tile("q_sb", [T], BF16)
            k_sb = AR.tile("k_sb", [T], BF16)
            qd_sb = AR.tile("qd_sb", [T], BF16)
            v_sb = AR.tile("v_sb", [NT, 128], BF16)
            kd_c = AR.tile("kd_c", [NCH, 128], BF16)
            vr_c = AR.tile("vr_c", [NCH, 128], BF16)
            gs_sb = AR.tile("gs_sb", [T], F32)
            o_sb = AR.tile("o_sb", [T], F32)
            cat_sb = [AR.tile("cat_sb", [T], BF16) for _ in range(2)]
            bias_sb = AR.tile("bias_sb", [5, 128], F32)
            pTs = [AR.tile("pT", [5, 128], BF16) for _ in range(3)]
            e1s = [AR.tile("e1", [5, 128], F32) for _ in range(3)]
            tmpn = [[AR.tile("sq", [512], F32), AR.tile("rt", [512], F32), AR.tile("ri", [512], F32)] for _ in range(2)]
            xs_t = [AR.tile("xs", [512], F32) for _ in range(2)]
            t1_t = [AR.tile("t1", [512], F32) for _ in range(2)]
            t2_t = [AR.tile("t2", [512], F32) for _ in range(2)]
            rot_t = [AR.tile("rot", [512], F32) for _ in range(2)]
            cs_t = [AR.tile("cs", [2, 512], F32) for _ in range(2)]
            rz_t = [AR.tile("rz", [128], F32) for _ in range(2)]
            sm_t = [AR.tile("sm", [64], BF16) for _ in range(3)]
            st_f = AR.tile("st_f", [128], F32)
            st_b = [AR.tile("st_b", [128], BF16) for _ in range(2)]
            nrm_f = [AR.tile("nrm_f", [512], F32) for _ in range(2)]
            cnt = {"hb": 0, "tn": 0, "w": 0, "cat": 0, "pt": 0, "sm": 0, "sb": 0, "x": 0}

            def next_hblk(tb):
                hb = hblk[cnt["hb"] % 2]
                cnt["hb"] += 1
                S.dma("sp", hb[:], rowsT(hnT_d)[:, :, tb * 512:(tb + 1) * 512], reads=["hnT"], writes=[hb.key])
                return hb

            def next_tmp():
                cnt["tn"] += 1
                return tmpn[cnt["tn"] % 2]

            def get_w(col0):
                wt = wts[cnt["w"] % 6]
                cnt["w"] += 1
                load_w(wt, w_in_v[l][:, :, col0:col0 + 128], l)
                return wt

            for h in range(NH):
                wq_t = get_w(h * 128)
                wk_t = get_w(1024 + h * 128)
                wv_t = get_w(2048 + h * 128)
                S.dma("sp", bias_sb[:], biasT[l, h].rearrange("(a p) q -> p a q", p=128), writes=[bias_sb.key])
                for tb in range(NB):
                    hb = next_hblk(tb)
                    for wt, dst, gc in ((wq_t, q_sb, 18), (wk_t, k_sb, 1)):
                        bk = S.bank()
                        proj_fm(bk, wt, hb)
                        fm_rmsnorm(bk.ap, [bk.key], gv_sb[:, gc:gc + 1], ["gv"], dst[:, tb * 512:(tb + 1) * 512], [dst.key], next_tmp())
                    bk = S.bank()
                    for ti in range(4):
                        S.group("pe", [lambda e, kc=kc, ti=ti, bk=bk, hb=hb, wv_t=wv_t: e.matmul(bk.ap[:, ti * 128:(ti + 1) * 128], lhsT=hb[:, kc, ti * 128:(ti + 1) * 128],
                                                                                               rhs=wv_t[:, kc, :], start=(kc == 0), stop=(kc == KC - 1)) for kc in range(KC)],
                                reads=[hb.key, wv_t.key], writes=[bk.key])
                    S.op("act", lambda e, bk=bk, tb=tb: e.copy(out=v_sb[:, tb * 4:(tb + 1) * 4, :], in_=bk.ap.rearrange("p (a d) -> p a d", d=128)),
                         reads=[bk.key], writes=[v_sb.key])
                def att_s1(j):
                    a0 = max(0, 4 - j)
                    pT = pTs[cnt["pt"] % 3]
                    e1 = e1s[cnt["pt"] % 3]
                    cnt["pt"] += 1
                    bkA = S.bank()
                    bkB = S.bank()
                    for a in range(a0, 5):
                        kt = j - 4 + a
                        bk_, col = (bkA, a) if a < 4 else (bkB, 0)
                        S.op("pe", lambda e, bk_=bk_, col=col, kt=kt, j=j: e.matmul(bk_.ap[:, col * 128:(col + 1) * 128], lhsT=k_sb[:, kt * 128:(kt + 1) * 128],
                                                                                  rhs=q_sb[:, j * 128:(j + 1) * 128], start=True, stop=True),
                             reads=[k_sb.key, q_sb.key], writes=[bk_.key])
                    if a0 < 4:
                        S.op("dve", lambda e, a0=a0, bkA=bkA, e1=e1: e.tensor_tensor(out=e1[:, a0:4, :], in0=bkA.ap.rearrange("p (a q) -> p a q", q=128)[:, a0:4, :],
                                                                                    in1=bias_sb[:, a0:4, :], op=ALU.add),
                             reads=[bkA.key, bias_sb.key], writes=[e1.key])
                    S.op("dve", lambda e, bkB=bkB, e1=e1: e.tensor_tensor(out=e1[:, 4, :], in0=bkB.ap[:, 0:128], in1=bias_sb[:, 4, :], op=ALU.add),
                         reads=[bkB.key, bias_sb.key], writes=[e1.key])
                    S.op("act", lambda e, a0=a0, e1=e1, pT=pT: e.activation(out=pT[:, a0:5, :], in_=e1[:, a0:5, :], func=AF.Exp),
                         reads=[e1.key], writes=[pT.key])
                    return a0, pT

                def att_s2(j, a0, pT):
                    bko = S.bank()
                    fns = []
                    for a in range(a0, 5):
                        kt = j - 4 + a
                        fns.append(lambda e, a=a, kt=kt, bko=bko, pT=pT, a0=a0: e.matmul(bko.ap[:, 0:128], lhsT=v_sb[:, kt, :], rhs=pT[:, a, :],
                                                                                        start=(a == a0), stop=(a == 4)))
                    for a in range(a0, 5):
                        fns.append(lambda e, a=a, bko=bko, pT=pT, a0=a0: e.matmul(bko.ap[:, 128:256], lhsT=ones_b, rhs=pT[:, a, :],
                                                                                 start=(a == a0), stop=(a == 4)))
                    S.group("pe", fns, reads=[v_sb.key, pT.key] + CONST, writes=[bko.key])
                    rz = rz_t[j % 2]
                    S.op("dve", lambda e, rz=rz, bko=bko: e.reciprocal(out=rz[:], in_=bko.ap[:, 128:256]), reads=[bko.key], writes=[rz.key])
                    S.op("dve", lambda e, rz=rz, bko=bko, j=j: e.tensor_tensor(out=o_sb[:, j * 128:(j + 1) * 128], in0=bko.ap[:, 0:128], in1=rz[:], op=ALU.mult),
                         reads=[bko.key, rz.key], writes=[o_sb.key])
                pend = att_s1(0)
                for j in range(NT):
                    nxt = att_s1(j + 1) if j + 1 < NT else None
                    att_s2(j, *pend)
                    pend = nxt
                cat = cat_sb[cnt["cat"] % 2]
                cnt["cat"] += 1
                for tb in range(NB):
                    fm_rmsnorm(o_sb[:, tb * 512:(tb + 1) * 512], [o_sb.key], gv_sb[:, 2 + h:3 + h], ["gv"], cat[:, tb * 512:(tb + 1) * 512], [cat.key], next_tmp())
                S.dma("sp", catT_d[h * 128:(h + 1) * 128, :], cat[:], reads=[cat.key], writes=["catT"])

            for h in range(NH):
                wq_t = get_w(3072 + h * 128)
                wk_t = get_w(4096 + h * 128)
                wv_t = get_w(5120 + h * 128)
                wg_t = get_w(6144 + h * 128)
                for tb in range(NB):
                    hb = next_hblk(tb)
                    cs = cs_t[tb % 2]
                    S.dma("sp", cs[:, 0, :], cosT[:, tb * 512:(tb + 1) * 512], writes=[cs.key])
                    S.dma("sp", cs[:, 1, :], sinT[:, tb * 512:(tb + 1) * 512], writes=[cs.key])
                    for which, wt in (("q", wq_t), ("k", wk_t)):
                        bk = S.bank()
                        proj_fm(bk, wt, hb)
                        xs = xs_t[cnt["x"] % 2]
                        t1 = t1_t[cnt["x"] % 2]
                        t2 = t2_t[cnt["x"] % 2]
                        rot = rot_t[cnt["x"] % 2]
                        cnt["x"] += 1
                        S.op("act", lambda e, xs=xs, bk=bk: e.copy(out=xs[:], in_=bk.ap), reads=[bk.key], writes=[xs.key])
                        bk2 = S.bank()
                        S.op("pe", lambda e, bk2=bk2, xs=xs: e.matmul(bk2.ap, lhsT=swap_f, rhs=xs[:], start=True, stop=True), reads=[xs.key] + CONST, writes=[bk2.key])
                        S.op("dve", lambda e, t1=t1, xs=xs, cs=cs: e.tensor_tensor(out=t1[:], in0=xs[:], in1=cs[:, 0, :], op=ALU.mult),
                             reads=[xs.key, cs.key], writes=[t1.key])
                        S.op("dve", lambda e, t2=t2, bk2=bk2, cs=cs: e.tensor_tensor(out=t2[:], in0=bk2.ap, in1=cs[:, 1, :], op=ALU.mult),
                             reads=[bk2.key, cs.key], writes=[t2.key])
                        sl = slice(tb * 512, (tb + 1) * 512)
                        if which == "q":
                            S.op("dve", lambda e, rot=rot, t1=t1, t2=t2: e.tensor_tensor(out=rot[:], in0=t1[:], in1=t2[:], op=ALU.add),
                                 reads=[t1.key, t2.key], writes=[rot.key])
                            S.op("act", lambda e, rot=rot, sl=sl: e.copy(out=q_sb[:, sl], in_=rot[:]), reads=[rot.key], writes=[q_sb.key])
                            S.op("dve", lambda e, rot=rot, sl=sl, h=h: e.tensor_tensor(
                                out=qd_sb[:, sl].rearrange("p (c q) -> p c q", q=64), in0=rot[:].rearrange("p (c q) -> p c q", q=64),
                                in1=cst_sb[:, C_QD + h * 64:C_QD + (h + 1) * 64].unsqueeze(1).broadcast_to([128, 8, 64]), op=ALU.mult),
                                reads=[rot.key] + CONST, writes=[qd_sb.key])
                        else:
                            S.op("dve", lambda e, t1=t1, t2=t2, sl=sl: e.tensor_tensor(out=k_sb[:, sl], in0=t1[:], in1=t2[:], op=ALU.add),
                                 reads=[t1.key, t2.key], writes=[k_sb.key])
                    bk = S.bank()
                    bv = bk.ap.bitcast(BF16)
                    S.group("pe", [lambda e, c=c, bv=bv, tb=tb: e.transpose(out=bv[0:64, c * 128:(c + 1) * 128], in_=k_sb[:, (tb * 8 + c) * 64:(tb * 8 + c + 1) * 64],
                                                                          identity=ident_b) for c in range(8)],
                            reads=[k_sb.key] + CONST, writes=[bk.key])
                    S.op("dve", lambda e, bv=bv, tb=tb, h=h: e.tensor_scalar(out=kd_c[0:64, tb * 8:(tb + 1) * 8, :], in0=bv[0:64, :].rearrange("p (c d) -> p c d", d=128),
                                                                            scalar1=cst_sb[0:64, C_KD + h:C_KD + h + 1], scalar2=None, op0=ALU.mult),
                         reads=[bk.key] + CONST, writes=[kd_c.key])
                    for half in range(2):
                        bk = S.bank()
                        for c in range(4):
                            ch = half * 4 + c
                            S.group("pe", [lambda e, kc=kc, c=c, ch=ch, bk=bk, hb=hb, wv_t=wv_t: e.matmul(bk.ap[0:64, c * 128:(c + 1) * 128], lhsT=hb[:, kc, ch * 64:(ch + 1) * 64],
                                                                                                        rhs=wv_t[:, kc, :], start=(kc == 0), stop=(kc == KC - 1)) for kc in range(KC)],
                                    reads=[hb.key, wv_t.key], writes=[bk.key])
                        S.op("act", lambda e, bk=bk, tb=tb, half=half: e.copy(out=vr_c[0:64, tb * 8 + half * 4:tb * 8 + half * 4 + 4, :],
                                                                             in_=bk.ap[0:64, :].rearrange("p (c d) -> p c d", d=128)),
                             reads=[bk.key], writes=[vr_c.key])
                    bk = S.bank()
                    proj_fm(bk, wg_t, hb)
                    S.op("act", lambda e, bk=bk, tb=tb: e.activation(out=gs_sb[:, tb * 512:(tb + 1) * 512], in_=bk.ap, func=AF.Silu), reads=[bk.key], writes=[gs_sb.key])
                def ret_A(n):
                    csl = slice(n * 64, (n + 1) * 64)
                    sm = sm_t[n % 3]
                    bks = S.bank()
                    S.op("pe", lambda e, bks=bks, csl=csl: e.matmul(bks.ap[0:64, 0:64], lhsT=k_sb[:, csl], rhs=q_sb[:, csl], start=True, stop=True),
                         reads=[k_sb.key, q_sb.key], writes=[bks.key])
                    S.op("dve", lambda e, bks=bks, sm=sm, h=h: e.tensor_tensor(out=sm[0:64, :], in0=bks.ap[0:64, 0:64], in1=cst_sb[0:64, C_DEC + h * 64:C_DEC + (h + 1) * 64], op=ALU.mult),
                         reads=[bks.key] + CONST, writes=[sm.key])

                def ret_C(n):
                    bkv = S.bank()
                    S.op("pe", lambda e, bkv=bkv, n=n: e.matmul(bkv.ap[:, 0:128], lhsT=kd_c[0:64, n, :], rhs=vr_c[0:64, n, :], start=True, stop=True),
                         reads=[kd_c.key, vr_c.key], writes=[bkv.key])
                    if n == 0:
                        S.op("dve", lambda e, bkv=bkv: e.tensor_copy(out=st_f[:], in_=bkv.ap[:, 0:128]), reads=[bkv.key], writes=[st_f.key])
                    else:
                        S.op("dve", lambda e, bkv=bkv, h=h: e.scalar_tensor_tensor(out=st_f[:], in0=st_f[:], scalar=cst_sb[:, C_CD + h:C_CD + h + 1], in1=bkv.ap[:, 0:128],
                                                                                  op0=ALU.mult, op1=ALU.add),
                             reads=[bkv.key, st_f.key] + CONST, writes=[st_f.key])
                    sb = st_b[(n + 1) % 2]
                    S.op("act", lambda e, sb=sb: e.copy(out=sb[:], in_=st_f[:]), reads=[st_f.key], writes=[sb.key])

                def ret_B(n):
                    csl = slice(n * 64, (n + 1) * 64)
                    sm = sm_t[n % 3]
                    bko = S.bank()
                    fns = [lambda e, bko=bko, sm=sm, n=n: e.matmul(bko.ap[:, 0:64], lhsT=vr_c[0:64, n, :], rhs=sm[0:64, :], start=True, stop=(n == 0))]
                    rds = [vr_c.key, sm.key]
                    if n > 0:
                        sb = st_b[n % 2]
                        fns.append(lambda e, bko=bko, sb=sb, csl=csl: e.matmul(bko.ap[:, 0:64], lhsT=sb[:], rhs=qd_sb[:, csl], start=False, stop=True))
                        rds += [sb.key, qd_sb.key]
                    S.group("pe", fns, reads=rds, writes=[bko.key])
                    S.op("act", lambda e, bko=bko, csl=csl: e.copy(out=o_sb[:, csl], in_=bko.ap[:, 0:64]), reads=[bko.key], writes=[o_sb.key])
                ret_A(0)
                if NCH > 1:
                    ret_A(1)
                for n in range(NCH):
                    if n < NCH - 1:
                        ret_C(n)
                    if n + 2 < NCH:
                        ret_A(n + 2)
                    ret_B(n)
                cat = cat_sb[cnt["cat"] % 2]
                cnt["cat"] += 1
                for tb in range(NB):
                    nf = nrm_f[tb % 2]
                    sl = slice(tb * 512, (tb + 1) * 512)
                    fm_rmsnorm(o_sb[:, sl], [o_sb.key], gv_sb[:, 10 + h:11 + h], ["gv"], nf[:], [nf.key], next_tmp())
                    S.op("dve", lambda e, nf=nf, sl=sl, cat=cat: e.tensor_tensor(out=cat[:, sl], in0=nf[:], in1=gs_sb[:, sl], op=ALU.mult),
                         reads=[nf.key, gs_sb.key], writes=[cat.key])
                S.dma("sp", catT_d[(NH + h) * 128:(NH + h + 1) * 128, :], cat[:], reads=[cat.key], writes=["catT"])
            S.barrier()

            AR.reset()
            wob = [AR.tile("wob", [KC, 512], BF16) for _ in range(2)]
            cblk = [AR.tile("cblk", [KC, 512], BF16) for _ in range(2)]
            xp = [AR.tile("xp", [512], F32) for _ in range(4)]
            w_out_v = w_out[l].rearrange("(kc p) c -> p kc c", p=128)
            k3 = 0
            for cb in range(4):
                wo = wob[cb % 2]
                S.dma("pool", wo[:], w_out_v[:, :, cb * 512:(cb + 1) * 512], writes=[wo.key])
                for tb in range(NB):
                    cbk = cblk[(cb * NB + tb) % 2]
                    S.dma("sp", cbk[:], rowsT(catT_d)[:, :, tb * 512:(tb + 1) * 512], reads=["catT"], writes=[cbk.key])
                    for ti in range(4):
                        i = tb * 4 + ti
                        xq = xp[k3 % 4]
                        k3 += 1
                        S.dma("sp", xq[:], xsrc[i * 128:(i + 1) * 128, cb * 512:(cb + 1) * 512], reads=["ydram"], writes=[xq.key])
                        bk = S.bank()
                        S.group("pe", [lambda e, kc=kc, bk=bk, cbk=cbk, ti=ti, wo=wo: e.matmul(bk.ap, lhsT=cbk[:, kc, ti * 128:(ti + 1) * 128], rhs=wo[:, kc, :],
                                                                                             start=(kc == 0), stop=(kc == KC - 1)) for kc in range(KC)],
                                reads=[cbk.key, wo.key], writes=[bk.key])
                        S.op("dve", lambda e, xq=xq, bk=bk: e.tensor_tensor(out=xq[:], in0=bk.ap, in1=xq[:], op=ALU.add), reads=[bk.key, xq.key], writes=[xq.key])
                        S.dma("sp", y[i * 128:(i + 1) * 128, cb * 512:(cb + 1) * 512], xq[:], reads=[xq.key], writes=["y1"])
            S.barrier()

            AR.reset()
            xts = [AR.tile("xt", [D], F32) for _ in range(2)]

            def src2(i):
                xt = xts[i % 2]
                S.dma("sp", xt[:], y[i * 128:(i + 1) * 128, :], writes=[xt.key])
                return xt
            norm_tiles(l, g2, src2, h2T_d, "h2T")
            S.barrier()

            AR.reset()
            HT = min(1024, T)
            NHF = T // HT
            NBH = HT // 512
            h2h = AR.tile("h2h", [KC, HT], BF16)
            wqj = [AR.tile("wqj", [KC, 128], BF16) for _ in range(2)]
            qp_blk = AR.tile("qp_blk", [16, 512], F32)
            sk_sb = AR.tile("sk_sb", [16, 128], F32)
            Gblk = AR.tile("Gblk", [128, 128], BF16)
            s_sb = AR.tile("s_sb", [16, 128], F32)
            m1 = AR.tile("m1", [16, 16], F32)
            idx = AR.tile("idx", [16, 16], U32)
            idxf = AR.tile("idxf", [16, 16], F32)
            wk = [AR.tile("wk", [128], F32) for _ in range(4)]
            cand = AR.tile("cand", [8, 16, 16], F32)
            E = cand
            wk2 = [AR.tile("wk2", [256], F32) for _ in range(4)]
            g16 = AR.tile("g16", [8, 16], F32)
            cidx = AR.tile("cidx", [8, 16], U32)
            cf = AR.tile("cf", [128], F32)
            ci = AR.tile("ci", [128], I32)
            a0t = AR.tile("a0t", [128], F32)
            b0t = AR.tile("b0t", [128], F32)
            ngt = AR.tile("ngt", [128], F32)
            cabf = AR.tile("cabf", [2, 8, 16], F32)
            sel = AR.tile("sel", [3, 128], F32)
            gz = AR.tile("gz", [16], F32)
            tr_sb = AR.tile("tr_sb", [3, 128], F32)
            A4 = [AR.tile("A4", [4, 128], BF16) for _ in range(3)]
            B4 = [AR.tile("B4", [4, 128], BF16) for _ in range(3)]
            ublk = [AR.tile("ublk", [KC, 256], BF16) for _ in range(2)]
            ge_sb = [AR.tile("ge_sb", [HT], BF16) for _ in range(2)]
            S.dma("sp", sk_sb[:], skT[l].rearrange("j p k -> p j k"), writes=[sk_sb.key])
            wq_v = wq[l].rearrange("(kc p) c -> p kc c", p=128)
            uT_v = uT[l].rearrange("(kc p) e -> p kc e", p=128)
            kw = 0
            k4 = 0

            def ugen(hf):
                for eg in range(NEXP // 256):
                    ub = ublk[eg % 2]
                    S.dma("pool", ub[:], uT_v[:, :, eg * 256:(eg + 1) * 256], writes=[ub.key])
                    for ei in range(2):
                        g = eg * 2 + ei
                        ge_ = ge_sb[g % 2]
                        for tbl in range(NBH):
                            sl = slice(tbl * 512, (tbl + 1) * 512)
                            bk = S.bank()
                            S.group("pe", [lambda e, kc=kc, bk=bk, ub=ub, ei=ei, sl=sl: e.matmul(bk.ap, lhsT=ub[:, kc, ei * 128:(ei + 1) * 128], rhs=h2h[:, kc, sl],
                                                                                               start=(kc == 0), stop=(kc == KC - 1)) for kc in range(KC)],
                                    reads=[ub.key, h2h.key], writes=[bk.key])
                            S.op("act", lambda e, bk=bk, ge_=ge_, sl=sl: e.activation(out=ge_[:, sl], in_=bk.ap, func=AF.Gelu), reads=[bk.key], writes=[ge_.key])
                            yield
                        S.dma("sp", geT_d[g * 128:(g + 1) * 128, hf * HT:(hf + 1) * HT], ge_[:], reads=[ge_.key], writes=["geT"])

            for hf in range(NHF):
                for tbl in range(NBH):
                    S.dma("sp", h2h[:, :, tbl * 512:(tbl + 1) * 512], rowsT(h2T_d)[:, :, hf * HT + tbl * 512:hf * HT + (tbl + 1) * 512], reads=["h2T"], writes=[h2h.key])
                ug = ugen(hf)

                def pull(n):
                    for _ in range(n):
                        next(ug, None)
                for tbl in range(NBH):
                    for j in range(16):
                        wt = wqj[kw % 2]
                        kw += 1
                        S.dma("pool", wt[:], wq_v[:, :, j * 128:(j + 1) * 128], writes=[wt.key])
                        bk = S.bank()
                        proj_fm(bk, wt, h2h, 512, tbl * 512)
                        S.op("act", lambda e, bk=bk, j=j: e.copy(out=qp_blk[:, j, :], in_=bk.ap), reads=[bk.key], writes=[qp_blk.key])
                    for ti in range(4):
                        i = hf * (HT // 128) + tbl * 4 + ti
                        tsl = slice(ti * 128, (ti + 1) * 128)
                        for jb in range(4):
                            bk = S.bank()
                            S.group("pe", [lambda e, jj=jj, jb=jb, bk=bk, tsl=tsl: e.matmul(bk.ap[:, jj * 128:(jj + 1) * 128], lhsT=qp_blk[:, jb * 4 + jj, tsl], rhs=sk_sb[:, jb * 4 + jj, :],
                                                                                          start=True, stop=True) for jj in range(4)],
                                    reads=[qp_blk.key, sk_sb.key], writes=[bk.key])
                            S.op("act", lambda e, bk=bk, jb=jb: e.copy(out=s_sb[:, jb * 4:(jb + 1) * 4, :], in_=bk.ap.rearrange("p (a k) -> p a k", k=128)),
                                 reads=[bk.key], writes=[s_sb.key])
                        pull(24)
                        for jg in range(4):
                            js = [jg * 4 + q_ for q_ in range(4)]
                            for j in js:
                                S.op("dve", lambda e, j=j: e.max(out=m1[:, j, 0:8], in_=s_sb[:, j, :]), reads=[s_sb.key], writes=[(m1.key, j)])
                            for j in js:
                                w_ = wk[j % 4]
                                S.op("dve", lambda e, j=j, w_=w_: e.match_replace(out=w_[:], in_to_replace=m1[:, j, 0:8], in_values=s_sb[:, j, :], imm_value=-1e30),
                                     reads=[s_sb.key, (m1.key, j)], writes=[w_.key])
                            for j in js:
                                w_ = wk[j % 4]
                                S.op("dve", lambda e, j=j, w_=w_: e.max(out=m1[:, j, 8:16], in_=w_[:]), reads=[w_.key], writes=[(m1.key, j, 1)])
                            for j in js:
                                S.op("dve", lambda e, j=j: e.max_index(out=idx[:, j, 0:8], in_max=m1[:, j, 0:8], in_values=s_sb[:, j, :]), reads=[s_sb.key, (m1.key, j)], writes=[(idx.key, j)])
                            for j in js:
                                w_ = wk[j % 4]
                                S.op("dve", lambda e, j=j, w_=w_: e.max_index(out=idx[:, j, 8:16], in_max=m1[:, j, 8:16], in_values=w_[:]), reads=[w_.key, (m1.key, j, 1)], writes=[(idx.key, j, 1)])
                        m1keys = [(m1.key, j) for j in range(16)] + [(m1.key, j, 1) for j in range(16)]
                        idxkeys = [(idx.key, j) for j in range(16)] + [(idx.key, j, 1) for j in range(16)]
                        S.op("dve", lambda e: e.tensor_copy(out=idxf[:], in_=idx[:]), reads=idxkeys, writes=[idxf.key])
                        m1v = m1[:].rearrange("p (h two) k -> p h two k", two=2)
                        idv = idxf[:].rearrange("p (h two) k -> p h two k", two=2)
                        S.op("dve", lambda e, m1v=m1v: e.tensor_tensor(out=cand[:], in0=m1v[:, :, 0, :].unsqueeze(3).broadcast_to([128, 8, 16, 16]),
                                                                      in1=m1v[:, :, 1, :].unsqueeze(2).broadcast_to([128, 8, 16, 16]), op=ALU.add),
                             reads=m1keys, writes=[cand.key])
                        for hg in range(2):
                            hs = [hg * 4 + q_ for q_ in range(4)]
                            cvs = {h: cand[:, h].rearrange("p a b -> p (a b)") for h in hs}
                            for h in hs:
                                S.op("dve", lambda e, h=h, cv=cvs[h]: e.max(out=g16[:, h, 0:8], in_=cv), reads=[cand.key], writes=[(g16.key, h)])
                            for h in hs:
                                w_ = wk2[h % 4]
                                S.op("dve", lambda e, h=h, cv=cvs[h], w_=w_: e.match_replace(out=w_[:], in_to_replace=g16[:, h, 0:8], in_values=cv, imm_value=-1e30),
                                     reads=[cand.key, (g16.key, h)], writes=[w_.key])
                            for h in hs:
                                w_ = wk2[h % 4]
                                S.op("dve", lambda e, h=h, w_=w_: e.max(out=g16[:, h, 8:16], in_=w_[:]), reads=[w_.key], writes=[(g16.key, h, 1)])
                            for h in hs:
                                S.op("dve", lambda e, h=h, cv=cvs[h]: e.max_index(out=cidx[:, h, 0:8], in_max=g16[:, h, 0:8], in_values=cv), reads=[cand.key, (g16.key, h)], writes=[(cidx.key, h)])
                            for h in hs:
                                w_ = wk2[h % 4]
                                S.op("dve", lambda e, h=h, w_=w_: e.max_index(out=cidx[:, h, 8:16], in_max=g16[:, h, 8:16], in_values=w_[:]), reads=[w_.key, (g16.key, h, 1)], writes=[(cidx.key, h, 1)])
                        g16keys = [(g16.key, h) for h in range(8)] + [(g16.key, h, 1) for h in range(8)]
                        cidxkeys = [(cidx.key, h) for h in range(8)] + [(cidx.key, h, 1) for h in range(8)]
                        cflat = cidx[:].rearrange("p h k -> p (h k)")
                        ca_v = cabf[:, 0].rearrange("p h k -> p (h k)")
                        cb_v = cabf[:, 1].rearrange("p h k -> p (h k)")
                        S.op("dve", lambda e, cflat=cflat: e.tensor_copy(out=cf[:], in_=cflat), reads=cidxkeys, writes=[cf.key])
                        S.op("dve", lambda e: e.tensor_scalar(out=ci[:], in0=cf[:], scalar1=1.0 / 16.0, scalar2=None, op0=ALU.mult), reads=[cf.key], writes=[ci.key])
                        S.op("dve", lambda e: e.tensor_copy(out=a0t[:], in_=ci[:]), reads=[ci.key], writes=[a0t.key])
                        S.op("dve", lambda e: e.scalar_tensor_tensor(out=b0t[:], in0=a0t[:], scalar=-16.0, in1=cf[:], op0=ALU.mult, op1=ALU.add),
                             reads=[a0t.key, cf.key], writes=[b0t.key])
                        S.op("dve", lambda e: e.tensor_single_scalar(out=ngt[:], in_=b0t[:], scalar=0.0, op=ALU.is_lt), reads=[b0t.key], writes=[ngt.key])
                        S.op("dve", lambda e, ca_v=ca_v: e.tensor_tensor(out=ca_v, in0=a0t[:], in1=ngt[:], op=ALU.subtract), reads=[a0t.key, ngt.key], writes=[(cabf.key, 0)])
                        S.op("dve", lambda e, cb_v=cb_v: e.scalar_tensor_tensor(out=cb_v, in0=ngt[:], scalar=16.0, in1=b0t[:], op0=ALU.mult, op1=ALU.add),
                             reads=[ngt.key, b0t.key], writes=[(cabf.key, 1)])
                        gsel = sel[:, 2, :].rearrange("p (h k) -> p h k", k=16)
                        S.op("dve", lambda e, gsel=gsel: e.tensor_tensor(out=gsel, in0=g16[:], in1=g16[:, :, 0:1].broadcast_to([128, 8, 16]), op=ALU.subtract),
                             reads=g16keys, writes=[(sel.key, 2)])
                        S.op("act", lambda e, gsel=gsel: e.activation(out=gsel, in_=gsel, func=AF.Exp), reads=[(sel.key, 2)], writes=[(sel.key, 2)])
                        io4 = iota16.rearrange("p (h k a) -> p h k a", k=16, a=16)
                        for p_ in range(2):
                            S.op("dve", lambda e, p_=p_: e.tensor_tensor(out=E[:], in0=io4, in1=cabf[:, p_].unsqueeze(3).broadcast_to([128, 8, 16, 16]), op=ALU.is_equal),
                                 reads=[(cabf.key, p_)] + CONST + cidxkeys, writes=[E.key])
                            S.op("dve", lambda e, p_=p_, idv=idv: e.tensor_tensor(out=E[:], in0=E[:], in1=idv[:, :, p_, :].unsqueeze(2).broadcast_to([128, 8, 16, 16]), op=ALU.mult),
                                 reads=[idxf.key, E.key], writes=[E.key])
                            S.op("dve", lambda e, p_=p_: e.tensor_reduce(out=sel[:, p_, :], in_=E[:].rearrange("p h k a -> p (h k) a"), axis=AX.X, op=ALU.add),
                                 reads=[E.key], writes=[(sel.key, p_)])
                        S.op("dve", lambda e, gsel=gsel: e.tensor_reduce(out=gz[:, 0:8], in_=gsel, axis=AX.X, op=ALU.add), reads=[(sel.key, 2)], writes=[gz.key])
                        S.op("dve", lambda e: e.reciprocal(out=gz[:, 8:16], in_=gz[:, 0:8]), reads=[gz.key], writes=[gz.key])
                        S.op("dve", lambda e, gsel=gsel: e.tensor_tensor(out=gsel, in0=gsel, in1=gz[:, 8:16].unsqueeze(2).broadcast_to([128, 8, 16]), op=ALU.mult),
                             reads=[(sel.key, 2), gz.key], writes=[(sel.key, 2)])
                        bk = S.bank()
                        S.group("pe", [lambda e, c=c, bk=bk: e.transpose(out=bk.ap[:, c * 128:(c + 1) * 128], in_=sel[:, c, :], identity=ident_f) for c in range(3)],
                                reads=[(sel.key, 0), (sel.key, 1), (sel.key, 2)] + CONST, writes=[bk.key])
                        S.op("act", lambda e, bk=bk: e.copy(out=tr_sb[:], in_=bk.ap[:, 0:384].rearrange("p (c t) -> p c t", t=128)), reads=[bk.key], writes=[tr_sb.key])
                        for t4 in range(32):
                            a4 = A4[k4 % 3]
                            b4 = B4[k4 % 3]
                            k4 += 1
                            fns = []
                            for tt in range(4):
                                t = t4 * 4 + tt
                                fns.append(lambda e, a4=a4, tt=tt, t=t: e.tensor_scalar(out=a4[:, tt, :], in0=iota_b, scalar1=tr_sb[:, 0, t:t + 1], scalar2=tr_sb[:, 2, t:t + 1],
                                                                                     op0=ALU.is_equal, op1=ALU.mult))
                                fns.append(lambda e, b4=b4, tt=tt, t=t: e.tensor_scalar(out=b4[:, tt, :], in0=iota_b, scalar1=tr_sb[:, 1, t:t + 1], scalar2=None,
                                                                                     op0=ALU.is_equal))
                            S.group("dve", fns, reads=[tr_sb.key] + CONST, writes=[a4.key, b4.key])
                            bk = S.bank()
                            S.group("pe", [lambda e, tt=tt, bk=bk, a4=a4, b4=b4: e.matmul(bk.ap[:, tt * 128:(tt + 1) * 128], lhsT=b4[:, tt, :], rhs=a4[:, tt, :], start=True, stop=True)
                                           for tt in range(4)], reads=[a4.key, b4.key], writes=[bk.key])
                            S.op("act", lambda e, bk=bk, t4=t4: e.copy(out=Gblk[:, :, t4 * 4:(t4 + 1) * 4].rearrange("p i t -> p t i"),
                                                                     in_=bk.ap.rearrange("p (t i) -> p t i", i=128)),
                                 reads=[bk.key], writes=[Gblk.key])
                            if t4 % 4 == 3:
                                pull(1)
                        for qd4 in range(4):
                            S.dma("sp", G_d.rearrange("a b t -> b a t")[:, qd4 * 32:(qd4 + 1) * 32, i * 128:(i + 1) * 128], Gblk[:, qd4 * 32:(qd4 + 1) * 32, :],
                                  reads=[Gblk.key], writes=["Gd"])
                for _ in ug:
                    pass
            S.barrier()

            AR.reset()
            ga4 = [AR.tile("ga4", [4, TG * 128], BF16) for _ in range(3)]
            G4 = [AR.tile("G4", [4, TG * 128], BF16) for _ in range(2)]
            v4 = [AR.tile("v4", [4, 512], BF16) for _ in range(3)]
            xo = [AR.tile("xo", [512], F32) for _ in range(4)]
            gaT_v = gaT_d.rearrange("(g p) t -> p g t", p=128)
            geT_v = geT_d.rearrange("(g p) t -> p g t", p=128)
            pv_v = pv[l].rearrange("(g p) d -> p g d", p=128)
            k7 = 0
            k8 = 0
            for hf in range(NT // TG):
                tsl = slice(hf * TG * 128, (hf + 1) * TG * 128)
                for cb in range(4):
                    for g4 in range(32):
                        ga_ = ga4[k7 % 3]
                        v_ = v4[k7 % 3]
                        k7 += 1
                        gsl = slice(g4 * 4, (g4 + 1) * 4)
                        if cb == 0:
                            G_ = G4[g4 % 2]
                            S.dma("sp", ga_[:], geT_v[:, gsl, tsl], writes=[ga_.key])
                            S.dma("sp", G_[:], G_d[gsl].rearrange("a b t -> b a t")[:, :, tsl], writes=[G_.key])
                            S.op("dve", lambda e, ga_=ga_, G_=G_: e.tensor_tensor(out=ga_[:], in0=ga_[:], in1=G_[:], op=ALU.mult), reads=[ga_.key, G_.key], writes=[ga_.key])
                            S.dma("act", gaT_v[:, gsl, tsl], ga_[:], reads=[ga_.key], writes=["gaT"])
                        else:
                            S.dma("sp", ga_[:], gaT_v[:, gsl, tsl], reads=["gaT"], writes=[ga_.key])
                        S.dma("pool", v_[:], pv_v[:, gsl, cb * 512:(cb + 1) * 512], writes=[v_.key])
                        fns = []
                        for gi in range(4):
                            g = g4 * 4 + gi
                            for tt in range(TG):
                                fns.append(lambda e, gi=gi, tt=tt, g=g, ga_=ga_, v_=v_: e.matmul(S.banks[tt].ap, lhsT=ga_[:, gi, tt * 128:(tt + 1) * 128], rhs=v_[:, gi, :],
                                                                                           start=(g == 0), stop=(g == 127)))
                        S.group("pe", fns, reads=[ga_.key, v_.key], writes=[S.banks[tt].key for tt in range(TG)])
                    for tt in range(TG):
                        i = hf * TG + tt
                        xq = xo[k8 % 4]
                        k8 += 1
                        S.dma("sp", xq[:], y[i * 128:(i + 1) * 128, cb * 512:(cb + 1) * 512], writes=[xq.key])
                        S.op("dve", lambda e, xq=xq, tt=tt: e.tensor_tensor(out=xq[:], in0=S.banks[tt].ap, in1=xq[:], op=ALU.add),
                             reads=[S.banks[tt].key, xq.key], writes=[xq.key])
                        S.dma("act", y[i * 128:(i + 1) * 128, cb * 512:(cb + 1) * 512], xq[:], reads=[xq.key], writes=["ydram"])
            S.barrier()

        S.finish()
        with nc.Block() as block:
            @block.tensor
            def _(e):
                for f in S.prog["pe"]:
                    f(e)

            @block.scalar
            def _(e):
                for f in S.prog["act"]:
                    f(e)

            @block.vector
            def _(e):
                for f in S.prog["dve"]:
                    f(e)

            @block.gpsimd
            def _(e):
                for f in S.prog["pool"]:
                    f(e)

            @block.sync
            def _(e):
                for f in S.prog["sp"]:
                    f(e)
    return nc


def make_consts(T):
    cst = np.zeros((128, CW), np.float32)
    cst[:, C_ID:C_ID + 128] = np.eye(128, dtype=np.float32)
    k = np.arange(128)
    sw = np.zeros((128, 128), np.float32)
    sw[(k + 64) % 128, k] = 1.0
    cst[:, C_SW:C_SW + 128] = sw
    cst[:, C_ON:C_ON + 128] = 1.0
    cst[:, C_IO:C_IO + 128] = np.arange(128, dtype=np.float32)[None, :]
    cst[:, C_I16:C_I16 + 2048] = (np.arange(2048) % 16).astype(np.float32)[None, :]
    hh = np.arange(8, dtype=np.float32)
    lg = np.log1p(-(np.float32(2.0) ** (-5.0 - hh))).astype(np.float32)
    pos = np.arange(64, dtype=np.float32)
    diff = pos[:, None] - pos[None, :]
    dec = np.where(diff >= 0, np.exp(np.maximum(diff, 0.0)[None] * lg[:, None, None]), 0.0).astype(np.float32)
    decT = np.transpose(dec, (2, 0, 1)) * np.float32(SCALE)
    cst[0:64, C_DEC:C_DEC + 512] = decT.reshape(64, 512)
    qd = np.exp((pos + 1.0)[None, :] * lg[:, None]).astype(np.float32)
    cst[:, C_QD:C_QD + 512] = qd.reshape(1, 512)
    kd = (np.exp((63.0 - pos)[None, :] * lg[:, None]) * SCALE).astype(np.float32)
    cst[0:64, C_KD:C_KD + 8] = kd.T
    cst[64:128, C_KD:C_KD + 8] = kd.T
    cst[:, C_CD:C_CD + 8] = np.exp(64.0 * lg)[None, :]
    half = 64
    inv_freq = (np.float32(10000.0) ** (-np.arange(half, dtype=np.float32) / half)).astype(np.float32)
    ang = np.arange(T, dtype=np.float32)[:, None] * inv_freq[None, :]
    cos = np.cos(ang).astype(np.float32).T
    sin = np.sin(ang).astype(np.float32).T
    cosT = np.ascontiguousarray(np.concatenate([cos, cos], 0))
    sinT = np.ascontiguousarray(np.concatenate([-sin, sin], 0))
    return cst, cosT, sinT


def bias_layout(rel_bias):
    kk = np.arange(640)[:, None]
    qq = np.arange(128)[None, :]
    rel = np.clip(qq + 512 - kk, -128, 128) + 128
    ck = kk // 64 - 8
    cq = qq // 64
    valid = (ck >= cq - 8) & (ck <= cq)
    b = rel_bias[:, :, rel]
    return np.ascontiguousarray(np.where(valid[None, None], b, np.float32(-30000.0)).astype(np.float32))


def layout_weights(inp, layers):
    L = list(layers)
    d = {}
    d["w_in"] = np.ascontiguousarray(inp["w_in"][L])
    d["w_out"] = np.ascontiguousarray(inp["w_out"][L])
    d["wq"] = np.ascontiguousarray(inp["peer_wq"][L])
    sk = np.asarray(inp["peer_subkeys"])[L]
    d["skT"] = np.ascontiguousarray(np.transpose(sk, (0, 1, 2, 4, 3)).reshape(len(L), 16, 128, 128))
    d["uT"] = np.ascontiguousarray(np.transpose(np.asarray(inp["peer_u"])[L], (0, 2, 1)))
    d["pv"] = np.ascontiguousarray(inp["peer_v"][L])
    d["g1"] = np.ascontiguousarray(inp["norm1_g"][L])
    d["g2"] = np.ascontiguousarray(inp["norm2_g"][L])
    gv = np.zeros((len(L), 128, 18), np.float32)
    gv[:, :, 0] = np.asarray(inp["qa_norm_g"])[L]
    gv[:, :, 1] = np.asarray(inp["ka_norm_g"])[L]
    gv[:, :, 2:10] = np.transpose(np.asarray(inp["attn_out_g"])[L].reshape(len(L), 8, 128), (0, 2, 1))
    gv[:, :, 10:18] = np.transpose(np.asarray(inp["ret_out_g"])[L].reshape(len(L), 8, 128), (0, 2, 1))
    d["gvec"] = gv
    d["biasT"] = bias_layout(np.asarray(inp["rel_bias"])[L])
    return d


_CACHE = {}


def kernel(**inputs):
    inp = {k: np.asarray(v) for k, v in inputs.items()}
    x = inp["x"]
    B, T, _ = x.shape
    DEPTH = inp["w_in"].shape[0]
    key = (T, DEPTH)
    if key not in _CACHE:
        _CACHE[key] = build(T, DEPTH)
    nc = _CACHE[key]
    cst, cosT, sinT = make_consts(T)
    wd = layout_weights(inp, range(DEPTH))
    wd.update(cst=cst, cosT=cosT, sinT=sinT)
    in_maps = []
    for b in range(B):
        m = dict(wd)
        m["x"] = np.ascontiguousarray(x[b])
        in_maps.append(m)
    res = run_bass_kernel_spmd(nc, in_maps, core_ids=list(range(B)))
    return np.stack([res.results[b]["y"] for b in range(B)], 0).astype(np.float32)
```

```python
import numpy as np
import concourse.bass as bass
import concourse.mybir as mybir
from concourse.bass_utils import run_bass_kernel_spmd
from contextlib import ExitStack

F32 = mybir.dt.float32
BF16 = mybir.dt.bfloat16
U32 = mybir.dt.uint32
I32 = mybir.dt.int32
AF = mybir.ActivationFunctionType
ALU = mybir.AluOpType
AX = mybir.AxisListType

D = 2048
KC = 16
HD = 128
NH = 8
EPS = 1e-6
NEXP = 16384
IN_COLS = 7168
SCALE = HD ** -0.5
EPOCH = 20000

C_ID, C_SW, C_ON, C_IO, C_I16, C_DEC, C_QD, C_KD, C_CD = 0, 128, 256, 384, 512, 2560, 3072, 3584, 3592
CW = 3600


class Sched:
    ENGS = ("pe", "act", "dve", "pool", "sp")

    def __init__(self, nc, stack, n_dma=32, n_ep=8):
        self.nc = nc
        self.prog = {e: [] for e in self.ENGS}
        self.esems = {e: [stack.enter_context(nc.semaphore(f"s_{e}_{i}")) for i in range(n_ep)] for e in self.ENGS}
        self.eidx = {e: 0 for e in self.ENGS}
        self.ecnt = {e: 0 for e in self.ENGS}
        self.dsems = [stack.enter_context(nc.semaphore(f"s_dma_{i}")) for i in range(n_dma)]
        self.dval = {s: 0 for s in self.dsems}
        self.di = 0
        self.res = {}
        self.waited = {e: {} for e in self.ENGS}
        self.pe_sems = set(self.esems["pe"])
        self.banks = None
        self.bi = 0
        self.ninstr = 0

    def _deps(self, reads, writes):
        deps = {}

        def add(s, v):
            if deps.get(s, 0) < v:
                deps[s] = v
        for r in reads:
            st = self.res.get(r)
            if st and st[0]:
                add(*st[0])
        for w in writes:
            st = self.res.get(w)
            if st:
                if st[0]:
                    add(*st[0])
                for s, v in st[1].items():
                    add(s, v)
        return deps

    def _waits(self, eng, deps):
        wd = self.waited[eng]
        for s, v in deps.items():
            if eng == "pe" and s in self.pe_sems:
                continue
            if wd.get(s, 0) >= v:
                continue
            wd[s] = v
            self.prog[eng].append(lambda e, s=s, v=v: e.wait_ge(s, v))
            self.ninstr += 1

    def _mark(self, reads, writes, s, v):
        for r in reads:
            st = self.res.setdefault(r, [None, {}])
            if st[1].get(s, 0) < v:
                st[1][s] = v
        for w in writes:
            self.res[w] = [(s, v), {}]

    def _tick(self, eng):
        if self.ecnt[eng] >= EPOCH:
            self.eidx[eng] += 1
            self.ecnt[eng] = 0
        s = self.esems[eng][self.eidx[eng]]
        self.ecnt[eng] += 1
        return s, self.ecnt[eng]

    def group(self, eng, fns, reads=(), writes=()):
        self._waits(eng, self._deps(reads, writes))
        s, v = self._tick(eng)
        n = len(fns)
        for i, fn in enumerate(fns):
            if i == n - 1:
                self.prog[eng].append(lambda e, fn=fn, s=s: fn(e).then_inc(s, 1))
            else:
                self.prog[eng].append(fn)
        self.ninstr += n
        self._mark(reads, writes, s, v)

    def op(self, eng, fn, reads=(), writes=()):
        self.group(eng, [fn], reads, writes)

    def dma(self, q, out, in_, reads=(), writes=()):
        s = self.dsems[self.di % len(self.dsems)]
        self.di += 1
        deps = self._deps(reads, writes)
        prev = self.dval[s]
        if prev > 0 and deps.get(s, 0) < prev:
            deps[s] = prev
        self._waits(q, deps)
        self.dval[s] = prev + 16
        self.prog[q].append(lambda e, s=s: e.dma_start(out=out, in_=in_).then_inc(s, 16))
        self.ninstr += 1
        self._mark(reads, writes, s, prev + 16)

    def barrier(self):
        cur = {}
        for e in self.ENGS:
            if self.ecnt[e] > 0:
                cur[self.esems[e][self.eidx[e]]] = self.ecnt[e]
        for s, v in self.dval.items():
            if v > 0:
                cur[s] = v
        for e in self.ENGS:
            wd = self.waited[e]
            for s, v in cur.items():
                if wd.get(s, 0) >= v:
                    continue
                wd[s] = v
                self.prog[e].append(lambda en, s=s, v=v: en.wait_ge(s, v))
        self.res = {}

    def finish(self):
        for s, v in self.dval.items():
            if v > 0 and self.waited["sp"].get(s, 0) < v:
                self.prog["sp"].append(lambda e, s=s, v=v: e.wait_ge(s, v))

    def bank(self):
        b = self.banks[self.bi % 8]
        self.bi += 1
        return b


class Tile:
    def __init__(self, ap, key):
        self.ap = ap
        self.key = key

    def __getitem__(self, k):
        return self.ap[k]


class Arena:
    def __init__(self, big, nbytes):
        self.big = big
        self.nbytes = nbytes
        self.off = 0
        self.n = 0

    def reset(self):
        self.off = 0

    def tile(self, name, free, dtype, parts=128):
        esz = 2 if dtype == BF16 else 4
        n = int(np.prod(free))
        nb = (n * esz + 31) // 32 * 32
        assert self.off + nb <= self.nbytes, f"arena overflow at {name}: {self.off}+{nb}>{self.nbytes}"
        a = self.big[:, self.off // 2:(self.off + n * esz) // 2]
        self.off += nb
        if dtype != BF16:
            a = a.bitcast(dtype)
        if len(free) == 2:
            a = a.rearrange("p (a b) -> p a b", b=free[1])
        elif len(free) == 3:
            a = a.rearrange("p (a b c) -> p a b c", b=free[1], c=free[2])
        if parts != 128:
            a = a[0:parts]
        self.n += 1
        return Tile(a, f"{name}#{self.n}")


def build(T, DEPTH, debug=False):
    NT = T // 128
    NB = T // 512
    NCH = T // 64
    assert T % 512 == 0
    TG = min(8, NT)
    nc = bass.Bass("TRN2", target_bir_lowering=False)

    def din(name, shape, dt=F32):
        return nc.dram_tensor(name, list(shape), dt, kind="ExternalInput").ap()

    x_in = din("x", [T, D])
    w_in = din("w_in", [DEPTH, D, IN_COLS])
    w_out = din("w_out", [DEPTH, D, D])
    wq = din("wq", [DEPTH, D, D])
    skT = din("skT", [DEPTH, 16, 128, 128])
    uT = din("uT", [DEPTH, D, NEXP])
    pv = din("pv", [DEPTH, NEXP, D])
    g1 = din("g1", [DEPTH, D])
    g2 = din("g2", [DEPTH, D])
    gvec = din("gvec", [DEPTH, 128, 18])
    biasT = din("biasT", [DEPTH, NH, 640, 128])
    cst = din("cst", [128, CW])
    cosT = din("cosT", [128, T])
    sinT = din("sinT", [128, T])
    y = nc.dram_tensor("y", [T, D], F32, kind="ExternalOutput").ap()
    skind = "ExternalOutput" if debug else "Internal"
    hnT_d = nc.dram_tensor("hnT_d", [D, T], BF16, kind=skind).ap()
    catT_d = nc.dram_tensor("catT_d", [D, T], BF16, kind=skind).ap()
    h2T_d = nc.dram_tensor("h2T_d", [D, T], BF16, kind=skind).ap()
    G_d = nc.dram_tensor("G_d", [128, 128, T], BF16, kind=skind).ap()
    gaT_d = nc.dram_tensor("gaT_d", [NEXP, T], BF16, kind="Internal").ap()
    geT_d = nc.dram_tensor("geT_d", [NEXP, T], BF16, kind="Internal").ap()

    ARENA_BYTES = 190 * 1024
    with ExitStack() as stack:
        ec = stack.enter_context
        S = Sched(nc, stack)
        cst_sb = ec(nc.sbuf_tensor("cst_sb", [128, CW], F32))
        cbf = ec(nc.sbuf_tensor("cbf", [128, 384], BF16))
        gv_sb = ec(nc.sbuf_tensor("gv_sb", [128, 20], F32))
        big = ec(nc.sbuf_tensor("arena", [128, ARENA_BYTES // 2], BF16))
        S.banks = [Tile(ec(nc.psum_tensor(f"bank{i}", [128, 512], F32))[:], f"bank{i}") for i in range(8)]
        AR = Arena(big, ARENA_BYTES)

        ident_f = cst_sb[:, C_ID:C_ID + 128]
        swap_f = cst_sb[:, C_SW:C_SW + 128]
        ones_f = cst_sb[:, C_ON:C_ON + 128]
        iota_f = cst_sb[:, C_IO:C_IO + 128]
        iota16 = cst_sb[:, C_I16:C_I16 + 2048]
        ident_b = cbf[:, 0:128]
        ones_b = cbf[:, 128:256]
        iota_b = cbf[:, 256:384]
        CST = "cst"

        S.dma("sp", cst_sb[:], cst, writes=[CST])
        S.op("dve", lambda e: e.tensor_copy(out=cbf[:, 0:128], in_=cst_sb[:, C_ID:C_ID + 128]), reads=[CST], writes=["cbf"])
        S.op("dve", lambda e: e.tensor_copy(out=cbf[:, 128:256], in_=cst_sb[:, C_ON:C_ON + 128]), reads=[CST], writes=["cbf"])
        S.op("dve", lambda e: e.tensor_copy(out=cbf[:, 256:384], in_=cst_sb[:, C_IO:C_IO + 128]), reads=[CST], writes=["cbf"])
        CONST = [CST, "cbf"]

        def rowsT(d_ap):
            return d_ap.rearrange("(kc p) t -> p kc t", p=128)

        def norm_tiles(l, gsrc, src_fn, dstT_d, phase_tag, post_fn=None):
            gbc = AR.tile("gbc", [D], F32)
            S.dma("sp", gbc[:], gsrc[l:l + 1, :].broadcast_to([128, D]), writes=[gbc.key])
            junk = AR.tile("junk", [D], BF16)
            hn = [AR.tile("hn", [D], BF16) for _ in range(2)]
            hT = [AR.tile("hT", [KC, 512], BF16) for _ in range(2)]
            ss = [AR.tile("ss", [4], F32) for _ in range(2)]
            for i in range(NT):
                xt = src_fn(i)
                s_ = ss[i % 2]
                h_ = hn[i % 2]
                ht = hT[(i // 4) % 2]
                S.op("dve", lambda e, s_=s_: e.memset(s_[:, 0:1], 0.0), writes=[s_.key])
                S.op("act", lambda e, s_=s_, xt=xt: e.activation(out=junk[:], in_=xt[:], func=AF.Square, accum_out=s_[:, 0:1]),
                     reads=[xt.key], writes=[s_.key, junk.key])
                S.op("act", lambda e, s_=s_: e.activation(out=s_[:, 1:2], in_=s_[:, 0:1], func=AF.Sqrt, scale=1.0 / D, bias=EPS),
                     reads=[s_.key], writes=[s_.key])
                S.op("dve", lambda e, s_=s_: e.reciprocal(out=s_[:, 2:3], in_=s_[:, 1:2]), reads=[s_.key], writes=[s_.key])
                S.op("dve", lambda e, s_=s_, xt=xt, h_=h_: e.scalar_tensor_tensor(out=h_[:], in0=xt[:], scalar=s_[:, 2:3], in1=gbc[:],
                                                                                 op0=ALU.mult, op1=ALU.mult),
                     reads=[xt.key, s_.key, gbc.key], writes=[h_.key])
                for half in range(2):
                    bk = S.bank()
                    bv = bk.ap.bitcast(BF16)
                    S.group("pe", [lambda e, c=c, half=half, bv=bv, h_=h_: e.transpose(out=bv[:, c * 128:(c + 1) * 128],
                                                                                       in_=h_[:, (half * 8 + c) * 128:(half * 8 + c + 1) * 128],
                                                                                       identity=ident_b) for c in range(8)],
                            reads=[h_.key] + CONST, writes=[bk.key])
                    S.op("act", lambda e, half=half, bv=bv, ht=ht, i=i: e.copy(out=ht[:, half * 8:(half + 1) * 8, (i % 4) * 128:(i % 4 + 1) * 128],
                                                                             in_=bv.rearrange("p (c t) -> p c t", t=128)),
                         reads=[bk.key], writes=[ht.key])
                if post_fn:
                    post_fn(i, xt)
                if i % 4 == 3:
                    tb = i // 4
                    S.dma("sp", rowsT(dstT_d)[:, :, tb * 512:(tb + 1) * 512], ht[:], reads=[ht.key], writes=[phase_tag])

        def fm_rmsnorm(src_ap, src_keys, gcol_ap, gkeys, out_ap, out_keys, tmp):
            sq, rt, ri = tmp
            bk = S.bank()
            S.op("act", lambda e: e.activation(out=sq[:], in_=src_ap, func=AF.Square), reads=src_keys, writes=[sq.key])
            S.op("pe", lambda e: e.matmul(bk.ap, lhsT=ones_f, rhs=sq[:], start=True, stop=True), reads=[sq.key] + CONST, writes=[bk.key])
            S.op("act", lambda e: e.activation(out=rt[:], in_=bk.ap, func=AF.Sqrt, scale=1.0 / HD, bias=EPS), reads=[bk.key], writes=[rt.key])
            S.op("dve", lambda e: e.reciprocal(out=ri[:], in_=rt[:]), reads=[rt.key], writes=[ri.key])
            S.op("dve", lambda e: e.scalar_tensor_tensor(out=out_ap, in0=src_ap, scalar=gcol_ap, in1=ri[:], op0=ALU.mult, op1=ALU.mult),
                 reads=src_keys + [ri.key] + gkeys, writes=out_keys)

        def load_w(dst, src_ap, l):
            S.dma("pool", dst[:], src_ap, writes=[dst.key])

        def proj_fm(bk, wt, hb, n=512, c0=0):
            S.group("pe", [lambda e, kc=kc: e.matmul(bk.ap[:, 0:n], lhsT=wt[:, kc, :], rhs=hb[:, kc, c0:c0 + n], start=(kc == 0), stop=(kc == KC - 1))
                           for kc in range(KC)], reads=[wt.key, hb.key], writes=[bk.key])

        w_in_v = [w_in[l].rearrange("(kc p) c -> p kc c", p=128) for l in range(DEPTH)]

        for l in range(DEPTH):
            xsrc = x_in if l == 0 else y
            AR.reset()
            xts = [AR.tile("xt", [D], F32) for _ in range(2)]

            def src1(i):
                xt = xts[i % 2]
                S.dma("sp", xt[:], xsrc[i * 128:(i + 1) * 128, :], reads=["ydram"], writes=[xt.key])
                return xt
            norm_tiles(l, g1, src1, hnT_d, "hnT")
            S.dma("sp", gv_sb[:, 0:18], gvec[l], writes=["gv"])
            S.op("dve", lambda e: e.tensor_scalar(out=gv_sb[:, 18:19], in0=gv_sb[:, 0:1], scalar1=SCALE, scalar2=None, op0=ALU.mult),
                 reads=["gv"], writes=["gv"])
            S.barrier()

            AR.reset()
            hblk = [AR.tile("hblk", [KC, 512], BF16) for _ in range(2)]
            wts = [AR.tile("wt", [KC, 128], BF16) for _ in range(6)]
            q_sb = AR.tile("q_sb", [T], BF16)
            k_sb = AR.tile("k_sb", [T], BF16)
            qd_sb = AR.tile("qd_sb", [T], BF16)
            v_sb = AR.tile("v_sb", [NT, 128], BF16)
            kd_c = AR.tile("kd_c", [NCH, 128], BF16)
            vr_c = AR.tile("vr_c", [NCH, 128], BF16)
            gs_sb = AR.tile("gs_sb", [T], F32)
            o_sb = AR.tile("o_sb", [T], F32)
            cat_sb = [AR.tile("cat_sb", [T], BF16) for _ in range(2)]
            bias_sb = AR.tile("bias_sb", [5, 128], F32)
            pTs = [AR.tile("pT", [5, 128], BF16) for _ in range(3)]
            e1s = [AR.tile("e1", [5, 128], F32) for _ in range(3)]
            tmpn = [[AR.tile("sq", [512], F32), AR.tile("rt", [512], F32), AR.tile("ri", [512], F32)] for _ in range(2)]
            xs_t = [AR.tile("xs", [512], F32) for _ in range(2)]
            t1_t = [AR.tile("t1", [512], F32) for _ in range(2)]
            t2_t = [AR.tile("t2", [512], F32) for _ in range(2)]
            rot_t = [AR.tile("rot", [512], F32) for _ in range(2)]
            cs_t = [AR.tile("cs", [2, 512], F32) for _ in range(2)]
            rz_t = [AR.tile("rz", [128], F32) for _ in range(2)]
            sm_t = [AR.tile("sm", [64], BF16) for _ in range(3)]
            st_f = AR.tile("st_f", [128], F32)
            st_b = [AR.tile("st_b", [128], BF16) for _ in range(2)]
            nrm_f = [AR.tile("nrm_f", [512], F32) for _ in range(2)]
            cnt = {"hb": 0, "tn": 0, "w": 0, "cat": 0, "pt": 0, "sm": 0, "sb": 0, "x": 0}

            def next_hblk(tb):
                hb = hblk[cnt["hb"] % 2]
                cnt["hb"] += 1
                S.dma("sp", hb[:], rowsT(hnT_d)[:, :, tb * 512:(tb + 1) * 512], reads=["hnT"], writes=[hb.key])
                return hb

            def next_tmp():
                cnt["tn"] += 1
                return tmpn[cnt["tn"] % 2]

            def get_w(col0):
                wt = wts[cnt["w"] % 6]
                cnt["w"] += 1
                load_w(wt, w_in_v[l][:, :, col0:col0 + 128], l)
                return wt

            for h in range(NH):
                wq_t = get_w(h * 128)
                wk_t = get_w(1024 + h * 128)
                wv_t = get_w(2048 + h * 128)
                S.dma("sp", bias_sb[:], biasT[l, h].rearrange("(a p) q -> p a q", p=128), writes=[bias_sb.key])
                for tb in range(NB):
                    hb = next_hblk(tb)
                    for wt, dst, gc in ((wq_t, q_sb, 18), (wk_t, k_sb, 1)):
                        bk = S.bank()
                        proj_fm(bk, wt, hb)
                        fm_rmsnorm(bk.ap, [bk.key], gv_sb[:, gc:gc + 1], ["gv"], dst[:, tb * 512:(tb + 1) * 512], [dst.key], next_tmp())
                    bk = S.bank()
                    for ti in range(4):
                        S.group("pe", [lambda e, kc=kc, ti=ti, bk=bk, hb=hb, wv_t=wv_t: e.matmul(bk.ap[:, ti * 128:(ti + 1) * 128], lhsT=hb[:, kc, ti * 128:(ti + 1) * 128],
                                                                                               rhs=wv_t[:, kc, :], start=(kc == 0), stop=(kc == KC - 1)) for kc in range(KC)],
                                reads=[hb.key, wv_t.key], writes=[bk.key])
                    S.op("act", lambda e, bk=bk, tb=tb: e.copy(out=v_sb[:, tb * 4:(tb + 1) * 4, :], in_=bk.ap.rearrange("p (a d) -> p a d", d=128)),
                         reads=[bk.key], writes=[v_sb.key])
                def att_s1(j):
                    a0 = max(0, 4 - j)
                    pT = pTs[cnt["pt"] % 3]
                    e1 = e1s[cnt["pt"] % 3]
                    cnt["pt"] += 1
                    bkA = S.bank()
                    bkB = S.bank()
                    for a in range(a0, 5):
                        kt = j - 4 + a
                        bk_, col = (bkA, a) if a < 4 else (bkB, 0)
                        S.op("pe", lambda e, bk_=bk_, col=col, kt=kt, j=j: e.matmul(bk_.ap[:, col * 128:(col + 1) * 128], lhsT=k_sb[:, kt * 128:(kt + 1) * 128],
                                                                                  rhs=q_sb[:, j * 128:(j + 1) * 128], start=True, stop=True),
                             reads=[k_sb.key, q_sb.key], writes=[bk_.key])
                    if a0 < 4:
                        S.op("dve", lambda e, a0=a0, bkA=bkA, e1=e1: e.tensor_tensor(out=e1[:, a0:4, :], in0=bkA.ap.rearrange("p (a q) -> p a q", q=128)[:, a0:4, :],
                                                                                    in1=bias_sb[:, a0:4, :], op=ALU.add),
                             reads=[bkA.key, bias_sb.key], writes=[e1.key])
                    S.op("dve", lambda e, bkB=bkB, e1=e1: e.tensor_tensor(out=e1[:, 4, :], in0=bkB.ap[:, 0:128], in1=bias_sb[:, 4, :], op=ALU.add),
                         reads=[bkB.key, bias_sb.key], writes=[e1.key])
                    S.op("act", lambda e, a0=a0, e1=e1, pT=pT: e.activation(out=pT[:, a0:5, :], in_=e1[:, a0:5, :], func=AF.Exp),
                         reads=[e1.key], writes=[pT.key])
                    return a0, pT

                def att_s2(j, a0, pT):
                    bko = S.bank()
                    fns = []
                    for a in range(a0, 5):
                        kt = j - 4 + a
                        fns.append(lambda e, a=a, kt=kt, bko=bko, pT=pT, a0=a0: e.matmul(bko.ap[:, 0:128], lhsT=v_sb[:, kt, :], rhs=pT[:, a, :],
                                                                                        start=(a == a0), stop=(a == 4)))
                    for a in range(a0, 5):
                        fns.append(lambda e, a=a, bko=bko, pT=pT, a0=a0: e.matmul(bko.ap[:, 128:256], lhsT=ones_b, rhs=pT[:, a, :],
                                                                                 start=(a == a0), stop=(a == 4)))
                    S.group("pe", fns, reads=[v_sb.key, pT.key] + CONST, writes=[bko.key])
                    rz = rz_t[j % 2]
                    S.op("dve", lambda e, rz=rz, bko=bko: e.reciprocal(out=rz[:], in_=bko.ap[:, 128:256]), reads=[bko.key], writes=[rz.key])
                    S.op("dve", lambda e, rz=rz, bko=bko, j=j: e.tensor_tensor(out=o_sb[:, j * 128:(j + 1) * 128], in0=bko.ap[:, 0:128], in1=rz[:], op=ALU.mult),
                         reads=[bko.key, rz.key], writes=[o_sb.key])
                pend = att_s1(0)
                for j in range(NT):
                    nxt = att_s1(j + 1) if j + 1 < NT else None
                    att_s2(j, *pend)
                    pend = nxt
                cat = cat_sb[cnt["cat"] % 2]
                cnt["cat"] += 1
                for tb in range(NB):
                    fm_rmsnorm(o_sb[:, tb * 512:(tb + 1) * 512], [o_sb.key], gv_sb[:, 2 + h:3 + h], ["gv"], cat[:, tb * 512:(tb + 1) * 512], [cat.key], next_tmp())
                S.dma("sp", catT_d[h * 128:(h + 1) * 128, :], cat[:], reads=[cat.key], writes=["catT"])

            for h in range(NH):
                wq_t = get_w(3072 + h * 128)
                wk_t = get_w(4096 + h * 128)
                wv_t = get_w(5120 + h * 128)
                wg_t = get_w(6144 + h * 128)
                for tb in range(NB):
                    hb = next_hblk(tb)
                    cs = cs_t[tb % 2]
                    S.dma("sp", cs[:, 0, :], cosT[:, tb * 512:(tb + 1) * 512], writes=[cs.key])
                    S.dma("sp", cs[:, 1, :], sinT[:, tb * 512:(tb + 1) * 512], writes=[cs.key])
                    for which, wt in (("q", wq_t), ("k", wk_t)):
                        bk = S.bank()
                        proj_fm(bk, wt, hb)
                        xs = xs_t[cnt["x"] % 2]
                        t1 = t1_t[cnt["x"] % 2]
                        t2 = t2_t[cnt["x"] % 2]
                        rot = rot_t[cnt["x"] % 2]
                        cnt["x"] += 1
                        S.op("act", lambda e, xs=xs, bk=bk: e.copy(out=xs[:], in_=bk.ap), reads=[bk.key], writes=[xs.key])
                        bk2 = S.bank()
                        S.op("pe", lambda e, bk2=bk2, xs=xs: e.matmul(bk2.ap, lhsT=swap_f, rhs=xs[:], start=True, stop=True), reads=[xs.key] + CONST, writes=[bk2.key])
                        S.op("dve", lambda e, t1=t1, xs=xs, cs=cs: e.tensor_tensor(out=t1[:], in0=xs[:], in1=cs[:, 0, :], op=ALU.mult),
                             reads=[xs.key, cs.key], writes=[t1.key])
                        S.op("dve", lambda e, t2=t2, bk2=bk2, cs=cs: e.tensor_tensor(out=t2[:], in0=bk2.ap, in1=cs[:, 1, :], op=ALU.mult),
                             reads=[bk2.key, cs.key], writes=[t2.key])
                        sl = slice(tb * 512, (tb + 1) * 512)
                        if which == "q":
                            S.op("dve", lambda e, rot=rot, t1=t1, t2=t2: e.tensor_tensor(out=rot[:], in0=t1[:], in1=t2[:], op=ALU.add),
                                 reads=[t1.key, t2.key], writes=[rot.key])
                            S.op("act", lambda e, rot=rot, sl=sl: e.copy(out=q_sb[:, sl], in_=rot[:]), reads=[rot.key], writes=[q_sb.key])
                            S.op("dve", lambda e, rot=rot, sl=sl, h=h: e.tensor_tensor(
                                out=qd_sb[:, sl].rearrange("p (c q) -> p c q", q=64), in0=rot[:].rearrange("p (c q) -> p c q", q=64),
                                in1=cst_sb[:, C_QD + h * 64:C_QD + (h + 1) * 64].unsqueeze(1).broadcast_to([128, 8, 64]), op=ALU.mult),
                                reads=[rot.key] + CONST, writes=[qd_sb.key])
                        else:
                            S.op("dve", lambda e, t1=t1, t2=t2, sl=sl: e.tensor_tensor(out=k_sb[:, sl], in0=t1[:], in1=t2[:], op=ALU.add),
                                 reads=[t1.key, t2.key], writes=[k_sb.key])
                    bk = S.bank()
                    bv = bk.ap.bitcast(BF16)
                    S.group("pe", [lambda e, c=c, bv=bv, tb=tb: e.transpose(out=bv[0:64, c * 128:(c + 1) * 128], in_=k_sb[:, (tb * 8 + c) * 64:(tb * 8 + c + 1) * 64],
                                                                          identity=ident_b) for c in range(8)],
                            reads=[k_sb.key] + CONST, writes=[bk.key])
                    S.op("dve", lambda e, bv=bv, tb=tb, h=h: e.tensor_scalar(out=kd_c[0:64, tb * 8:(tb + 1) * 8, :], in0=bv[0:64, :].rearrange("p (c d) -> p c d", d=128),
                                                                            scalar1=cst_sb[0:64, C_KD + h:C_KD + h + 1], scalar2=None, op0=ALU.mult),
                         reads=[bk.key] + CONST, writes=[kd_c.key])
                    for half in range(2):
                        bk = S.bank()
                        for c in range(4):
                            ch = half * 4 + c
                            S.group("pe", [lambda e, kc=kc, c=c, ch=ch, bk=bk, hb=hb, wv_t=wv_t: e.matmul(bk.ap[0:64, c * 128:(c + 1) * 128], lhsT=hb[:, kc, ch * 64:(ch + 1) * 64],
                                                                                                        rhs=wv_t[:, kc, :], start=(kc == 0), stop=(kc == KC - 1)) for kc in range(KC)],
                                    reads=[hb.key, wv_t.key], writes=[bk.key])
                        S.op("act", lambda e, bk=bk, tb=tb, half=half: e.copy(out=vr_c[0:64, tb * 8 + half * 4:tb * 8 + half * 4 + 4, :],
                                                                             in_=bk.ap[0:64, :].rearrange("p (c d) -> p c d", d=128)),
                             reads=[bk.key], writes=[vr_c.key])
                    bk = S.bank()
                    proj_fm(bk, wg_t, hb)
                    S.op("act", lambda e, bk=bk, tb=tb: e.activation(out=gs_sb[:, tb * 512:(tb + 1) * 512], in_=bk.ap, func=AF.Silu), reads=[bk.key], writes=[gs_sb.key])
                def ret_A(n):
                    csl = slice(n * 64, (n + 1) * 64)
                    sm = sm_t[n % 3]
                    bks = S.bank()
                    S.op("pe", lambda e, bks=bks, csl=csl: e.matmul(bks.ap[0:64, 0:64], lhsT=k_sb[:, csl], rhs=q_sb[:, csl], start=True, stop=True),
                         reads=[k_sb.key, q_sb.key], writes=[bks.key])
                    S.op("dve", lambda e, bks=bks, sm=sm, h=h: e.tensor_tensor(out=sm[0:64, :], in0=bks.ap[0:64, 0:64], in1=cst_sb[0:64, C_DEC + h * 64:C_DEC + (h + 1) * 64], op=ALU.mult),
                         reads=[bks.key] + CONST, writes=[sm.key])

                def ret_C(n):
                    bkv = S.bank()
                    S.op("pe", lambda e, bkv=bkv, n=n: e.matmul(bkv.ap[:, 0:128], lhsT=kd_c[0:64, n, :], rhs=vr_c[0:64, n, :], start=True, stop=True),
                         reads=[kd_c.key, vr_c.key], writes=[bkv.key])
                    if n == 0:
                        S.op("dve", lambda e, bkv=bkv: e.tensor_copy(out=st_f[:], in_=bkv.ap[:, 0:128]), reads=[bkv.key], writes=[st_f.key])
                    else:
                        S.op("dve", lambda e, bkv=bkv, h=h: e.scalar_tensor_tensor(out=st_f[:], in0=st_f[:], scalar=cst_sb[:, C_CD + h:C_CD + h + 1], in1=bkv.ap[:, 0:128],
                                                                                  op0=ALU.mult, op1=ALU.add),
                             reads=[bkv.key, st_f.key] + CONST, writes=[st_f.key])
                    sb = st_b[(n + 1) % 2]
                    S.op("act", lambda e, sb=sb: e.copy(out=sb[:], in_=st_f[:]), reads=[st_f.key], writes=[sb.key])

                def ret_B(n):
                    csl = slice(n * 64, (n + 1) * 64)
                    sm = sm_t[n % 3]
                    bko = S.bank()
                    fns = [lambda e, bko=bko, sm=sm, n=n: e.matmul(bko.ap[:, 0:64], lhsT=vr_c[0:64, n, :], rhs=sm[0:64, :], start=True, stop=(n == 0))]
                    rds = [vr_c.key, sm.key]
                    if n > 0:
                        sb = st_b[n % 2]
                        fns.append(lambda e, bko=bko, sb=sb, csl=csl: e.matmul(bko.ap[:, 0:64], lhsT=sb[:], rhs=qd_sb[:, csl], start=False, stop=True))
                        rds += [sb.key, qd_sb.key]
                    S.group("pe", fns, reads=rds, writes=[bko.key])
                    S.op("act", lambda e, bko=bko, csl=csl: e.copy(out=o_sb[:, csl], in_=bko.ap[:, 0:64]), reads=[bko.key], writes=[o_sb.key])
                ret_A(0)
                if NCH > 1:
                    ret_A(1)
                for n in range(NCH):
                    if n < NCH - 1:
                        ret_C(n)
                    if n + 2 < NCH:
                        ret_A(n + 2)
                    ret_B(n)
                cat = cat_sb[cnt["cat"] % 2]
                cnt["cat"] += 1
                for tb in range(NB):
                    nf = nrm_f[tb % 2]
                    sl = slice(tb * 512, (tb + 1) * 512)
                    fm_rmsnorm(o_sb[:, sl], [o_sb.key], gv_sb[:, 10 + h:11 + h], ["gv"], nf[:], [nf.key], next_tmp())
                    S.op("dve", lambda e, nf=nf, sl=sl, cat=cat: e.tensor_tensor(out=cat[:, sl], in0=nf[:], in1=gs_sb[:, sl], op=ALU.mult),
                         reads=[nf.key, gs_sb.key], writes=[cat.key])
                S.dma("sp", catT_d[(NH + h) * 128:(NH + h + 1) * 128, :], cat[:], reads=[cat.key], writes=["catT"])
            S.barrier()

            AR.reset()
            wob = [AR.tile("wob", [KC, 512], BF16) for _ in range(2)]
            cblk = [AR.tile("cblk", [KC, 512], BF16) for _ in range(2)]
            xp = [AR.tile("xp", [512], F32) for _ in range(4)]
            w_out_v = w_out[l].rearrange("(kc p) c -> p kc c", p=128)
            k3 = 0
            for cb in range(4):
                wo = wob[cb % 2]
                S.dma("pool", wo[:], w_out_v[:, :, cb * 512:(cb + 1) * 512], writes=[wo.key])
                for tb in range(NB):
                    cbk = cblk[(cb * NB + tb) % 2]
                    S.dma("sp", cbk[:], rowsT(catT_d)[:, :, tb * 512:(tb + 1) * 512], reads=["catT"], writes=[cbk.key])
                    for ti in range(4):
                        i = tb * 4 + ti
                        xq = xp[k3 % 4]
                        k3 += 1
                        S.dma("sp", xq[:], xsrc[i * 128:(i + 1) * 128, cb * 512:(cb + 1) * 512], reads=["ydram"], writes=[xq.key])
                        bk = S.bank()
                        S.group("pe", [lambda e, kc=kc, bk=bk, cbk=cbk, ti=ti, wo=wo: e.matmul(bk.ap, lhsT=cbk[:, kc, ti * 128:(ti + 1) * 128], rhs=wo[:, kc, :],
                                                                                             start=(kc == 0), stop=(kc == KC - 1)) for kc in range(KC)],
                                reads=[cbk.key, wo.key], writes=[bk.key])
                        S.op("dve", lambda e, xq=xq, bk=bk: e.tensor_tensor(out=xq[:], in0=bk.ap, in1=xq[:], op=ALU.add), reads=[bk.key, xq.key], writes=[xq.key])
                        S.dma("sp", y[i * 128:(i + 1) * 128, cb * 512:(cb + 1) * 512], xq[:], reads=[xq.key], writes=["y1"])
            S.barrier()

            AR.reset()
            xts = [AR.tile("xt", [D], F32) for _ in range(2)]

            def src2(i):
                xt = xts[i % 2]
                S.dma("sp", xt[:], y[i * 128:(i + 1) * 128, :], writes=[xt.key])
                return xt
            norm_tiles(l, g2, src2, h2T_d, "h2T")
            S.barrier()

            AR.reset()
            HT = min(1024, T)
            NHF = T // HT
            NBH = HT // 512
            h2h = AR.tile("h2h", [KC, HT], BF16)
            wqj = [AR.tile("wqj", [KC, 128], BF16) for _ in range(2)]
            qp_blk = AR.tile("qp_blk", [16, 512], F32)
            sk_sb = AR.tile("sk_sb", [16, 128], F32)
            Gblk = AR.tile("Gblk", [128, 128], BF16)
            s_sb = AR.tile("s_sb", [16, 128], F32)
            m1 = AR.tile("m1", [16, 16], F32)
            idx = AR.tile("idx", [16, 16], U32)
            idxf = AR.tile("idxf", [16, 16], F32)
            wk = [AR.tile("wk", [128], F32) for _ in range(4)]
            cand = AR.tile("cand", [8, 16, 16], F32)
            E = cand
            wk2 = [AR.tile("wk2", [256], F32) for _ in range(4)]
            g16 = AR.tile("g16", [8, 16], F32)
            cidx = AR.tile("cidx", [8, 16], U32)
            cf = AR.tile("cf", [128], F32)
            ci = AR.tile("ci", [128], I32)
            a0t = AR.tile("a0t", [128], F32)
            b0t = AR.tile("b0t", [128], F32)
            ngt = AR.tile("ngt", [128], F32)
            cabf = AR.tile("cabf", [2, 8, 16], F32)
            sel = AR.tile("sel", [3, 128], F32)
            gz = AR.tile("gz", [16], F32)
            tr_sbs = [AR.tile("tr_sb", [3, 128], F32) for _ in range(2)]
            st5 = {"kw": 0, "k4": 0, "tr": 0}
            A4 = [AR.tile("A4", [4, 128], BF16) for _ in range(3)]
            B4 = [AR.tile("B4", [4, 128], BF16) for _ in range(3)]
            ublk = [AR.tile("ublk", [KC, 256], BF16) for _ in range(2)]
            ge_sb = [AR.tile("ge_sb", [HT], BF16) for _ in range(2)]
            S.dma("sp", sk_sb[:], skT[l].rearrange("j p k -> p j k"), writes=[sk_sb.key])
            wq_v = wq[l].rearrange("(kc p) c -> p kc c", p=128)
            uT_v = uT[l].rearrange("(kc p) e -> p kc e", p=128)
            kw = 0
            k4 = 0

            def ugen(hf):
                for eg in range(NEXP // 256):
                    ub = ublk[eg % 2]
                    S.dma("pool", ub[:], uT_v[:, :, eg * 256:(eg + 1) * 256], writes=[ub.key])
                    for ei in range(2):
                        g = eg * 2 + ei
                        ge_ = ge_sb[g % 2]
                        for tbl in range(NBH):
                            sl = slice(tbl * 512, (tbl + 1) * 512)
                            bk = S.bank()
                            S.group("pe", [lambda e, kc=kc, bk=bk, ub=ub, ei=ei, sl=sl: e.matmul(bk.ap, lhsT=ub[:, kc, ei * 128:(ei + 1) * 128], rhs=h2h[:, kc, sl],
                                                                                               start=(kc == 0), stop=(kc == KC - 1)) for kc in range(KC)],
                                    reads=[ub.key, h2h.key], writes=[bk.key])
                            S.op("act", lambda e, bk=bk, ge_=ge_, sl=sl: e.activation(out=ge_[:, sl], in_=bk.ap, func=AF.Gelu), reads=[bk.key], writes=[ge_.key])
                            yield
                        S.dma("sp", geT_d[g * 128:(g + 1) * 128, hf * HT:(hf + 1) * HT], ge_[:], reads=[ge_.key], writes=["geT"])

            for hf in range(NHF):
                for tbl in range(NBH):
                    S.dma("sp", h2h[:, :, tbl * 512:(tbl + 1) * 512], rowsT(h2T_d)[:, :, hf * HT + tbl * 512:hf * HT + (tbl + 1) * 512], reads=["h2T"], writes=[h2h.key])
                ug = ugen(hf)

                def pull(n):
                    for _ in range(n):
                        next(ug, None)
                def qp_block(tbl):
                    for j in range(16):
                        wt = wqj[st5['kw'] % 2]
                        st5['kw'] += 1
                        S.dma("pool", wt[:], wq_v[:, :, j * 128:(j + 1) * 128], writes=[wt.key])
                        bk = S.bank()
                        proj_fm(bk, wt, h2h, 512, tbl * 512)
                        S.op("act", lambda e, bk=bk, j=j: e.copy(out=qp_blk[:, j, :], in_=bk.ap), reads=[bk.key], writes=[qp_blk.key])

                def stageA(tbl, ti):
                    trt = tr_sbs[st5['tr'] % 2]
                    st5['tr'] += 1
                    i = hf * (HT // 128) + tbl * 4 + ti
                    tsl = slice(ti * 128, (ti + 1) * 128)
                    for jb in range(4):
                        bk = S.bank()
                        S.group("pe", [lambda e, jj=jj, jb=jb, bk=bk, tsl=tsl: e.matmul(bk.ap[:, jj * 128:(jj + 1) * 128], lhsT=qp_blk[:, jb * 4 + jj, tsl], rhs=sk_sb[:, jb * 4 + jj, :],
                                                                                      start=True, stop=True) for jj in range(4)],
                                reads=[qp_blk.key, sk_sb.key], writes=[bk.key])
                        S.op("act", lambda e, bk=bk, jb=jb: e.copy(out=s_sb[:, jb * 4:(jb + 1) * 4, :], in_=bk.ap.rearrange("p (a k) -> p a k", k=128)),
                             reads=[bk.key], writes=[s_sb.key])
                    pull(16)
                    for jg in range(4):
                        js = [jg * 4 + q_ for q_ in range(4)]
                        for j in js:
                            S.op("dve", lambda e, j=j: e.max(out=m1[:, j, 0:8], in_=s_sb[:, j, :]), reads=[s_sb.key], writes=[(m1.key, j)])
                        for j in js:
                            w_ = wk[j % 4]
                            S.op("dve", lambda e, j=j, w_=w_: e.match_replace(out=w_[:], in_to_replace=m1[:, j, 0:8], in_values=s_sb[:, j, :], imm_value=-1e30),
                                 reads=[s_sb.key, (m1.key, j)], writes=[w_.key])
                        for j in js:
                            w_ = wk[j % 4]
                            S.op("dve", lambda e, j=j, w_=w_: e.max(out=m1[:, j, 8:16], in_=w_[:]), reads=[w_.key], writes=[(m1.key, j, 1)])
                        for j in js:
                            S.op("dve", lambda e, j=j: e.max_index(out=idx[:, j, 0:8], in_max=m1[:, j, 0:8], in_values=s_sb[:, j, :]), reads=[s_sb.key, (m1.key, j)], writes=[(idx.key, j)])
                        for j in js:
                            w_ = wk[j % 4]
                            S.op("dve", lambda e, j=j, w_=w_: e.max_index(out=idx[:, j, 8:16], in_max=m1[:, j, 8:16], in_values=w_[:]), reads=[w_.key, (m1.key, j, 1)], writes=[(idx.key, j, 1)])
                    m1keys = [(m1.key, j) for j in range(16)] + [(m1.key, j, 1) for j in range(16)]
                    idxkeys = [(idx.key, j) for j in range(16)] + [(idx.key, j, 1) for j in range(16)]
                    S.op("dve", lambda e: e.tensor_copy(out=idxf[:], in_=idx[:]), reads=idxkeys, writes=[idxf.key])
                    m1v = m1[:].rearrange("p (h two) k -> p h two k", two=2)
                    idv = idxf[:].rearrange("p (h two) k -> p h two k", two=2)
                    S.op("dve", lambda e, m1v=m1v: e.tensor_tensor(out=cand[:], in0=m1v[:, :, 0, :].unsqueeze(3).broadcast_to([128, 8, 16, 16]),
                                                                  in1=m1v[:, :, 1, :].unsqueeze(2).broadcast_to([128, 8, 16, 16]), op=ALU.add),
                         reads=m1keys, writes=[cand.key])
                    for hg in range(2):
                        hs = [hg * 4 + q_ for q_ in range(4)]
                        cvs = {h: cand[:, h].rearrange("p a b -> p (a b)") for h in hs}
                        for h in hs:
                            S.op("dve", lambda e, h=h, cv=cvs[h]: e.max(out=g16[:, h, 0:8], in_=cv), reads=[cand.key], writes=[(g16.key, h)])
                        for h in hs:
                            w_ = wk2[h % 4]
                            S.op("dve", lambda e, h=h, cv=cvs[h], w_=w_: e.match_replace(out=w_[:], in_to_replace=g16[:, h, 0:8], in_values=cv, imm_value=-1e30),
                                 reads=[cand.key, (g16.key, h)], writes=[w_.key])
                        for h in hs:
                            w_ = wk2[h % 4]
                            S.op("dve", lambda e, h=h, w_=w_: e.max(out=g16[:, h, 8:16], in_=w_[:]), reads=[w_.key], writes=[(g16.key, h, 1)])
                        for h in hs:
                            S.op("dve", lambda e, h=h, cv=cvs[h]: e.max_index(out=cidx[:, h, 0:8], in_max=g16[:, h, 0:8], in_values=cv), reads=[cand.key, (g16.key, h)], writes=[(cidx.key, h)])
                        for h in hs:
                            w_ = wk2[h % 4]
                            S.op("dve", lambda e, h=h, w_=w_: e.max_index(out=cidx[:, h, 8:16], in_max=g16[:, h, 8:16], in_values=w_[:]), reads=[w_.key, (g16.key, h, 1)], writes=[(cidx.key, h, 1)])
                    g16keys = [(g16.key, h) for h in range(8)] + [(g16.key, h, 1) for h in range(8)]
                    cidxkeys = [(cidx.key, h) for h in range(8)] + [(cidx.key, h, 1) for h in range(8)]
                    cflat = cidx[:].rearrange("p h k -> p (h k)")
                    ca_v = cabf[:, 0].rearrange("p h k -> p (h k)")
                    cb_v = cabf[:, 1].rearrange("p h k -> p (h k)")
                    S.op("dve", lambda e, cflat=cflat: e.tensor_copy(out=cf[:], in_=cflat), reads=cidxkeys, writes=[cf.key])
                    S.op("dve", lambda e: e.tensor_scalar(out=ci[:], in0=cf[:], scalar1=1.0 / 16.0, scalar2=None, op0=ALU.mult), reads=[cf.key], writes=[ci.key])
                    S.op("dve", lambda e: e.tensor_copy(out=a0t[:], in_=ci[:]), reads=[ci.key], writes=[a0t.key])
                    S.op("dve", lambda e: e.scalar_tensor_tensor(out=b0t[:], in0=a0t[:], scalar=-16.0, in1=cf[:], op0=ALU.mult, op1=ALU.add),
                         reads=[a0t.key, cf.key], writes=[b0t.key])
                    S.op("dve", lambda e: e.tensor_single_scalar(out=ngt[:], in_=b0t[:], scalar=0.0, op=ALU.is_lt), reads=[b0t.key], writes=[ngt.key])
                    S.op("dve", lambda e, ca_v=ca_v: e.tensor_tensor(out=ca_v, in0=a0t[:], in1=ngt[:], op=ALU.subtract), reads=[a0t.key, ngt.key], writes=[(cabf.key, 0)])
                    S.op("dve", lambda e, cb_v=cb_v: e.scalar_tensor_tensor(out=cb_v, in0=ngt[:], scalar=16.0, in1=b0t[:], op0=ALU.mult, op1=ALU.add),
                         reads=[ngt.key, b0t.key], writes=[(cabf.key, 1)])
                    gsel = sel[:, 2, :].rearrange("p (h k) -> p h k", k=16)
                    S.op("dve", lambda e, gsel=gsel: e.tensor_tensor(out=gsel, in0=g16[:], in1=g16[:, :, 0:1].broadcast_to([128, 8, 16]), op=ALU.subtract),
                         reads=g16keys, writes=[(sel.key, 2)])
                    S.op("act", lambda e, gsel=gsel: e.activation(out=gsel, in_=gsel, func=AF.Exp), reads=[(sel.key, 2)], writes=[(sel.key, 2)])
                    io4 = iota16.rearrange("p (h k a) -> p h k a", k=16, a=16)
                    for p_ in range(2):
                        S.op("dve", lambda e, p_=p_: e.tensor_tensor(out=E[:], in0=io4, in1=cabf[:, p_].unsqueeze(3).broadcast_to([128, 8, 16, 16]), op=ALU.is_equal),
                             reads=[(cabf.key, p_)] + CONST + cidxkeys, writes=[E.key])
                        S.op("dve", lambda e, p_=p_, idv=idv: e.tensor_tensor(out=E[:], in0=E[:], in1=idv[:, :, p_, :].unsqueeze(2).broadcast_to([128, 8, 16, 16]), op=ALU.mult),
                             reads=[idxf.key, E.key], writes=[E.key])
                        S.op("dve", lambda e, p_=p_: e.tensor_reduce(out=sel[:, p_, :], in_=E[:].rearrange("p h k a -> p (h k) a"), axis=AX.X, op=ALU.add),
                             reads=[E.key], writes=[(sel.key, p_)])
                    S.op("dve", lambda e, gsel=gsel: e.tensor_reduce(out=gz[:, 0:8], in_=gsel, axis=AX.X, op=ALU.add), reads=[(sel.key, 2)], writes=[gz.key])
                    S.op("dve", lambda e: e.reciprocal(out=gz[:, 8:16], in_=gz[:, 0:8]), reads=[gz.key], writes=[gz.key])
                    S.op("dve", lambda e, gsel=gsel: e.tensor_tensor(out=gsel, in0=gsel, in1=gz[:, 8:16].unsqueeze(2).broadcast_to([128, 8, 16]), op=ALU.mult),
                         reads=[(sel.key, 2), gz.key], writes=[(sel.key, 2)])
                    bk = S.bank()
                    S.group("pe", [lambda e, c=c, bk=bk: e.transpose(out=bk.ap[:, c * 128:(c + 1) * 128], in_=sel[:, c, :], identity=ident_f) for c in range(3)],
                            reads=[(sel.key, 0), (sel.key, 1), (sel.key, 2)] + CONST, writes=[bk.key])
                    S.op("act", lambda e, bk=bk: e.copy(out=trt[:], in_=bk.ap[:, 0:384].rearrange("p (c t) -> p c t", t=128)), reads=[bk.key], writes=[trt.key])
                    return trt

                def stageB(tbl, ti, trt):
                    i = hf * (HT // 128) + tbl * 4 + ti
                    for t4 in range(32):
                        a4 = A4[st5['k4'] % 3]
                        b4 = B4[st5['k4'] % 3]
                        st5['k4'] += 1
                        fns = []
                        for tt in range(4):
                            t = t4 * 4 + tt
                            fns.append(lambda e, a4=a4, tt=tt, t=t: e.tensor_scalar(out=a4[:, tt, :], in0=iota_b, scalar1=trt[:, 0, t:t + 1], scalar2=trt[:, 2, t:t + 1],
                                                                                 op0=ALU.is_equal, op1=ALU.mult))
                            fns.append(lambda e, b4=b4, tt=tt, t=t: e.tensor_scalar(out=b4[:, tt, :], in0=iota_b, scalar1=trt[:, 1, t:t + 1], scalar2=None,
                                                                                 op0=ALU.is_equal))
                        S.group("dve", fns, reads=[trt.key] + CONST, writes=[a4.key, b4.key])
                        bk = S.bank()
                        S.group("pe", [lambda e, tt=tt, bk=bk, a4=a4, b4=b4: e.matmul(bk.ap[:, tt * 128:(tt + 1) * 128], lhsT=b4[:, tt, :], rhs=a4[:, tt, :], start=True, stop=True)
                                       for tt in range(4)], reads=[a4.key, b4.key], writes=[bk.key])
                        S.op("act", lambda e, bk=bk, t4=t4: e.copy(out=Gblk[:, :, t4 * 4:(t4 + 1) * 4].rearrange("p i t -> p t i"),
                                                                 in_=bk.ap.rearrange("p (t i) -> p t i", i=128)),
                             reads=[bk.key], writes=[Gblk.key])
                        if t4 % 2 == 1:
                            pull(1)
                    for qd4 in range(4):
                        S.dma("sp", G_d.rearrange("a b t -> b a t")[:, qd4 * 32:(qd4 + 1) * 32, i * 128:(i + 1) * 128], Gblk[:, qd4 * 32:(qd4 + 1) * 32, :],
                              reads=[Gblk.key], writes=["Gd"])

                tiles5 = [(tbl, ti) for tbl in range(NBH) for ti in range(4)]
                qp_block(0)
                cur_tr = stageA(*tiles5[0])
                for q5, (tbl, ti) in enumerate(tiles5):
                    nxt_tr = None
                    if q5 + 1 < len(tiles5):
                        ntbl, nti = tiles5[q5 + 1]
                        if ntbl != tbl:
                            stageB(tbl, ti, cur_tr)
                            qp_block(ntbl)
                            cur_tr = stageA(ntbl, nti)
                            continue
                        nxt_tr = stageA(ntbl, nti)
                    stageB(tbl, ti, cur_tr)
                    cur_tr = nxt_tr
                for _ in ug:
                    pass
            S.barrier()

            AR.reset()
            ga4 = [AR.tile("ga4", [4, TG * 128], BF16) for _ in range(3)]
            G4 = [AR.tile("G4", [4, TG * 128], BF16) for _ in range(2)]
            v4 = [AR.tile("v4", [4, 512], BF16) for _ in range(3)]
            xo = [AR.tile("xo", [512], F32) for _ in range(4)]
            gaT_v = gaT_d.rearrange("(g p) t -> p g t", p=128)
            geT_v = geT_d.rearrange("(g p) t -> p g t", p=128)
            pv_v = pv[l].rearrange("(g p) d -> p g d", p=128)
            k7 = 0
            k8 = 0
            for hf in range(NT // TG):
                tsl = slice(hf * TG * 128, (hf + 1) * TG * 128)
                for cb in range(4):
                    for g4 in range(32):
                        ga_ = ga4[k7 % 3]
                        v_ = v4[k7 % 3]
                        k7 += 1
                        gsl = slice(g4 * 4, (g4 + 1) * 4)
                        if cb == 0:
                            G_ = G4[g4 % 2]
                            S.dma("sp", ga_[:], geT_v[:, gsl, tsl], writes=[ga_.key])
                            S.dma("sp", G_[:], G_d[gsl].rearrange("a b t -> b a t")[:, :, tsl], writes=[G_.key])
                            S.op("dve", lambda e, ga_=ga_, G_=G_: e.tensor_tensor(out=ga_[:], in0=ga_[:], in1=G_[:], op=ALU.mult), reads=[ga_.key, G_.key], writes=[ga_.key])
                            S.dma("act", gaT_v[:, gsl, tsl], ga_[:], reads=[ga_.key], writes=["gaT"])
                        else:
                            S.dma("sp", ga_[:], gaT_v[:, gsl, tsl], reads=["gaT"], writes=[ga_.key])
                        S.dma("pool", v_[:], pv_v[:, gsl, cb * 512:(cb + 1) * 512], writes=[v_.key])
                        fns = []
                        for gi in range(4):
                            g = g4 * 4 + gi
                            for tt in range(TG):
                                fns.append(lambda e, gi=gi, tt=tt, g=g, ga_=ga_, v_=v_: e.matmul(S.banks[tt].ap, lhsT=ga_[:, gi, tt * 128:(tt + 1) * 128], rhs=v_[:, gi, :],
                                                                                           start=(g == 0), stop=(g == 127)))
                        S.group("pe", fns, reads=[ga_.key, v_.key], writes=[S.banks[tt].key for tt in range(TG)])
                    for tt in range(TG):
                        i = hf * TG + tt
                        xq = xo[k8 % 4]
                        k8 += 1
                        S.dma("sp", xq[:], y[i * 128:(i + 1) * 128, cb * 512:(cb + 1) * 512], writes=[xq.key])
                        S.op("dve", lambda e, xq=xq, tt=tt: e.tensor_tensor(out=xq[:], in0=S.banks[tt].ap, in1=xq[:], op=ALU.add),
                             reads=[S.banks[tt].key, xq.key], writes=[xq.key])
                        S.dma("act", y[i * 128:(i + 1) * 128, cb * 512:(cb + 1) * 512], xq[:], reads=[xq.key], writes=["ydram"])
            S.barrier()

        S.finish()
        with nc.Block() as block:
            @block.tensor
            def _(e):
                for f in S.prog["pe"]:
                    f(e)

            @block.scalar
            def _(e):
                for f in S.prog["act"]:
                    f(e)

            @block.vector
            def _(e):
                for f in S.prog["dve"]:
                    f(e)

            @block.gpsimd
            def _(e):
                for f in S.prog["pool"]:
                    f(e)

            @block.sync
            def _(e):
                for f in S.prog["sp"]:
                    f(e)
    return nc


def make_consts(T):
    cst = np.zeros((128, CW), np.float32)
    cst[:, C_ID:C_ID + 128] = np.eye(128, dtype=np.float32)
    k = np.arange(128)
    sw = np.zeros((128, 128), np.float32)
    sw[(k + 64) % 128, k] = 1.0
    cst[:, C_SW:C_SW + 128] = sw
    cst[:, C_ON:C_ON + 128] = 1.0
    cst[:, C_IO:C_IO + 128] = np.arange(128, dtype=np.float32)[None, :]
    cst[:, C_I16:C_I16 + 2048] = (np.arange(2048) % 16).astype(np.float32)[None, :]
    hh = np.arange(8, dtype=np.float32)
    lg = np.log1p(-(np.float32(2.0) ** (-5.0 - hh))).astype(np.float32)
    pos = np.arange(64, dtype=np.float32)
    diff = pos[:, None] - pos[None, :]
    dec = np.where(diff >= 0, np.exp(np.maximum(diff, 0.0)[None] * lg[:, None, None]), 0.0).astype(np.float32)
    decT = np.transpose(dec, (2, 0, 1)) * np.float32(SCALE)
    cst[0:64, C_DEC:C_DEC + 512] = decT.reshape(64, 512)
    qd = np.exp((pos + 1.0)[None, :] * lg[:, None]).astype(np.float32)
    cst[:, C_QD:C_QD + 512] = qd.reshape(1, 512)
    kd = (np.exp((63.0 - pos)[None, :] * lg[:, None]) * SCALE).astype(np.float32)
    cst[0:64, C_KD:C_KD + 8] = kd.T
    cst[64:128, C_KD:C_KD + 8] = kd.T
    cst[:, C_CD:C_CD + 8] = np.exp(64.0 * lg)[None, :]
    half = 64
    inv_freq = (np.float32(10000.0) ** (-np.arange(half, dtype=np.float32) / half)).astype(np.float32)
    ang = np.arange(T, dtype=np.float32)[:, None] * inv_freq[None, :]
    cos = np.cos(ang).astype(np.float32).T
    sin = np.sin(ang).astype(np.float32).T
    cosT = np.ascontiguousarray(np.concatenate([cos, cos], 0))
    sinT = np.ascontiguousarray(np.concatenate([-sin, sin], 0))
    return cst, cosT, sinT


def bias_layout(rel_bias):
    kk = np.arange(640)[:, None]
    qq = np.arange(128)[None, :]
    rel = np.clip(qq + 512 - kk, -128, 128) + 128
    ck = kk // 64 - 8
    cq = qq // 64
    valid = (ck >= cq - 8) & (ck <= cq)
    b = rel_bias[:, :, rel]
    return np.ascontiguousarray(np.where(valid[None, None], b, np.float32(-30000.0)).astype(np.float32))


def layout_weights(inp, layers):
    L = list(layers)
    d = {}
    d["w_in"] = np.ascontiguousarray(inp["w_in"][L])
    d["w_out"] = np.ascontiguousarray(inp["w_out"][L])
    d["wq"] = np.ascontiguousarray(inp["peer_wq"][L])
    sk = np.asarray(inp["peer_subkeys"])[L]
    d["skT"] = np.ascontiguousarray(np.transpose(sk, (0, 1, 2, 4, 3)).reshape(len(L), 16, 128, 128))
    d["uT"] = np.ascontiguousarray(np.transpose(np.asarray(inp["peer_u"])[L], (0, 2, 1)))
    d["pv"] = np.ascontiguousarray(inp["peer_v"][L])
    d["g1"] = np.ascontiguousarray(inp["norm1_g"][L])
    d["g2"] = np.ascontiguousarray(inp["norm2_g"][L])
    gv = np.zeros((len(L), 128, 18), np.float32)
    gv[:, :, 0] = np.asarray(inp["qa_norm_g"])[L]
    gv[:, :, 1] = np.asarray(inp["ka_norm_g"])[L]
    gv[:, :, 2:10] = np.transpose(np.asarray(inp["attn_out_g"])[L].reshape(len(L), 8, 128), (0, 2, 1))
    gv[:, :, 10:18] = np.transpose(np.asarray(inp["ret_out_g"])[L].reshape(len(L), 8, 128), (0, 2, 1))
    d["gvec"] = gv
    d["biasT"] = bias_layout(np.asarray(inp["rel_bias"])[L])
    return d


_CACHE = {}


def kernel(**inputs):
    inp = {k: np.asarray(v) for k, v in inputs.items()}
    x = inp["x"]
    B, T, _ = x.shape
    DEPTH = inp["w_in"].shape[0]
    key = (T, DEPTH)
    if key not in _CACHE:
        _CACHE[key] = build(T, DEPTH)
    nc = _CACHE[key]
    cst, cosT, sinT = make_consts(T)
    wd = layout_weights(inp, range(DEPTH))
    wd.update(cst=cst, cosT=cosT, sinT=sinT)
    in_maps = []
    for b in range(B):
        m = dict(wd)
        m["x"] = np.ascontiguousarray(x[b])
        in_maps.append(m)
    res = run_bass_kernel_spmd(nc, in_maps, core_ids=list(range(B)))
    return np.stack([res.results[b]["y"] for b in range(B)], 0).astype(np.float32)
```

```python
import numpy as np
import concourse.bass as bass
import concourse.mybir as mybir
from concourse.bass_utils import run_bass_kernel_spmd
from contextlib import ExitStack

F32 = mybir.dt.float32
BF16 = mybir.dt.bfloat16
U32 = mybir.dt.uint32
I32 = mybir.dt.int32
AF = mybir.ActivationFunctionType
ALU = mybir.AluOpType
AX = mybir.AxisListType

D = 2048
KC = 16
HD = 128
NH = 8
EPS = 1e-6
NEXP = 16384
IN_COLS = 7168
SCALE = HD ** -0.5
EPOCH = 20000

C_ID, C_SW, C_ON, C_IO, C_I16, C_DEC, C_QD, C_KD, C_CD = 0, 128, 256, 384, 512, 2560, 3072, 3584, 3592
CW = 3600


class Sched:
    ENGS = ("pe", "act", "dve", "pool", "sp")

    def __init__(self, nc, stack, n_dma=32, n_ep=8):
        self.nc = nc
        self.prog = {e: [] for e in self.ENGS}
        self.esems = {e: [stack.enter_context(nc.semaphore(f"s_{e}_{i}")) for i in range(n_ep)] for e in self.ENGS}
        self.eidx = {e: 0 for e in self.ENGS}
        self.ecnt = {e: 0 for e in self.ENGS}
        self.dsems = [stack.enter_context(nc.semaphore(f"s_dma_{i}")) for i in range(n_dma)]
        self.dval = {s: 0 for s in self.dsems}
        self.di = 0
        self.res = {}
        self.waited = {e: {} for e in self.ENGS}
        self.pe_sems = set(self.esems["pe"])
        self.banks = None
        self.bi = 0
        self.ninstr = 0

    def _deps(self, reads, writes):
        deps = {}

        def add(s, v):
            if deps.get(s, 0) < v:
                deps[s] = v
        for r in reads:
            st = self.res.get(r)
            if st and st[0]:
                add(*st[0])
        for w in writes:
            st = self.res.get(w)
            if st:
                if st[0]:
                    add(*st[0])
                for s, v in st[1].items():
                    add(s, v)
        return deps

    def _waits(self, eng, deps):
        wd = self.waited[eng]
        for s, v in deps.items():
            if eng == "pe" and s in self.pe_sems:
                continue
            if wd.get(s, 0) >= v:
                continue
            wd[s] = v
            self.prog[eng].append(lambda e, s=s, v=v: e.wait_ge(s, v))
            self.ninstr += 1

    def _mark(self, reads, writes, s, v):
        for r in reads:
            st = self.res.setdefault(r, [None, {}])
            if st[1].get(s, 0) < v:
                st[1][s] = v
        for w in writes:
            self.res[w] = [(s, v), {}]

    def _tick(self, eng):
        if self.ecnt[eng] >= EPOCH:
            self.eidx[eng] += 1
            self.ecnt[eng] = 0
        s = self.esems[eng][self.eidx[eng]]
        self.ecnt[eng] += 1
        return s, self.ecnt[eng]

    def group(self, eng, fns, reads=(), writes=()):
        self._waits(eng, self._deps(reads, writes))
        s, v = self._tick(eng)
        n = len(fns)
        for i, fn in enumerate(fns):
            if i == n - 1:
                self.prog[eng].append(lambda e, fn=fn, s=s: fn(e).then_inc(s, 1))
            else:
                self.prog[eng].append(fn)
        self.ninstr += n
        self._mark(reads, writes, s, v)

    def op(self, eng, fn, reads=(), writes=()):
        self.group(eng, [fn], reads, writes)

    def dma(self, q, out, in_, reads=(), writes=()):
        s = self.dsems[self.di % len(self.dsems)]
        self.di += 1
        deps = self._deps(reads, writes)
        prev = self.dval[s]
        if prev > 0 and deps.get(s, 0) < prev:
            deps[s] = prev
        self._waits(q, deps)
        self.dval[s] = prev + 16
        self.prog[q].append(lambda e, s=s: e.dma_start(out=out, in_=in_).then_inc(s, 16))
        self.ninstr += 1
        self._mark(reads, writes, s, prev + 16)

    def barrier(self):
        cur = {}
        for e in self.ENGS:
            if self.ecnt[e] > 0:
                cur[self.esems[e][self.eidx[e]]] = self.ecnt[e]
        for s, v in self.dval.items():
            if v > 0:
                cur[s] = v
        for e in self.ENGS:
            wd = self.waited[e]
            for s, v in cur.items():
                if wd.get(s, 0) >= v:
                    continue
                wd[s] = v
                self.prog[e].append(lambda en, s=s, v=v: en.wait_ge(s, v))
        self.res = {}

    def finish(self):
        for s, v in self.dval.items():
            if v > 0 and self.waited["sp"].get(s, 0) < v:
                self.prog["sp"].append(lambda e, s=s, v=v: e.wait_ge(s, v))

    def bank(self):
        b = self.banks[self.bi % 8]
        self.bi += 1
        return b


class Tile:
    def __init__(self, ap, key):
        self.ap = ap
        self.key = key

    def __getitem__(self, k):
        return self.ap[k]


class Arena:
    def __init__(self, big, nbytes):
        self.big = big
        self.nbytes = nbytes
        self.off = 0
        self.n = 0

    def reset(self):
        self.off = 0

    def tile(self, name, free, dtype, parts=128):
        esz = 2 if dtype == BF16 else 4
        n = int(np.prod(free))
        nb = (n * esz + 31) // 32 * 32
        assert self.off + nb <= self.nbytes, f"arena overflow at {name}: {self.off}+{nb}>{self.nbytes}"
        a = self.big[:, self.off // 2:(self.off + n * esz) // 2]
        self.off += nb
        if dtype != BF16:
            a = a.bitcast(dtype)
        if len(free) == 2:
            a = a.rearrange("p (a b) -> p a b", b=free[1])
        elif len(free) == 3:
            a = a.rearrange("p (a b c) -> p a b c", b=free[1], c=free[2])
        if parts != 128:
            a = a[0:parts]
        self.n += 1
        return Tile(a, f"{name}#{self.n}")


def build(T, DEPTH, debug=False):
    NT = T // 128
    NB = T // 512
    NCH = T // 64
    assert T % 512 == 0
    TG = min(8, NT)
    nc = bass.Bass("TRN2", target_bir_lowering=False)

    def din(name, shape, dt=F32):
        return nc.dram_tensor(name, list(shape), dt, kind="ExternalInput").ap()

    x_in = din("x", [T, D])
    w_in = din("w_in", [DEPTH, D, IN_COLS])
    w_out = din("w_out", [DEPTH, D, D])
    wq = din("wq", [DEPTH, D, D])
    skT = din("skT", [DEPTH, 16, 128, 128])
    uT = din("uT", [DEPTH, D, NEXP])
    pv = din("pv", [DEPTH, NEXP, D])
    g1 = din("g1", [DEPTH, D])
    g2 = din("g2", [DEPTH, D])
    gvec = din("gvec", [DEPTH, 128, 18])
    biasT = din("biasT", [DEPTH, NH, 640, 128])
    cst = din("cst", [128, CW])
    cosT = din("cosT", [128, T])
    sinT = din("sinT", [128, T])
    y = nc.dram_tensor("y", [T, D], F32, kind="ExternalOutput").ap()
    skind = "ExternalOutput" if debug else "Internal"
    hnT_d = nc.dram_tensor("hnT_d", [D, T], BF16, kind=skind).ap()
    catT_d = nc.dram_tensor("catT_d", [D, T], BF16, kind=skind).ap()
    h2T_d = nc.dram_tensor("h2T_d", [D, T], BF16, kind=skind).ap()
    G_d = nc.dram_tensor("G_d", [128, 128, T], BF16, kind=skind).ap()
    gaT_d = nc.dram_tensor("gaT_d", [NEXP, T], BF16, kind="Internal").ap()
    geT_d = nc.dram_tensor("geT_d", [NEXP, T], BF16, kind="Internal").ap()

    ARENA_BYTES = 190 * 1024
    with ExitStack() as stack:
        ec = stack.enter_context
        S = Sched(nc, stack)
        cst_sb = ec(nc.sbuf_tensor("cst_sb", [128, CW], F32))
        cbf = ec(nc.sbuf_tensor("cbf", [128, 384], BF16))
        gv_sb = ec(nc.sbuf_tensor("gv_sb", [128, 20], F32))
        big = ec(nc.sbuf_tensor("arena", [128, ARENA_BYTES // 2], BF16))
        S.banks = [Tile(ec(nc.psum_tensor(f"bank{i}", [128, 512], F32))[:], f"bank{i}") for i in range(8)]
        AR = Arena(big, ARENA_BYTES)

        ident_f = cst_sb[:, C_ID:C_ID + 128]
        swap_f = cst_sb[:, C_SW:C_SW + 128]
        ones_f = cst_sb[:, C_ON:C_ON + 128]
        iota_f = cst_sb[:, C_IO:C_IO + 128]
        iota16 = cst_sb[:, C_I16:C_I16 + 2048]
        ident_b = cbf[:, 0:128]
        ones_b = cbf[:, 128:256]
        iota_b = cbf[:, 256:384]
        CST = "cst"

        S.dma("sp", cst_sb[:], cst, writes=[CST])
        S.op("dve", lambda e: e.tensor_copy(out=cbf[:, 0:128], in_=cst_sb[:, C_ID:C_ID + 128]), reads=[CST], writes=["cbf"])
        S.op("dve", lambda e: e.tensor_copy(out=cbf[:, 128:256], in_=cst_sb[:, C_ON:C_ON + 128]), reads=[CST], writes=["cbf"])
        S.op("dve", lambda e: e.tensor_copy(out=cbf[:, 256:384], in_=cst_sb[:, C_IO:C_IO + 128]), reads=[CST], writes=["cbf"])
        CONST = [CST, "cbf"]

        def rowsT(d_ap):
            return d_ap.rearrange("(kc p) t -> p kc t", p=128)

        def norm_tiles(l, gsrc, src_fn, dstT_d, phase_tag, post_fn=None):
            gbc = AR.tile("gbc", [D], F32)
            S.dma("sp", gbc[:], gsrc[l:l + 1, :].broadcast_to([128, D]), writes=[gbc.key])
            junk = AR.tile("junk", [D], BF16)
            hn = [AR.tile("hn", [D], BF16) for _ in range(2)]
            hT = [AR.tile("hT", [KC, 512], BF16) for _ in range(2)]
            ss = [AR.tile("ss", [4], F32) for _ in range(2)]
            for i in range(NT):
                xt = src_fn(i)
                s_ = ss[i % 2]
                h_ = hn[i % 2]
                ht = hT[(i // 4) % 2]
                S.op("dve", lambda e, s_=s_: e.memset(s_[:, 0:1], 0.0), writes=[s_.key])
                S.op("act", lambda e, s_=s_, xt=xt: e.activation(out=junk[:], in_=xt[:], func=AF.Square, accum_out=s_[:, 0:1]),
                     reads=[xt.key], writes=[s_.key, junk.key])
                S.op("act", lambda e, s_=s_: e.activation(out=s_[:, 1:2], in_=s_[:, 0:1], func=AF.Sqrt, scale=1.0 / D, bias=EPS),
                     reads=[s_.key], writes=[s_.key])
                S.op("dve", lambda e, s_=s_: e.reciprocal(out=s_[:, 2:3], in_=s_[:, 1:2]), reads=[s_.key], writes=[s_.key])
                S.op("dve", lambda e, s_=s_, xt=xt, h_=h_: e.scalar_tensor_tensor(out=h_[:], in0=xt[:], scalar=s_[:, 2:3], in1=gbc[:],
                                                                                 op0=ALU.mult, op1=ALU.mult),
                     reads=[xt.key, s_.key, gbc.key], writes=[h_.key])
                for half in range(2):
                    bk = S.bank()
                    bv = bk.ap.bitcast(BF16)
                    S.group("pe", [lambda e, c=c, half=half, bv=bv, h_=h_: e.transpose(out=bv[:, c * 128:(c + 1) * 128],
                                                                                       in_=h_[:, (half * 8 + c) * 128:(half * 8 + c + 1) * 128],
                                                                                       identity=ident_b) for c in range(8)],
                            reads=[h_.key] + CONST, writes=[bk.key])
                    S.op("act", lambda e, half=half, bv=bv, ht=ht, i=i: e.copy(out=ht[:, half * 8:(half + 1) * 8, (i % 4) * 128:(i % 4 + 1) * 128],
                                                                             in_=bv.rearrange("p (c t) -> p c t", t=128)),
                         reads=[bk.key], writes=[ht.key])
                if post_fn:
                    post_fn(i, xt)
                if i % 4 == 3:
                    tb = i // 4
                    S.dma("sp", rowsT(dstT_d)[:, :, tb * 512:(tb + 1) * 512], ht[:], reads=[ht.key], writes=[phase_tag])

        def fm_rmsnorm(src_ap, src_keys, gcol_ap, gkeys, out_ap, out_keys, tmp):
            sq, rt, ri = tmp
            bk = S.bank()
            S.op("act", lambda e: e.activation(out=sq[:], in_=src_ap, func=AF.Square), reads=src_keys, writes=[sq.key])
            S.op("pe", lambda e: e.matmul(bk.ap, lhsT=ones_f, rhs=sq[:], start=True, stop=True), reads=[sq.key] + CONST, writes=[bk.key])
            S.op("act", lambda e: e.activation(out=rt[:], in_=bk.ap, func=AF.Ln, scale=1.0 / HD, bias=EPS), reads=[bk.key], writes=[rt.key])
            S.op("act", lambda e: e.activation(out=ri[:], in_=rt[:], func=AF.Exp, scale=-0.5), reads=[rt.key], writes=[ri.key])
            S.op("dve", lambda e: e.scalar_tensor_tensor(out=out_ap, in0=src_ap, scalar=gcol_ap, in1=ri[:], op0=ALU.mult, op1=ALU.mult),
                 reads=src_keys + [ri.key] + gkeys, writes=out_keys)

        def load_w(dst, src_ap, l):
            S.dma("pool", dst[:], src_ap, writes=[dst.key])

        def proj_fm(bk, wt, hb, n=512, c0=0):
            S.group("pe", [lambda e, kc=kc: e.matmul(bk.ap[:, 0:n], lhsT=wt[:, kc, :], rhs=hb[:, kc, c0:c0 + n], start=(kc == 0), stop=(kc == KC - 1))
                           for kc in range(KC)], reads=[wt.key, hb.key], writes=[bk.key])

        w_in_v = [w_in[l].rearrange("(kc p) c -> p kc c", p=128) for l in range(DEPTH)]

        for l in range(DEPTH):
            xsrc = x_in if l == 0 else y
            AR.reset()
            xts = [AR.tile("xt", [D], F32) for _ in range(2)]

            def src1(i):
                xt = xts[i % 2]
                S.dma("sp", xt[:], xsrc[i * 128:(i + 1) * 128, :], reads=["ydram"], writes=[xt.key])
                return xt
            norm_tiles(l, g1, src1, hnT_d, "hnT")
            S.dma("sp", gv_sb[:, 0:18], gvec[l], writes=["gv"])
            S.op("dve", lambda e: e.tensor_scalar(out=gv_sb[:, 18:19], in0=gv_sb[:, 0:1], scalar1=SCALE, scalar2=None, op0=ALU.mult),
                 reads=["gv"], writes=["gv"])
            S.barrier()

            AR.reset()
            hblk = [AR.tile("hblk", [KC, 512], BF16) for _ in range(2)]
            wts = [AR.tile("wt", [KC, 128], BF16) for _ in range(6)]
            q_sb = AR.tile("q_sb", [T], BF16)
            k_sb = AR.tile("k_sb", [T], BF16)
            qd_sb = AR.tile("qd_sb", [T], BF16)
            v_sb = AR.tile("v_sb", [NT, 128], BF16)
            kd_c = AR.tile("kd_c", [NCH, 128], BF16)
            vr_c = AR.tile("vr_c", [NCH, 128], BF16)
            gs_sb = AR.tile("gs_sb", [T], F32)
            o_sb = AR.tile("o_sb", [T], F32)
            cat_sb = [AR.tile("cat_sb", [T], BF16) for _ in range(2)]
            bias_sb = AR.tile("bias_sb", [5, 128], F32)
            pTs = [AR.tile("pT", [5, 128], BF16) for _ in range(3)]
            e1s = [AR.tile("e1", [5, 128], F32) for _ in range(3)]
            tmpn = [[AR.tile("sq", [512], F32), AR.tile("rt", [512], F32), AR.tile("ri", [512], F32)] for _ in range(2)]
            xs_t = [AR.tile("xs", [512], F32) for _ in range(2)]
            t1_t = [AR.tile("t1", [512], F32) for _ in range(2)]
            t2_t = [AR.tile("t2", [512], F32) for _ in range(2)]
            rot_t = [AR.tile("rot", [512], F32) for _ in range(2)]
            cs_t = [AR.tile("cs", [2, 512], F32) for _ in range(2)]
            rz_t = [AR.tile("rz", [128], F32) for _ in range(2)]
            sm_t = [AR.tile("sm", [64], BF16) for _ in range(3)]
            st_f = AR.tile("st_f", [128], F32)
            st_b = [AR.tile("st_b", [128], BF16) for _ in range(2)]
            nrm_f = [AR.tile("nrm_f", [512], F32) for _ in range(2)]
            cnt = {"hb": 0, "tn": 0, "w": 0, "cat": 0, "pt": 0, "sm": 0, "sb": 0, "x": 0}

            def next_hblk(tb):
                hb = hblk[cnt["hb"] % 2]
                cnt["hb"] += 1
                S.dma("sp", hb[:], rowsT(hnT_d)[:, :, tb * 512:(tb + 1) * 512], reads=["hnT"], writes=[hb.key])
                return hb

            def next_tmp():
                cnt["tn"] += 1
                return tmpn[cnt["tn"] % 2]

            def get_w(col0):
                wt = wts[cnt["w"] % 6]
                cnt["w"] += 1
                load_w(wt, w_in_v[l][:, :, col0:col0 + 128], l)
                return wt

            for h in range(NH):
                wq_t = get_w(h * 128)
                wk_t = get_w(1024 + h * 128)
                wv_t = get_w(2048 + h * 128)
                S.dma("sp", bias_sb[:], biasT[l, h].rearrange("(a p) q -> p a q", p=128), writes=[bias_sb.key])
                for tb in range(NB):
                    hb = next_hblk(tb)
                    for wt, dst, gc in ((wq_t, q_sb, 18), (wk_t, k_sb, 1)):
                        bk = S.bank()
                        proj_fm(bk, wt, hb)
                        fm_rmsnorm(bk.ap, [bk.key], gv_sb[:, gc:gc + 1], ["gv"], dst[:, tb * 512:(tb + 1) * 512], [dst.key], next_tmp())
                    bk = S.bank()
                    for ti in range(4):
                        S.group("pe", [lambda e, kc=kc, ti=ti, bk=bk, hb=hb, wv_t=wv_t: e.matmul(bk.ap[:, ti * 128:(ti + 1) * 128], lhsT=hb[:, kc, ti * 128:(ti + 1) * 128],
                                                                                               rhs=wv_t[:, kc, :], start=(kc == 0), stop=(kc == KC - 1)) for kc in range(KC)],
                                reads=[hb.key, wv_t.key], writes=[bk.key])
                    S.op("act", lambda e, bk=bk, tb=tb: e.copy(out=v_sb[:, tb * 4:(tb + 1) * 4, :], in_=bk.ap.rearrange("p (a d) -> p a d", d=128)),
                         reads=[bk.key], writes=[v_sb.key])
                def att_s1(j):
                    a0 = max(0, 4 - j)
                    pT = pTs[cnt["pt"] % 3]
                    e1 = e1s[cnt["pt"] % 3]
                    cnt["pt"] += 1
                    bkA = S.bank()
                    bkB = S.bank()
                    for a in range(a0, 5):
                        kt = j - 4 + a
                        bk_, col = (bkA, a) if a < 4 else (bkB, 0)
                        S.op("pe", lambda e, bk_=bk_, col=col, kt=kt, j=j: e.matmul(bk_.ap[:, col * 128:(col + 1) * 128], lhsT=k_sb[:, kt * 128:(kt + 1) * 128],
                                                                                  rhs=q_sb[:, j * 128:(j + 1) * 128], start=True, stop=True),
                             reads=[k_sb.key, q_sb.key], writes=[bk_.key])
                    if a0 < 4:
                        S.op("dve", lambda e, a0=a0, bkA=bkA, e1=e1: e.tensor_tensor(out=e1[:, a0:4, :], in0=bkA.ap.rearrange("p (a q) -> p a q", q=128)[:, a0:4, :],
                                                                                    in1=bias_sb[:, a0:4, :], op=ALU.add),
                             reads=[bkA.key, bias_sb.key], writes=[e1.key])
                    S.op("dve", lambda e, bkB=bkB, e1=e1: e.tensor_tensor(out=e1[:, 4, :], in0=bkB.ap[:, 0:128], in1=bias_sb[:, 4, :], op=ALU.add),
                         reads=[bkB.key, bias_sb.key], writes=[e1.key])
                    S.op("act", lambda e, a0=a0, e1=e1, pT=pT: e.activation(out=pT[:, a0:5, :], in_=e1[:, a0:5, :], func=AF.Exp),
                         reads=[e1.key], writes=[pT.key])
                    return a0, pT

                def att_s2(j, a0, pT):
                    bko = S.bank()
                    fns = []
                    for a in range(a0, 5):
                        kt = j - 4 + a
                        fns.append(lambda e, a=a, kt=kt, bko=bko, pT=pT, a0=a0: e.matmul(bko.ap[:, 0:128], lhsT=v_sb[:, kt, :], rhs=pT[:, a, :],
                                                                                        start=(a == a0), stop=(a == 4)))
                    for a in range(a0, 5):
                        fns.append(lambda e, a=a, bko=bko, pT=pT, a0=a0: e.matmul(bko.ap[:, 128:256], lhsT=ones_b, rhs=pT[:, a, :],
                                                                                 start=(a == a0), stop=(a == 4)))
                    S.group("pe", fns, reads=[v_sb.key, pT.key] + CONST, writes=[bko.key])
                    rz = rz_t[j % 2]
                    S.op("dve", lambda e, rz=rz, bko=bko: e.reciprocal(out=rz[:], in_=bko.ap[:, 128:256]), reads=[bko.key], writes=[rz.key])
                    S.op("dve", lambda e, rz=rz, bko=bko, j=j: e.tensor_tensor(out=o_sb[:, j * 128:(j + 1) * 128], in0=bko.ap[:, 0:128], in1=rz[:], op=ALU.mult),
                         reads=[bko.key, rz.key], writes=[o_sb.key])
                pend = att_s1(0)
                for j in range(NT):
                    nxt = att_s1(j + 1) if j + 1 < NT else None
                    att_s2(j, *pend)
                    pend = nxt
                cat = cat_sb[cnt["cat"] % 2]
                cnt["cat"] += 1
                for tb in range(NB):
                    fm_rmsnorm(o_sb[:, tb * 512:(tb + 1) * 512], [o_sb.key], gv_sb[:, 2 + h:3 + h], ["gv"], cat[:, tb * 512:(tb + 1) * 512], [cat.key], next_tmp())
                S.dma("sp", catT_d[h * 128:(h + 1) * 128, :], cat[:], reads=[cat.key], writes=["catT"])

            for h in range(NH):
                wq_t = get_w(3072 + h * 128)
                wk_t = get_w(4096 + h * 128)
                wv_t = get_w(5120 + h * 128)
                wg_t = get_w(6144 + h * 128)
                for tb in range(NB):
                    hb = next_hblk(tb)
                    cs = cs_t[tb % 2]
                    S.dma("sp", cs[:, 0, :], cosT[:, tb * 512:(tb + 1) * 512], writes=[cs.key])
                    S.dma("sp", cs[:, 1, :], sinT[:, tb * 512:(tb + 1) * 512], writes=[cs.key])
                    for which, wt in (("q", wq_t), ("k", wk_t)):
                        bk = S.bank()
                        proj_fm(bk, wt, hb)
                        xs = xs_t[cnt["x"] % 2]
                        t1 = t1_t[cnt["x"] % 2]
                        t2 = t2_t[cnt["x"] % 2]
                        rot = rot_t[cnt["x"] % 2]
                        cnt["x"] += 1
                        S.op("act", lambda e, xs=xs, bk=bk: e.copy(out=xs[:], in_=bk.ap), reads=[bk.key], writes=[xs.key])
                        bk2 = S.bank()
                        S.op("pe", lambda e, bk2=bk2, xs=xs: e.matmul(bk2.ap, lhsT=swap_f, rhs=xs[:], start=True, stop=True), reads=[xs.key] + CONST, writes=[bk2.key])
                        S.op("dve", lambda e, t1=t1, xs=xs, cs=cs: e.tensor_tensor(out=t1[:], in0=xs[:], in1=cs[:, 0, :], op=ALU.mult),
                             reads=[xs.key, cs.key], writes=[t1.key])
                        S.op("dve", lambda e, t2=t2, bk2=bk2, cs=cs: e.tensor_tensor(out=t2[:], in0=bk2.ap, in1=cs[:, 1, :], op=ALU.mult),
                             reads=[bk2.key, cs.key], writes=[t2.key])
                        sl = slice(tb * 512, (tb + 1) * 512)
                        if which == "q":
                            S.op("dve", lambda e, rot=rot, t1=t1, t2=t2: e.tensor_tensor(out=rot[:], in0=t1[:], in1=t2[:], op=ALU.add),
                                 reads=[t1.key, t2.key], writes=[rot.key])
                            S.op("act", lambda e, rot=rot, sl=sl: e.copy(out=q_sb[:, sl], in_=rot[:]), reads=[rot.key], writes=[q_sb.key])
                            S.op("dve", lambda e, rot=rot, sl=sl, h=h: e.tensor_tensor(
                                out=qd_sb[:, sl].rearrange("p (c q) -> p c q", q=64), in0=rot[:].rearrange("p (c q) -> p c q", q=64),
                                in1=cst_sb[:, C_QD + h * 64:C_QD + (h + 1) * 64].unsqueeze(1).broadcast_to([128, 8, 64]), op=ALU.mult),
                                reads=[rot.key] + CONST, writes=[qd_sb.key])
                        else:
                            S.op("dve", lambda e, t1=t1, t2=t2, sl=sl: e.tensor_tensor(out=k_sb[:, sl], in0=t1[:], in1=t2[:], op=ALU.add),
                                 reads=[t1.key, t2.key], writes=[k_sb.key])
                    bk = S.bank()
                    bv = bk.ap.bitcast(BF16)
                    S.group("pe", [lambda e, c=c, bv=bv, tb=tb: e.transpose(out=bv[0:64, c * 128:(c + 1) * 128], in_=k_sb[:, (tb * 8 + c) * 64:(tb * 8 + c + 1) * 64],
                                                                          identity=ident_b) for c in range(8)],
                            reads=[k_sb.key] + CONST, writes=[bk.key])
                    S.op("dve", lambda e, bv=bv, tb=tb, h=h: e.tensor_scalar(out=kd_c[0:64, tb * 8:(tb + 1) * 8, :], in0=bv[0:64, :].rearrange("p (c d) -> p c d", d=128),
                                                                            scalar1=cst_sb[0:64, C_KD + h:C_KD + h + 1], scalar2=None, op0=ALU.mult),
                         reads=[bk.key] + CONST, writes=[kd_c.key])
                    for half in range(2):
                        bk = S.bank()
                        for c in range(4):
                            ch = half * 4 + c
                            S.group("pe", [lambda e, kc=kc, c=c, ch=ch, bk=bk, hb=hb, wv_t=wv_t: e.matmul(bk.ap[0:64, c * 128:(c + 1) * 128], lhsT=hb[:, kc, ch * 64:(ch + 1) * 64],
                                                                                                        rhs=wv_t[:, kc, :], start=(kc == 0), stop=(kc == KC - 1)) for kc in range(KC)],
                                    reads=[hb.key, wv_t.key], writes=[bk.key])
                        S.op("act", lambda e, bk=bk, tb=tb, half=half: e.copy(out=vr_c[0:64, tb * 8 + half * 4:tb * 8 + half * 4 + 4, :],
                                                                             in_=bk.ap[0:64, :].rearrange("p (c d) -> p c d", d=128)),
                             reads=[bk.key], writes=[vr_c.key])
                    bk = S.bank()
                    proj_fm(bk, wg_t, hb)
                    S.op("act", lambda e, bk=bk, tb=tb: e.activation(out=gs_sb[:, tb * 512:(tb + 1) * 512], in_=bk.ap, func=AF.Silu), reads=[bk.key], writes=[gs_sb.key])
                def ret_A(n):
                    csl = slice(n * 64, (n + 1) * 64)
                    sm = sm_t[n % 3]
                    bks = S.bank()
                    S.op("pe", lambda e, bks=bks, csl=csl: e.matmul(bks.ap[0:64, 0:64], lhsT=k_sb[:, csl], rhs=q_sb[:, csl], start=True, stop=True),
                         reads=[k_sb.key, q_sb.key], writes=[bks.key])
                    S.op("dve", lambda e, bks=bks, sm=sm, h=h: e.tensor_tensor(out=sm[0:64, :], in0=bks.ap[0:64, 0:64], in1=cst_sb[0:64, C_DEC + h * 64:C_DEC + (h + 1) * 64], op=ALU.mult),
                         reads=[bks.key] + CONST, writes=[sm.key])

                def ret_C(n):
                    bkv = S.bank()
                    S.op("pe", lambda e, bkv=bkv, n=n: e.matmul(bkv.ap[:, 0:128], lhsT=kd_c[0:64, n, :], rhs=vr_c[0:64, n, :], start=True, stop=True),
                         reads=[kd_c.key, vr_c.key], writes=[bkv.key])
                    if n == 0:
                        S.op("dve", lambda e, bkv=bkv: e.tensor_copy(out=st_f[:], in_=bkv.ap[:, 0:128]), reads=[bkv.key], writes=[st_f.key])
                    else:
                        S.op("dve", lambda e, bkv=bkv, h=h: e.scalar_tensor_tensor(out=st_f[:], in0=st_f[:], scalar=cst_sb[:, C_CD + h:C_CD + h + 1], in1=bkv.ap[:, 0:128],
                                                                                  op0=ALU.mult, op1=ALU.add),
                             reads=[bkv.key, st_f.key] + CONST, writes=[st_f.key])
                    sb = st_b[(n + 1) % 2]
                    S.op("act", lambda e, sb=sb: e.copy(out=sb[:], in_=st_f[:]), reads=[st_f.key], writes=[sb.key])

                def ret_B(n):
                    csl = slice(n * 64, (n + 1) * 64)
                    sm = sm_t[n % 3]
                    bko = S.bank()
                    fns = [lambda e, bko=bko, sm=sm, n=n: e.matmul(bko.ap[:, 0:64], lhsT=vr_c[0:64, n, :], rhs=sm[0:64, :], start=True, stop=(n == 0))]
                    rds = [vr_c.key, sm.key]
                    if n > 0:
                        sb = st_b[n % 2]
                        fns.append(lambda e, bko=bko, sb=sb, csl=csl: e.matmul(bko.ap[:, 0:64], lhsT=sb[:], rhs=qd_sb[:, csl], start=False, stop=True))
                        rds += [sb.key, qd_sb.key]
                    S.group("pe", fns, reads=rds, writes=[bko.key])
                    S.op("act", lambda e, bko=bko, csl=csl: e.copy(out=o_sb[:, csl], in_=bko.ap[:, 0:64]), reads=[bko.key], writes=[o_sb.key])
                ret_A(0)
                if NCH > 1:
                    ret_A(1)
                for n in range(NCH):
                    if n < NCH - 1:
                        ret_C(n)
                    if n + 2 < NCH:
                        ret_A(n + 2)
                    ret_B(n)
                cat = cat_sb[cnt["cat"] % 2]
                cnt["cat"] += 1
                for tb in range(NB):
                    nf = nrm_f[tb % 2]
                    sl = slice(tb * 512, (tb + 1) * 512)
                    fm_rmsnorm(o_sb[:, sl], [o_sb.key], gv_sb[:, 10 + h:11 + h], ["gv"], nf[:], [nf.key], next_tmp())
                    S.op("dve", lambda e, nf=nf, sl=sl, cat=cat: e.tensor_tensor(out=cat[:, sl], in0=nf[:], in1=gs_sb[:, sl], op=ALU.mult),
                         reads=[nf.key, gs_sb.key], writes=[cat.key])
                S.dma("sp", catT_d[(NH + h) * 128:(NH + h + 1) * 128, :], cat[:], reads=[cat.key], writes=["catT"])
            S.barrier()

            AR.reset()
            wob = [AR.tile("wob", [KC, 512], BF16) for _ in range(2)]
            cblk = [AR.tile("cblk", [KC, 512], BF16) for _ in range(2)]
            xp = [AR.tile("xp", [512], F32) for _ in range(4)]
            w_out_v = w_out[l].rearrange("(kc p) c -> p kc c", p=128)
            k3 = 0
            for cb in range(4):
                wo = wob[cb % 2]
                S.dma("pool", wo[:], w_out_v[:, :, cb * 512:(cb + 1) * 512], writes=[wo.key])
                for tb in range(NB):
                    cbk = cblk[(cb * NB + tb) % 2]
                    S.dma("sp", cbk[:], rowsT(catT_d)[:, :, tb * 512:(tb + 1) * 512], reads=["catT"], writes=[cbk.key])
                    for ti in range(4):
                        i = tb * 4 + ti
                        xq = xp[k3 % 4]
                        k3 += 1
                        S.dma("sp", xq[:], xsrc[i * 128:(i + 1) * 128, cb * 512:(cb + 1) * 512], reads=["ydram"], writes=[xq.key])
                        bk = S.bank()
                        S.group("pe", [lambda e, kc=kc, bk=bk, cbk=cbk, ti=ti, wo=wo: e.matmul(bk.ap, lhsT=cbk[:, kc, ti * 128:(ti + 1) * 128], rhs=wo[:, kc, :],
                                                                                             start=(kc == 0), stop=(kc == KC - 1)) for kc in range(KC)],
                                reads=[cbk.key, wo.key], writes=[bk.key])
                        S.op("dve", lambda e, xq=xq, bk=bk: e.tensor_tensor(out=xq[:], in0=bk.ap, in1=xq[:], op=ALU.add), reads=[bk.key, xq.key], writes=[xq.key])
                        S.dma("sp", y[i * 128:(i + 1) * 128, cb * 512:(cb + 1) * 512], xq[:], reads=[xq.key], writes=["y1"])
            S.barrier()

            AR.reset()
            xts = [AR.tile("xt", [D], F32) for _ in range(2)]

            def src2(i):
                xt = xts[i % 2]
                S.dma("sp", xt[:], y[i * 128:(i + 1) * 128, :], writes=[xt.key])
                return xt
            norm_tiles(l, g2, src2, h2T_d, "h2T")
            S.barrier()

            AR.reset()
            HT = min(1024, T)
            NHF = T // HT
            NBH = HT // 512
            h2h = AR.tile("h2h", [KC, HT], BF16)
            wqj = [AR.tile("wqj", [KC, 128], BF16) for _ in range(2)]
            qp_blk = AR.tile("qp_blk", [16, 512], F32)
            sk_sb = AR.tile("sk_sb", [16, 128], F32)
            Gblk = AR.tile("Gblk", [128, 128], BF16)
            s_sb = AR.tile("s_sb", [16, 128], F32)
            m1 = AR.tile("m1", [16, 16], F32)
            idx = AR.tile("idx", [16, 16], U32)
            idxf = AR.tile("idxf", [16, 16], F32)
            wk = [AR.tile("wk", [128], F32) for _ in range(4)]
            cand = AR.tile("cand", [8, 16, 16], F32)
            E = cand
            wk2 = [AR.tile("wk2", [256], F32) for _ in range(4)]
            g16 = AR.tile("g16", [8, 16], F32)
            cidx = AR.tile("cidx", [8, 16], U32)
            cf = AR.tile("cf", [128], F32)
            ci = AR.tile("ci", [128], I32)
            a0t = AR.tile("a0t", [128], F32)
            b0t = AR.tile("b0t", [128], F32)
            ngt = AR.tile("ngt", [128], F32)
            cabf = AR.tile("cabf", [2, 8, 16], F32)
            sel = AR.tile("sel", [3, 128], F32)
            gz = AR.tile("gz", [16], F32)
            tr_sbs = [AR.tile("tr_sb", [3, 128], F32) for _ in range(2)]
            st5 = {"kw": 0, "k4": 0, "tr": 0}
            A4 = [AR.tile("A4", [4, 128], BF16) for _ in range(3)]
            B4 = [AR.tile("B4", [4, 128], BF16) for _ in range(3)]
            ublk = [AR.tile("ublk", [KC, 256], BF16) for _ in range(2)]
            ge_sb = [AR.tile("ge_sb", [HT], BF16) for _ in range(2)]
            S.dma("sp", sk_sb[:], skT[l].rearrange("j p k -> p j k"), writes=[sk_sb.key])
            wq_v = wq[l].rearrange("(kc p) c -> p kc c", p=128)
            uT_v = uT[l].rearrange("(kc p) e -> p kc e", p=128)
            kw = 0
            k4 = 0

            def ugen(hf):
                for eg in range(NEXP // 256):
                    ub = ublk[eg % 2]
                    S.dma("pool", ub[:], uT_v[:, :, eg * 256:(eg + 1) * 256], writes=[ub.key])
                    for ei in range(2):
                        g = eg * 2 + ei
                        ge_ = ge_sb[g % 2]
                        for tbl in range(NBH):
                            sl = slice(tbl * 512, (tbl + 1) * 512)
                            bk = S.bank()
                            S.group("pe", [lambda e, kc=kc, bk=bk, ub=ub, ei=ei, sl=sl: e.matmul(bk.ap, lhsT=ub[:, kc, ei * 128:(ei + 1) * 128], rhs=h2h[:, kc, sl],
                                                                                               start=(kc == 0), stop=(kc == KC - 1)) for kc in range(KC)],
                                    reads=[ub.key, h2h.key], writes=[bk.key])
                            S.op("act", lambda e, bk=bk, ge_=ge_, sl=sl: e.activation(out=ge_[:, sl], in_=bk.ap, func=AF.Gelu), reads=[bk.key], writes=[ge_.key])
                            yield
                        S.dma("sp", geT_d[g * 128:(g + 1) * 128, hf * HT:(hf + 1) * HT], ge_[:], reads=[ge_.key], writes=["geT"])

            for hf in range(NHF):
                for tbl in range(NBH):
                    S.dma("sp", h2h[:, :, tbl * 512:(tbl + 1) * 512], rowsT(h2T_d)[:, :, hf * HT + tbl * 512:hf * HT + (tbl + 1) * 512], reads=["h2T"], writes=[h2h.key])
                ug = ugen(hf)

                def pull(n):
                    for _ in range(n):
                        next(ug, None)
                def qp_block(tbl):
                    for j in range(16):
                        wt = wqj[st5['kw'] % 2]
                        st5['kw'] += 1
                        S.dma("pool", wt[:], wq_v[:, :, j * 128:(j + 1) * 128], writes=[wt.key])
                        bk = S.bank()
                        proj_fm(bk, wt, h2h, 512, tbl * 512)
                        S.op("act", lambda e, bk=bk, j=j: e.copy(out=qp_blk[:, j, :], in_=bk.ap), reads=[bk.key], writes=[qp_blk.key])

                def stageA(tbl, ti):
                    trt = tr_sbs[st5['tr'] % 2]
                    st5['tr'] += 1
                    i = hf * (HT // 128) + tbl * 4 + ti
                    tsl = slice(ti * 128, (ti + 1) * 128)
                    for jb in range(4):
                        bk = S.bank()
                        S.group("pe", [lambda e, jj=jj, jb=jb, bk=bk, tsl=tsl: e.matmul(bk.ap[:, jj * 128:(jj + 1) * 128], lhsT=qp_blk[:, jb * 4 + jj, tsl], rhs=sk_sb[:, jb * 4 + jj, :],
                                                                                      start=True, stop=True) for jj in range(4)],
                                reads=[qp_blk.key, sk_sb.key], writes=[bk.key])
                        S.op("act", lambda e, bk=bk, jb=jb: e.copy(out=s_sb[:, jb * 4:(jb + 1) * 4, :], in_=bk.ap.rearrange("p (a k) -> p a k", k=128)),
                             reads=[bk.key], writes=[s_sb.key])
                    pull(16)
                    for jg in range(4):
                        js = [jg * 4 + q_ for q_ in range(4)]
                        for j in js:
                            S.op("dve", lambda e, j=j: e.max(out=m1[:, j, 0:8], in_=s_sb[:, j, :]), reads=[s_sb.key], writes=[(m1.key, j)])
                        for j in js:
                            w_ = wk[j % 4]
                            S.op("dve", lambda e, j=j, w_=w_: e.match_replace(out=w_[:], in_to_replace=m1[:, j, 0:8], in_values=s_sb[:, j, :], imm_value=-1e30),
                                 reads=[s_sb.key, (m1.key, j)], writes=[w_.key])
                        for j in js:
                            w_ = wk[j % 4]
                            S.op("dve", lambda e, j=j, w_=w_: e.max(out=m1[:, j, 8:16], in_=w_[:]), reads=[w_.key], writes=[(m1.key, j, 1)])
                        for j in js:
                            S.op("dve", lambda e, j=j: e.max_index(out=idx[:, j, 0:8], in_max=m1[:, j, 0:8], in_values=s_sb[:, j, :]), reads=[s_sb.key, (m1.key, j)], writes=[(idx.key, j)])
                        for j in js:
                            w_ = wk[j % 4]
                            S.op("dve", lambda e, j=j, w_=w_: e.max_index(out=idx[:, j, 8:16], in_max=m1[:, j, 8:16], in_values=w_[:]), reads=[w_.key, (m1.key, j, 1)], writes=[(idx.key, j, 1)])
                    m1keys = [(m1.key, j) for j in range(16)] + [(m1.key, j, 1) for j in range(16)]
                    idxkeys = [(idx.key, j) for j in range(16)] + [(idx.key, j, 1) for j in range(16)]
                    S.op("dve", lambda e: e.tensor_copy(out=idxf[:], in_=idx[:]), reads=idxkeys, writes=[idxf.key])
                    m1v = m1[:].rearrange("p (h two) k -> p h two k", two=2)
                    idv = idxf[:].rearrange("p (h two) k -> p h two k", two=2)
                    S.op("dve", lambda e, m1v=m1v: e.tensor_tensor(out=cand[:], in0=m1v[:, :, 0, :].unsqueeze(3).broadcast_to([128, 8, 16, 16]),
                                                                  in1=m1v[:, :, 1, :].unsqueeze(2).broadcast_to([128, 8, 16, 16]), op=ALU.add),
                         reads=m1keys, writes=[cand.key])
                    for hg in range(2):
                        hs = [hg * 4 + q_ for q_ in range(4)]
                        cvs = {h: cand[:, h].rearrange("p a b -> p (a b)") for h in hs}
                        for h in hs:
                            S.op("dve", lambda e, h=h, cv=cvs[h]: e.max(out=g16[:, h, 0:8], in_=cv), reads=[cand.key], writes=[(g16.key, h)])
                        for h in hs:
                            w_ = wk2[h % 4]
                            S.op("dve", lambda e, h=h, cv=cvs[h], w_=w_: e.match_replace(out=w_[:], in_to_replace=g16[:, h, 0:8], in_values=cv, imm_value=-1e30),
                                 reads=[cand.key, (g16.key, h)], writes=[w_.key])
                        for h in hs:
                            w_ = wk2[h % 4]
                            S.op("dve", lambda e, h=h, w_=w_: e.max(out=g16[:, h, 8:16], in_=w_[:]), reads=[w_.key], writes=[(g16.key, h, 1)])
                        for h in hs:
                            S.op("dve", lambda e, h=h, cv=cvs[h]: e.max_index(out=cidx[:, h, 0:8], in_max=g16[:, h, 0:8], in_values=cv), reads=[cand.key, (g16.key, h)], writes=[(cidx.key, h)])
                        for h in hs:
                            w_ = wk2[h % 4]
                            S.op("dve", lambda e, h=h, w_=w_: e.max_index(out=cidx[:, h, 8:16], in_max=g16[:, h, 8:16], in_values=w_[:]), reads=[w_.key, (g16.key, h, 1)], writes=[(cidx.key, h, 1)])
                    g16keys = [(g16.key, h) for h in range(8)] + [(g16.key, h, 1) for h in range(8)]
                    cidxkeys = [(cidx.key, h) for h in range(8)] + [(cidx.key, h, 1) for h in range(8)]
                    cflat = cidx[:].rearrange("p h k -> p (h k)")
                    ca_v = cabf[:, 0].rearrange("p h k -> p (h k)")
                    cb_v = cabf[:, 1].rearrange("p h k -> p (h k)")
                    S.op("dve", lambda e, cflat=cflat: e.tensor_copy(out=cf[:], in_=cflat), reads=cidxkeys, writes=[cf.key])
                    S.op("dve", lambda e: e.tensor_scalar(out=ci[:], in0=cf[:], scalar1=1.0 / 16.0, scalar2=None, op0=ALU.mult), reads=[cf.key], writes=[ci.key])
                    S.op("dve", lambda e: e.tensor_copy(out=a0t[:], in_=ci[:]), reads=[ci.key], writes=[a0t.key])
                    S.op("dve", lambda e: e.scalar_tensor_tensor(out=b0t[:], in0=a0t[:], scalar=-16.0, in1=cf[:], op0=ALU.mult, op1=ALU.add),
                         reads=[a0t.key, cf.key], writes=[b0t.key])
                    S.op("dve", lambda e: e.tensor_single_scalar(out=ngt[:], in_=b0t[:], scalar=0.0, op=ALU.is_lt), reads=[b0t.key], writes=[ngt.key])
                    S.op("dve", lambda e, ca_v=ca_v: e.tensor_tensor(out=ca_v, in0=a0t[:], in1=ngt[:], op=ALU.subtract), reads=[a0t.key, ngt.key], writes=[(cabf.key, 0)])
                    S.op("dve", lambda e, cb_v=cb_v: e.scalar_tensor_tensor(out=cb_v, in0=ngt[:], scalar=16.0, in1=b0t[:], op0=ALU.mult, op1=ALU.add),
                         reads=[ngt.key, b0t.key], writes=[(cabf.key, 1)])
                    gsel = sel[:, 2, :].rearrange("p (h k) -> p h k", k=16)
                    S.op("dve", lambda e, gsel=gsel: e.tensor_tensor(out=gsel, in0=g16[:], in1=g16[:, :, 0:1].broadcast_to([128, 8, 16]), op=ALU.subtract),
                         reads=g16keys, writes=[(sel.key, 2)])
                    S.op("act", lambda e, gsel=gsel: e.activation(out=gsel, in_=gsel, func=AF.Exp), reads=[(sel.key, 2)], writes=[(sel.key, 2)])
                    io4 = iota16.rearrange("p (h k a) -> p h k a", k=16, a=16)
                    for p_ in range(2):
                        S.op("dve", lambda e, p_=p_: e.tensor_tensor(out=E[:], in0=io4, in1=cabf[:, p_].unsqueeze(3).broadcast_to([128, 8, 16, 16]), op=ALU.is_equal),
                             reads=[(cabf.key, p_)] + CONST + cidxkeys, writes=[E.key])
                        S.op("dve", lambda e, p_=p_, idv=idv: e.tensor_tensor(out=E[:], in0=E[:], in1=idv[:, :, p_, :].unsqueeze(2).broadcast_to([128, 8, 16, 16]), op=ALU.mult),
                             reads=[idxf.key, E.key], writes=[E.key])
                        S.op("dve", lambda e, p_=p_: e.tensor_reduce(out=sel[:, p_, :], in_=E[:].rearrange("p h k a -> p (h k) a"), axis=AX.X, op=ALU.add),
                             reads=[E.key], writes=[(sel.key, p_)])
                    S.op("dve", lambda e, gsel=gsel: e.tensor_reduce(out=gz[:, 0:8], in_=gsel, axis=AX.X, op=ALU.add), reads=[(sel.key, 2)], writes=[gz.key])
                    S.op("dve", lambda e: e.reciprocal(out=gz[:, 8:16], in_=gz[:, 0:8]), reads=[gz.key], writes=[gz.key])
                    S.op("dve", lambda e, gsel=gsel: e.tensor_tensor(out=gsel, in0=gsel, in1=gz[:, 8:16].unsqueeze(2).broadcast_to([128, 8, 16]), op=ALU.mult),
                         reads=[(sel.key, 2), gz.key], writes=[(sel.key, 2)])
                    bk = S.bank()
                    S.group("pe", [lambda e, c=c, bk=bk: e.transpose(out=bk.ap[:, c * 128:(c + 1) * 128], in_=sel[:, c, :], identity=ident_f) for c in range(3)],
                            reads=[(sel.key, 0), (sel.key, 1), (sel.key, 2)] + CONST, writes=[bk.key])
                    S.op("act", lambda e, bk=bk: e.copy(out=trt[:], in_=bk.ap[:, 0:384].rearrange("p (c t) -> p c t", t=128)), reads=[bk.key], writes=[trt.key])
                    return trt

                def stageB(tbl, ti, trt):
                    i = hf * (HT // 128) + tbl * 4 + ti
                    for t4 in range(32):
                        a4 = A4[st5['k4'] % 3]
                        b4 = B4[st5['k4'] % 3]
                        st5['k4'] += 1
                        fns = []
                        for tt in range(4):
                            t = t4 * 4 + tt
                            fns.append(lambda e, a4=a4, tt=tt, t=t: e.tensor_scalar(out=a4[:, tt, :], in0=iota_b, scalar1=trt[:, 0, t:t + 1], scalar2=trt[:, 2, t:t + 1],
                                                                                 op0=ALU.is_equal, op1=ALU.mult))
                            fns.append(lambda e, b4=b4, tt=tt, t=t: e.tensor_scalar(out=b4[:, tt, :], in0=iota_b, scalar1=trt[:, 1, t:t + 1], scalar2=None,
                                                                                 op0=ALU.is_equal))
                        S.group("dve", fns, reads=[trt.key] + CONST, writes=[a4.key, b4.key])
                        bk = S.bank()
                        S.group("pe", [lambda e, tt=tt, bk=bk, a4=a4, b4=b4: e.matmul(bk.ap[:, tt * 128:(tt + 1) * 128], lhsT=b4[:, tt, :], rhs=a4[:, tt, :], start=True, stop=True)
                                       for tt in range(4)], reads=[a4.key, b4.key], writes=[bk.key])
                        S.op("act", lambda e, bk=bk, t4=t4: e.copy(out=Gblk[:, :, t4 * 4:(t4 + 1) * 4].rearrange("p i t -> p t i"),
                                                                 in_=bk.ap.rearrange("p (t i) -> p t i", i=128)),
                             reads=[bk.key], writes=[Gblk.key])
                        if t4 % 2 == 1:
                            pull(1)
                    for qd4 in range(4):
                        S.dma("sp", G_d.rearrange("a b t -> b a t")[:, qd4 * 32:(qd4 + 1) * 32, i * 128:(i + 1) * 128], Gblk[:, qd4 * 32:(qd4 + 1) * 32, :],
                              reads=[Gblk.key], writes=["Gd"])

                tiles5 = [(tbl, ti) for tbl in range(NBH) for ti in range(4)]
                qp_block(0)
                cur_tr = stageA(*tiles5[0])
                for q5, (tbl, ti) in enumerate(tiles5):
                    nxt_tr = None
                    if q5 + 1 < len(tiles5):
                        ntbl, nti = tiles5[q5 + 1]
                        if ntbl != tbl:
                            stageB(tbl, ti, cur_tr)
                            qp_block(ntbl)
                            cur_tr = stageA(ntbl, nti)
                            continue
                        nxt_tr = stageA(ntbl, nti)
                    stageB(tbl, ti, cur_tr)
                    cur_tr = nxt_tr
                for _ in ug:
                    pass
            S.barrier()

            AR.reset()
            ga4 = [AR.tile("ga4", [4, TG * 128], BF16) for _ in range(3)]
            G4 = [AR.tile("G4", [4, TG * 128], BF16) for _ in range(2)]
            v4 = [AR.tile("v4", [4, 512], BF16) for _ in range(3)]
            xo = [AR.tile("xo", [512], F32) for _ in range(4)]
            gaT_v = gaT_d.rearrange("(g p) t -> p g t", p=128)
            geT_v = geT_d.rearrange("(g p) t -> p g t", p=128)
            pv_v = pv[l].rearrange("(g p) d -> p g d", p=128)
            k7 = 0
            k8 = 0
            for hf in range(NT // TG):
                tsl = slice(hf * TG * 128, (hf + 1) * TG * 128)
                for cb in range(4):
                    for g4 in range(32):
                        ga_ = ga4[k7 % 3]
                        v_ = v4[k7 % 3]
                        k7 += 1
                        gsl = slice(g4 * 4, (g4 + 1) * 4)
                        if cb == 0:
                            G_ = G4[g4 % 2]
                            S.dma("sp", ga_[:], geT_v[:, gsl, tsl], writes=[ga_.key])
                            S.dma("sp", G_[:], G_d[gsl].rearrange("a b t -> b a t")[:, :, tsl], writes=[G_.key])
                            S.op("dve", lambda e, ga_=ga_, G_=G_: e.tensor_tensor(out=ga_[:], in0=ga_[:], in1=G_[:], op=ALU.mult), reads=[ga_.key, G_.key], writes=[ga_.key])
                            S.dma("act", gaT_v[:, gsl, tsl], ga_[:], reads=[ga_.key], writes=["gaT"])
                        else:
                            S.dma("sp", ga_[:], gaT_v[:, gsl, tsl], reads=["gaT"], writes=[ga_.key])
                        S.dma("pool", v_[:], pv_v[:, gsl, cb * 512:(cb + 1) * 512], writes=[v_.key])
                        fns = []
                        for gi in range(4):
                            g = g4 * 4 + gi
                            for tt in range(TG):
                                fns.append(lambda e, gi=gi, tt=tt, g=g, ga_=ga_, v_=v_: e.matmul(S.banks[tt].ap, lhsT=ga_[:, gi, tt * 128:(tt + 1) * 128], rhs=v_[:, gi, :],
                                                                                           start=(g == 0), stop=(g == 127)))
                        S.group("pe", fns, reads=[ga_.key, v_.key], writes=[S.banks[tt].key for tt in range(TG)])
                    for tt in range(TG):
                        i = hf * TG + tt
                        xq = xo[k8 % 4]
                        k8 += 1
                        S.dma("sp", xq[:], y[i * 128:(i + 1) * 128, cb * 512:(cb + 1) * 512], writes=[xq.key])
                        S.op("dve", lambda e, xq=xq, tt=tt: e.tensor_tensor(out=xq[:], in0=S.banks[tt].ap, in1=xq[:], op=ALU.add),
                             reads=[S.banks[tt].key, xq.key], writes=[xq.key])
                        S.dma("act", y[i * 128:(i + 1) * 128, cb * 512:(cb + 1) * 512], xq[:], reads=[xq.key], writes=["ydram"])
            S.barrier()

        S.finish()
        with nc.Block() as block:
            @block.tensor
            def _(e):
                for f in S.prog["pe"]:
                    f(e)

            @block.scalar
            def _(e):
                for f in S.prog["act"]:
                    f(e)

            @block.vector
            def _(e):
                for f in S.prog["dve"]:
                    f(e)

            @block.gpsimd
            def _(e):
                for f in S.prog["pool"]:
                    f(e)

            @block.sync
            def _(e):
                for f in S.prog["sp"]:
                    f(e)
    return nc


def make_consts(T):
    cst = np.zeros((128, CW), np.float32)
    cst[:, C_ID:C_ID + 128] = np.eye(128, dtype=np.float32)
    k = np.arange(128)
    sw = np.zeros((128, 128), np.float32)
    sw[(k + 64) % 128, k] = 1.0
    cst[:, C_SW:C_SW + 128] = sw
    cst[:, C_ON:C_ON + 128] = 1.0
    cst[:, C_IO:C_IO + 128] = np.arange(128, dtype=np.float32)[None, :]
    cst[:, C_I16:C_I16 + 2048] = (np.arange(2048) % 16).astype(np.float32)[None, :]
    hh = np.arange(8, dtype=np.float32)
    lg = np.log1p(-(np.float32(2.0) ** (-5.0 - hh))).astype(np.float32)
    pos = np.arange(64, dtype=np.float32)
    diff = pos[:, None] - pos[None, :]
    dec = np.where(diff >= 0, np.exp(np.maximum(diff, 0.0)[None] * lg[:, None, None]), 0.0).astype(np.float32)
    decT = np.transpose(dec, (2, 0, 1)) * np.float32(SCALE)
    cst[0:64, C_DEC:C_DEC + 512] = decT.reshape(64, 512)
    qd = np.exp((pos + 1.0)[None, :] * lg[:, None]).astype(np.float32)
    cst[:, C_QD:C_QD + 512] = qd.reshape(1, 512)
    kd = (np.exp((63.0 - pos)[None, :] * lg[:, None]) * SCALE).astype(np.float32)
    cst[0:64, C_KD:C_KD + 8] = kd.T
    cst[64:128, C_KD:C_KD + 8] = kd.T
    cst[:, C_CD:C_CD + 8] = np.exp(64.0 * lg)[None, :]
    half = 64
    inv_freq = (np.float32(10000.0) ** (-np.arange(half, dtype=np.float32) / half)).astype(np.float32)
    ang = np.arange(T, dtype=np.float32)[:, None] * inv_freq[None, :]
    cos = np.cos(ang).astype(np.float32).T
    sin = np.sin(ang).astype(np.float32).T
    cosT = np.ascontiguousarray(np.concatenate([cos, cos], 0))
    sinT = np.ascontiguousarray(np.concatenate([-sin, sin], 0))
    return cst, cosT, sinT


def bias_layout(rel_bias):
    kk = np.arange(640)[:, None]
    qq = np.arange(128)[None, :]
    rel = np.clip(qq + 512 - kk, -128, 128) + 128
    ck = kk // 64 - 8
    cq = qq // 64
    valid = (ck >= cq - 8) & (ck <= cq)
    b = rel_bias[:, :, rel]
    return np.ascontiguousarray(np.where(valid[None, None], b, np.float32(-30000.0)).astype(np.float32))


def layout_weights(inp, layers):
    L = list(layers)
    d = {}
    d["w_in"] = np.ascontiguousarray(inp["w_in"][L])
    d["w_out"] = np.ascontiguousarray(inp["w_out"][L])
    d["wq"] = np.ascontiguousarray(inp["peer_wq"][L])
    sk = np.asarray(inp["peer_subkeys"])[L]
    d["skT"] = np.ascontiguousarray(np.transpose(sk, (0, 1, 2, 4, 3)).reshape(len(L), 16, 128, 128))
    d["uT"] = np.ascontiguousarray(np.transpose(np.asarray(inp["peer_u"])[L], (0, 2, 1)))
    d["pv"] = np.ascontiguousarray(inp["peer_v"][L])
    d["g1"] = np.ascontiguousarray(inp["norm1_g"][L])
    d["g2"] = np.ascontiguousarray(inp["norm2_g"][L])
    gv = np.zeros((len(L), 128, 18), np.float32)
    gv[:, :, 0] = np.asarray(inp["qa_norm_g"])[L]
    gv[:, :, 1] = np.asarray(inp["ka_norm_g"])[L]
    gv[:, :, 2:10] = np.transpose(np.asarray(inp["attn_out_g"])[L].reshape(len(L), 8, 128), (0, 2, 1))
    gv[:, :, 10:18] = np.transpose(np.asarray(inp["ret_out_g"])[L].reshape(len(L), 8, 128), (0, 2, 1))
    d["gvec"] = gv
    d["biasT"] = bias_layout(np.asarray(inp["rel_bias"])[L])
    return d


_CACHE = {}


def kernel(**inputs):
    inp = {k: np.asarray(v) for k, v in inputs.items()}
    x = inp["x"]
    B, T, _ = x.shape
    DEPTH = inp["w_in"].shape[0]
    key = (T, DEPTH)
    if key not in _CACHE:
        _CACHE[key] = build(T, DEPTH)
    nc = _CACHE[key]
    cst, cosT, sinT = make_consts(T)
    wd = layout_weights(inp, range(DEPTH))
    wd.update(cst=cst, cosT=cosT, sinT=sinT)
    in_maps = []
    for b in range(B):
        m = dict(wd)
        m["x"] = np.ascontiguousarray(x[b])
        in_maps.append(m)
    res = run_bass_kernel_spmd(nc, in_maps, core_ids=list(range(B)))
    return np.stack([res.results[b]["y"] for b in range(B)], 0).astype(np.float32)
```
